# Optimizing a Trainium2 kernel written in Bass

```python
import jax, jax.numpy as jnp
from jax import lax
import numpy as np

D_MODEL = 2048
BATCH = 2
SEQ = 4096
DEPTH = 1

CHUNK = 64
SG_WIDTH = 1024
SG_GROUPS = 4
SG_GROUP_DIM = SG_WIDTH // SG_GROUPS
SG_BLOCK = 128
ATT_HEADS = 8
ATT_HEAD_DIM = 128
ATT_WIDTH = ATT_HEADS * ATT_HEAD_DIM
LEFT_CHUNKS = 8
BAND = LEFT_CHUNKS + 1
REL_CLIP = 128
MEM_LEN = 256
MEM_HEADS = 4
MEM_HEAD_DIM = D_MODEL // MEM_HEADS
PEER_KEYS = 128
PEER_EXPERTS = PEER_KEYS * PEER_KEYS
PEER_HEADS = 8
PEER_QDIM = 256
PEER_HALF = PEER_QDIM // 2
PEER_TOPK = 16
PEER_BLOCK = 128

EPS = 1e-6
NEG_INF = -1e30
IN_SPLITS = [SG_WIDTH, 2 * SG_WIDTH, 2 * SG_WIDTH + ATT_WIDTH, 2 * SG_WIDTH + 2 * ATT_WIDTH,
             2 * SG_WIDTH + 3 * ATT_WIDTH, 2 * SG_WIDTH + 3 * ATT_WIDTH + D_MODEL]
IN_COLS = 2 * SG_WIDTH + 3 * ATT_WIDTH + 2 * D_MODEL

kernel_name = "hybrid_chunk_gmlp_relattn_peer"


def rmsnorm(x, g):
    xf = x.astype(jnp.float32)
    xf = xf * lax.rsqrt(jnp.mean(xf * xf, axis=-1, keepdims=True) + EPS)
    return xf.astype(x.dtype) * g


def layernorm(x, g, b):
    xf = x.astype(jnp.float32)
    mu = jnp.mean(xf, axis=-1, keepdims=True)
    var = jnp.mean(jnp.square(xf - mu), axis=-1, keepdims=True)
    return ((xf - mu) * lax.rsqrt(var + EPS)).astype(x.dtype) * g + b


def spatial_gating_mixer(z_u, z_v, ln_g, ln_b, w_s, b_s):
    B_, S_, _ = z_u.shape
    nb = S_ // SG_BLOCK
    v = layernorm(z_v, ln_g, ln_b).reshape(B_, nb, SG_BLOCK, SG_GROUPS, SG_GROUP_DIM)
    cpos = jnp.arange(SG_BLOCK) // CHUNK
    mask = (cpos[:, None] >= cpos[None, :]).astype(w_s.dtype)
    w = w_s * mask[None]
    s = jnp.einsum('gij,bnjgc->bnigc', w, v) + jnp.transpose(b_s)[:, :, None]
    return z_u * s.reshape(B_, S_, SG_WIDTH)


def chunked_relpos_attention(q, k, v, rel_bias):
    B_, S_, _ = q.shape
    nc = S_ // CHUNK

    def heads(t):
        return t.reshape(B_, nc, CHUNK, ATT_HEADS, ATT_HEAD_DIM)

    q, k, v = heads(q), heads(k), heads(v)
    pad = ((0, 0), (LEFT_CHUNKS, 0), (0, 0), (0, 0), (0, 0))
    kp, vp = jnp.pad(k, pad), jnp.pad(v, pad)
    k_band = jnp.concatenate([kp[:, w:w + nc] for w in range(BAND)], axis=2)
    v_band = jnp.concatenate([vp[:, w:w + nc] for w in range(BAND)], axis=2)
    scale = ATT_HEAD_DIM ** -0.5
    scores = jnp.einsum('bcqhd,bckhd->bhcqk', q, k_band).astype(jnp.float32) * scale
    qi = jnp.arange(CHUNK)
    kj = jnp.arange(BAND * CHUNK)
    dist = (LEFT_CHUNKS * CHUNK + qi[:, None]) - kj[None, :]
    idx = jnp.clip(dist, -REL_CLIP, REL_CLIP) + REL_CLIP
    bias = rel_bias[:, idx].astype(jnp.float32)
    scores = scores + bias[None, :, None]
    chunk_ids = jnp.arange(nc)
    valid = (chunk_ids[:, None] - LEFT_CHUNKS + (kj // CHUNK)[None, :]) >= 0
    scores = jnp.where(valid[None, None, :, None, :], scores, NEG_INF)
    p = jax.nn.softmax(scores, axis=-1).astype(v_band.dtype)
    o = jnp.einsum('bhcqk,bckhd->bcqhd', p, v_band)
    return o.reshape(B_, S_, ATT_WIDTH)


def memory_cross_attention(a, m, w_q, w_kv, w_o):
    B_, S_, _ = a.shape
    q = (a @ w_q).reshape(B_, S_, MEM_HEADS, MEM_HEAD_DIM)
    kv = m @ w_kv
    k = kv[..., :D_MODEL].reshape(B_, -1, MEM_HEADS, MEM_HEAD_DIM)
    v = kv[..., D_MODEL:].reshape(B_, -1, MEM_HEADS, MEM_HEAD_DIM)
    s = jnp.einsum('bshd,bmhd->bhsm', q, k).astype(jnp.float32) * (MEM_HEAD_DIM ** -0.5)
    p = jax.nn.softmax(s, axis=-1).astype(v.dtype)
    o = jnp.einsum('bhsm,bmhd->bshd', p, v).reshape(B_, S_, D_MODEL)
    return o @ w_o


def peer_ffn(a, w_pq, sub_keys, expert_u, expert_v):
    B_, S_, D = a.shape
    t = a.reshape(-1, D)
    T = t.shape[0]
    q = (t @ w_pq).reshape(T, PEER_HEADS, 2, PEER_HALF)
    s = jnp.einsum('thpd,pnd->thpn', q, sub_keys).astype(jnp.float32)
    top_s, top_i = lax.top_k(s, PEER_TOPK)
    cand = (top_s[:, :, 0, :, None] + top_s[:, :, 1, None, :]).reshape(T, PEER_HEADS, PEER_TOPK * PEER_TOPK)
    best_s, best_c = lax.top_k(cand, PEER_TOPK)
    i1 = jnp.take_along_axis(top_i[:, :, 0], best_c // PEER_TOPK, axis=-1)
    i2 = jnp.take_along_axis(top_i[:, :, 1], best_c % PEER_TOPK, axis=-1)
    expert = (i1 * PEER_KEYS + i2).astype(jnp.int32)
    gate = jax.nn.softmax(best_s, axis=-1).astype(t.dtype)
    nt = T // PEER_BLOCK

    def block(args):
        xb, eb, gb = args
        u = expert_u[eb]
        pre = jnp.einsum('bd,bed->be', xb, u)
        act = jax.nn.gelu(pre.astype(jnp.float32), approximate=False).astype(xb.dtype) * gb
        return jnp.einsum('be,bed->bd', act, expert_v[eb])

    out = lax.map(block, (t.reshape(nt, PEER_BLOCK, D),
                          expert.reshape(nt, PEER_BLOCK, PEER_HEADS * PEER_TOPK),
                          gate.reshape(nt, PEER_BLOCK, PEER_HEADS * PEER_TOPK)))
    return out.reshape(B_, S_, D)


def _normal(k, shape, std):
    return jax.random.normal(k, shape, jnp.float32) * std


def setup_inputs(seed: int = 0) -> dict:
    key = jax.random.key(seed)
    ks = jax.random.split(key, 23)
    L, D = DEPTH, D_MODEL
    return {
        "x": _normal(ks[0], (BATCH, SEQ, D), 1.0),
        "mem": _normal(ks[1], (BATCH, MEM_LEN, D), 1.0),
        "g_mix": 1.0 + _normal(ks[2], (L, D), 0.02),
        "w_in": _normal(ks[3], (L, D, IN_COLS), D ** -0.5),
        "sg_ln_g": 1.0 + _normal(ks[4], (L, SG_WIDTH), 0.02),
        "sg_ln_b": _normal(ks[5], (L, SG_WIDTH), 0.02),
        "sg_w_s": _normal(ks[6], (L, SG_GROUPS, SG_BLOCK, SG_BLOCK), 0.5 * SG_BLOCK ** -0.5),
        "sg_b_s": 1.0 + _normal(ks[7], (L, SG_GROUPS, SG_BLOCK), 0.02),
        "att_rel_bias": _normal(ks[8], (L, ATT_HEADS, 2 * REL_CLIP + 1), 0.5),
        "w_up_a": _normal(ks[9], (L, SG_WIDTH, D), SG_WIDTH ** -0.5),
        "w_up_b": _normal(ks[10], (L, ATT_WIDTH, D), ATT_WIDTH ** -0.5),
        "w_out": _normal(ks[11], (L, D, D), D ** -0.5),
        "g_mem_q": 1.0 + _normal(ks[12], (L, D), 0.02),
        "g_mem_kv": 1.0 + _normal(ks[13], (L, D), 0.02),
        "mem_w_q": _normal(ks[14], (L, D, D), D ** -0.5),
        "mem_w_kv": _normal(ks[15], (L, D, 2 * D), D ** -0.5),
        "mem_w_o": _normal(ks[16], (L, D, D), D ** -0.5),
        "g_ffn": 1.0 + _normal(ks[17], (L, D), 0.02),
        "peer_w_q": _normal(ks[18], (L, D, PEER_HEADS * PEER_QDIM), D ** -0.5),
        "peer_sub_keys": _normal(ks[19], (L, 2, PEER_KEYS, PEER_HALF), PEER_HALF ** -0.5),
        "peer_u": _normal(ks[20], (L, PEER_EXPERTS, D), D ** -0.5),
        "peer_v": _normal(ks[21], (L, PEER_EXPERTS, D), PEER_HEADS ** -0.5),
        "g_final": 1.0 + _normal(ks[22], (D,), 0.02),
    }


def reference(x, mem, g_mix, w_in, sg_ln_g, sg_ln_b, sg_w_s, sg_b_s, att_rel_bias, w_up_a, w_up_b, w_out,
              g_mem_q, g_mem_kv, mem_w_q, mem_w_kv, mem_w_o, g_ffn, peer_w_q, peer_sub_keys, peer_u, peer_v,
              g_final):
    h = x
    for l in range(DEPTH):
        a = rmsnorm(h, g_mix[l])
        z = a @ w_in[l]
        z_u, z_v, q, k, v, gate_a, gate_b = jnp.split(z, IN_SPLITS, axis=-1)
        y_a = spatial_gating_mixer(jax.nn.gelu(z_u, approximate=False), jax.nn.gelu(z_v, approximate=False),
                                   sg_ln_g[l], sg_ln_b[l], sg_w_s[l], sg_b_s[l])
        y_b = chunked_relpos_attention(q, k, v, att_rel_bias[l])
        merged = jax.nn.sigmoid(gate_a) * (y_a @ w_up_a[l]) + jax.nn.sigmoid(gate_b) * (y_b @ w_up_b[l])
        h = h + merged @ w_out[l]
        h = h + memory_cross_attention(rmsnorm(h, g_mem_q[l]), rmsnorm(mem, g_mem_kv[l]),
                                       mem_w_q[l], mem_w_kv[l], mem_w_o[l])
        h = h + peer_ffn(rmsnorm(h, g_ffn[l]), peer_w_q[l], peer_sub_keys[l], peer_u[l], peer_v[l])
    return rmsnorm(h, g_final)
```

```python
ENG = ('pe', 'act', 'dve', 'pool', 'sp')


class Tile:
    def __init__(self, prog, name):
        self.name = name
        self.w = {}
        self.r = {}
        self.dsem = None
        prog.tiles.append(self)


class Prog:
    def __init__(self, nc, block, stack):
        self.nc = nc
        self.block = block
        self.stack = stack
        self.bfn = {'pe': block.tensor, 'act': block.scalar, 'dve': block.vector, 'pool': block.gpsimd, 'sp': block.sync}
        self.sem = {e: stack.enter_context(nc.semaphore('s_' + e)) for e in ENG}
        self.ops = {e: [] for e in ENG}
        self.base = {e: 0 for e in ENG}
        self.waited = {e: {} for e in ENG}
        self.tiles = []
        self.dsems = []
        self.nsem = 0
        self.n_instr = 0

    def tile(self, name):
        return Tile(self, name)

    def tiles_n(self, name, n):
        return [Tile(self, '%s%d' % (name, i)) for i in range(n)]

    def _dsem(self, t):
        if t.dsem is None:
            s = self.stack.enter_context(self.nc.semaphore('d%d' % self.nsem))
            self.nsem += 1
            t.dsem = [s, 0]
            self.dsems.append(t.dsem)
        return t.dsem

    @staticmethod
    def _key(d):
        return d[1] if d[0] == 'e' else id(d[1])

    def _collect(self, eng, reads, writes):
        waits = []
        for t in reads:
            for k, d in t.w.items():
                if k == eng and eng == 'pe':
                    continue
                waits.append(d)
        for t in writes:
            for k, d in t.w.items():
                if k == eng:
                    continue
                waits.append(d)
            for k, d in t.r.items():
                if k == eng:
                    continue
                waits.append(d)
        return waits

    def op(self, eng, fn, reads=(), writes=(), inc=None):
        if inc is None:
            inc = (eng != 'pe')
        idx = len(self.ops[eng])
        waits = self._collect(eng, reads, writes)
        me = ('e', eng, idx)
        self.ops[eng].append({'fn': fn, 'waits': waits, 'inc': inc, 'dma': None})
        for t in reads:
            t.r[eng] = me
        for t in writes:
            t.w = {eng: me}
            t.r = {}
        return me

    def dma(self, eng, fn, reads=(), writes=(), part=False):
        assert len(writes) == 1
        wt = writes[0]
        waits = self._collect('dma', reads, () if part else writes)
        ds = self._dsem(wt)
        ds[1] += 16
        me = ('d', ds[0], ds[1])
        self.ops[eng].append({'fn': fn, 'waits': waits, 'inc': False, 'dma': ds[0]})
        for t in reads:
            t.r[id(ds[0])] = me
        wt.w = {id(ds[0]): me}
        wt.r = {}
        return me

    def _resolve_prepare(self):
        for e in ENG:
            for o in self.ops[e]:
                for d in o['waits']:
                    if d[0] == 'e':
                        lst = self.ops[d[1]]
                        found = False
                        for j in range(d[2], len(lst)):
                            if lst[j]['inc']:
                                found = True
                                break
                        if not found:
                            j = len(lst) - 1
                            while j >= d[2] and (lst[j]['dma'] is not None or lst[j]['fn'] is None):
                                j -= 1
                            assert j >= d[2], "no compute op to carry inc"
                            lst[j]['inc'] = True

    def flush(self, barrier=True):
        if barrier:
            self._add_barrier()
        self._resolve_prepare()
        val = {}
        for e in ENG:
            c = self.base[e]
            arr = []
            for o in self.ops[e]:
                if o['inc']:
                    c += 1
                arr.append(c)
            need = [None] * len(arr)
            nxt = None
            for j in range(len(arr) - 1, -1, -1):
                if self.ops[e][j]['inc']:
                    nxt = arr[j]
                need[j] = nxt
            val[e] = need
        prog = self
        for e in ENG:
            ops = self.ops[e]
            if not ops:
                continue

            def body(engine, ops=ops, waited=self.waited[e], semh=self.sem[e]):
                for o in ops:
                    for d in o['waits']:
                        if d[0] == 'e':
                            s = prog.sem[d[1]]
                            v = val[d[1]][d[2]]
                            assert v is not None
                        else:
                            s, v = d[1], d[2]
                        k = id(s)
                        if waited.get(k, 0) >= v:
                            continue
                        waited[k] = v
                        engine.wait_ge(s, v)
                        prog.n_instr += 1
                    if o['fn'] is None:
                        continue
                    ins = o['fn'](engine)
                    prog.n_instr += 1
                    if o['dma'] is not None:
                        ins.then_inc(o['dma'], 16)
                    elif o['inc']:
                        ins.then_inc(semh, 1)
            self.bfn[e](body)
        for e in ENG:
            self.base[e] += sum(1 for o in self.ops[e] if o['inc'])
            self.ops[e] = []
        if barrier:
            for t in self.tiles:
                t.w = {}
                t.r = {}

    def _add_barrier(self):
        lasts = []
        for e in ENG:
            lst = self.ops[e]
            j = len(lst) - 1
            while j >= 0 and (lst[j]['dma'] is not None or lst[j]['fn'] is None):
                j -= 1
            if j >= 0:
                lst[j]['inc'] = True
                lasts.append(('e', e, j))
        dm = [('d', s[0], s[1]) for s in self.dsems if s[1] > 0]
        for e in ENG:
            self.ops[e].append({'fn': None, 'waits': [d for d in lasts if d[1] != e] + dm, 'inc': False, 'dma': None})

import numpy as np
from contextlib import ExitStack
import concourse.bass as bass
import concourse.mybir as mybir
from concourse.bass_utils import run_bass_kernel_spmd

F32 = mybir.dt.float32
BF16 = mybir.dt.bfloat16
AF = mybir.ActivationFunctionType
ALU = mybir.AluOpType
NEG = -1.0e30
EPS = 1e-6


def build(stage=9):
    nc = bass.Bass("TRN2", target_bir_lowering=False)

    def DI(name, shape, dt=F32):
        return nc.dram_tensor(name, shape, dt, kind="ExternalInput")
    x = DI("x", [1536, 2048]); keymask = DI("keymask", [128, 12]); memx = DI("mem", [256, 2048])
    gvecs = DI("gvecs", [5, 2048])
    w_in = DI("w_in", [2048, 9216]); lng = DI("sg_ln_g", [1024]); lnb = DI("sg_ln_b", [1024])
    wsT = DI("sg_wsT", [128, 4, 128]); bsT = DI("sg_bsT", [128, 4]); BTd = DI("att_bt", [128, 8, 5, 128])
    w_up_a = DI("w_up_a", [1024, 2048]); w_up_b = DI("w_up_b", [1024, 2048]); w_out = DI("w_out", [2048, 2048])
    mem_w_q = DI("mem_w_q", [2048, 2048]); mem_w_kv = DI("mem_w_kv", [2048, 4096]); mem_w_o = DI("mem_w_o", [2048, 2048])
    peer_w_q = DI("peer_w_q", [2048, 2048]); keysTd = DI("peer_keysT", [128, 2, 128])
    pu = DI("peer_u", [16384, 2048]); pv = DI("peer_v", [16384, 2048])
    identd = DI("ident", [128, 128]); sel32d = DI("sel32", [128, 32])
    out = nc.dram_tensor("out", [1024, 2048], F32, kind="ExternalOutput")
    Gd = nc.dram_tensor("g_scratch", [1024, 16384], BF16, kind="Internal")

    def wpanel(w, ncols_total, kchunks, c0, ncols):
        return bass.AP(w, c0, [[ncols_total, 128], [128 * ncols_total, kchunks], [1, ncols]])

    KB = 1024
    with ExitStack() as G:
        block = G.enter_context(nc.Block())
        P = Prog(nc, block, G)
        base0 = (nc.sbuf_base + 63) // 64 * 64
        ARENA = 207 * KB
        G.enter_context(nc.sbuf_tensor("arena", [128, ARENA + 64], mybir.dt.uint8))
        uid = [0]

        class Region:
            def __init__(self, start, end):
                self.start, self.end, self.p = start, end, start

            def reset(self):
                self.p = self.start

            def alloc(self, name, shape, dt=F32):
                n = 4 if dt == F32 else 2
                for d in shape[1:]:
                    n *= d
                n = (n + 63) // 64 * 64
                assert self.p + n <= self.end, "region overflow %s need %d have %d" % (name, n, self.end - self.p)
                uid[0] += 1
                t = nc.alloc_sbuf_tensor_at("%s_%d" % (name, uid[0]), shape, dt, offset=base0 + self.p)
                self.p += n
                return t

        PS = lambda st, name, shape, dt=F32: st.enter_context(nc.psum_tensor(name, shape, dt))
        RG = Region(0, 11 * KB)
        RH = Region(11 * KB, 75 * KB)
        R1 = Region(75 * KB, 123 * KB)
        R2 = Region(123 * KB, 155 * KB)
        R3 = Region(155 * KB, 187 * KB)
        R4 = Region(187 * KB, 207 * KB)

        h = RH.alloc("h", [128, 8, 2048]); T_h = P.tiles_n("h", 8)
        identf = RG.alloc("identf", [128, 128]); identb = RG.alloc("identb", [128, 128], BF16)
        onesb = RG.alloc("onesb", [128, 128], BF16); sel32b = RG.alloc("sel32b", [128, 32], BF16)
        gb = RG.alloc("gb", [128, 2048]); T_gb = P.tile("gb")
        T_c = P.tile("consts"); T_c2 = P.tile("consts2"); T_c3 = P.tile("consts3"); T_c4 = P.tile("consts4")
        P.dma('sp', lambda e: e.dma_start(out=identf[:], in_=identd.ap()), writes=[T_c])
        P.dma('pool', lambda e: e.dma_start(out=identb[:], in_=identd.ap()), writes=[T_c2])
        P.dma('pool', lambda e: e.dma_start(out=sel32b[:], in_=sel32d.ap()), writes=[T_c3])
        P.op('dve', lambda e: e.memset(onesb[:], 1.0), writes=[T_c4])
        P.flush()

        def load_gb(i):
            P.dma('sp', lambda e: e.dma_start(out=gb[:], in_=bass.AP(gvecs, i * 2048, [[0, 128], [1, 2048]])), writes=[T_gb])

        def rmsnorm_T(st, R, src_fn, ntiles, dstT, T_dst, col0):
            junk = R.alloc("junk", [128, 2048], BF16); T_j = P.tile("junk")
            stat = R.alloc("stat", [128, 3 * ntiles]); T_st = P.tiles_n("st", ntiles)
            xs = [R.alloc("xs", [128, 2048], BF16) for i in range(2)]; T_xs = P.tiles_n("xs", 2)
            tp = [PS(st, "tp%d_%d" % (i, uid[0]), [128, 8, 128], BF16) for i in range(2)]; T_tp = P.tiles_n("tp", 2)
            for tt in range(ntiles):
                s = tt % 2
                src, Ts = src_fn(tt)
                c = 3 * tt
                P.op('act', lambda e, src=src, c=c: e.activation(out=junk[:], in_=src, func=AF.Square, accum_out=stat[:, c:c + 1]),
                     reads=[Ts], writes=[T_j, T_st[tt]])
                P.op('act', lambda e, c=c: e.activation(out=stat[:, c + 1:c + 2], in_=stat[:, c:c + 1], func=AF.Sqrt, scale=1.0 / 2048, bias=EPS),
                     reads=[T_st[tt]], writes=[T_st[tt]])
                P.op('dve', lambda e, c=c: e.reciprocal(stat[:, c + 2:c + 3], stat[:, c + 1:c + 2]), reads=[T_st[tt]], writes=[T_st[tt]])
                P.op('dve', lambda e, src=src, c=c, s=s: e.scalar_tensor_tensor(out=xs[s][:], in0=src, scalar=stat[:, c + 2:c + 3], in1=gb[:],
                                                                               op0=ALU.mult, op1=ALU.mult),
                     reads=[Ts, T_st[tt], T_gb], writes=[T_xs[s]])
                for hf in range(2):
                    for k in range(8):
                        dc = hf * 8 + k
                        P.op('pe', lambda e, s=s, hf=hf, k=k, dc=dc: e.transpose(out=tp[hf][:, k, :], in_=xs[s][:, dc * 128:(dc + 1) * 128], identity=identb[:]),
                             reads=[T_xs[s], T_c2], writes=[T_tp[hf]], inc=(k == 7))
                    c0 = col0 + tt * 128
                    if hf == 0:
                        P.op('act', lambda e, hf=hf, c0=c0: e.activation(out=dstT[:, hf * 8:(hf + 1) * 8, c0:c0 + 128], in_=tp[hf][:], func=AF.Copy),
                             reads=[T_tp[hf]], writes=[T_dst])
                    else:
                        P.op('dve', lambda e, hf=hf, c0=c0: e.tensor_copy(dstT[:, hf * 8:(hf + 1) * 8, c0:c0 + 128], tp[hf][:]),
                             reads=[T_tp[hf]], writes=[T_dst])

        def proj_feat(st, R, w, wcols, kch, c0, nout_chunks, srcT, T_src, tok0, ntok, evac):
            npan = (nout_chunks + 3) // 4
            wp = [R.alloc("wp", [128, kch, 512], BF16) for i in range(2)]; T_wp = P.tiles_n("wp", 2)
            ps = [PS(st, "pp%d_%d" % (i, uid[0]), [128, 512]) for i in range(2)]; T_ps = P.tiles_n("pp", 2)
            cnt = 0
            for pn in range(npan):
                s = pn % 2
                ncl = min(4, nout_chunks - pn * 4)
                P.dma('pool', lambda e, s=s, pn=pn, ncl=ncl: e.dma_start(out=wp[s][:, :, 0:ncl * 128], in_=wpanel(w, wcols, kch, c0 + pn * 512, ncl * 128)),
                      writes=[T_wp[s]])
                for ocl in range(ncl):
                    oc = pn * 4 + ocl
                    t0 = 0
                    while t0 < ntok:
                        n = min(512, ntok - t0)
                        b = cnt % 2
                        cnt += 1
                        for kc in range(kch):
                            P.op('pe', lambda e, s=s, ocl=ocl, kc=kc, b=b, t0=t0, n=n: e.matmul(ps[b][:, 0:n], lhsT=wp[s][:, kc, ocl * 128:(ocl + 1) * 128],
                                                                                           rhs=srcT[:, kc, tok0 + t0:tok0 + t0 + n], start=(kc == 0), stop=(kc == kch - 1)),
                                 reads=[T_wp[s], T_src], writes=[T_ps[b]], inc=(kc == kch - 1))
                        evac(oc, t0, n, ps[b], T_ps[b], cnt)
                        t0 += n

        def proj_tok(st, R, w, wcols, kch, c0, npan, srcT, T_src, tok0, ntiles, evac):
            wp = [R.alloc("wq", [128, kch, 512], BF16) for i in range(2)]; T_wp = P.tiles_n("wq", 2)
            ps = [PS(st, "pq%d_%d" % (i, uid[0]), [128, 512]) for i in range(2)]; T_ps = P.tiles_n("pq", 2)
            cnt = 0
            for pn in range(npan):
                s = pn % 2
                P.dma('pool', lambda e, s=s, pn=pn: e.dma_start(out=wp[s][:], in_=wpanel(w, wcols, kch, c0 + pn * 512, 512)), writes=[T_wp[s]])
                for tt in range(ntiles):
                    b = cnt % 2
                    cnt += 1
                    for kc in range(kch):
                        P.op('pe', lambda e, s=s, kc=kc, b=b, tt=tt: e.matmul(ps[b][:], lhsT=srcT[:, kc, tok0 + tt * 128:tok0 + (tt + 1) * 128], rhs=wp[s][:, kc, :],
                                                                        start=(kc == 0), stop=(kc == kch - 1)),
                             reads=[T_wp[s], T_src], writes=[T_ps[b]], inc=(kc == kch - 1))
                    evac(pn, tt, ps[b], T_ps[b])

        def add_to_h(pn, tt, ps, T_ps):
            P.op('dve', lambda e, pn=pn, tt=tt, ps=ps: e.tensor_tensor(out=h[:, tt, pn * 512:(pn + 1) * 512], in0=h[:, tt, pn * 512:(pn + 1) * 512], in1=ps[:], op=ALU.add),
                 reads=[T_ps, T_h[tt]], writes=[T_h[tt]])

        def copy_evac(dst_fn, T_dst):
            def ev(oc, t0, n, ps, T_ps, cnt):
                if cnt % 2 == 0:
                    P.op('act', lambda e, oc=oc, t0=t0, n=n, ps=ps: e.activation(out=dst_fn(oc, t0, n), in_=ps[:, 0:n], func=AF.Copy), reads=[T_ps], writes=[T_dst])
                else:
                    P.op('dve', lambda e, oc=oc, t0=t0, n=n, ps=ps: e.tensor_copy(dst_fn(oc, t0, n), ps[:, 0:n]), reads=[T_ps], writes=[T_dst])
            return ev

        def load_x_into_h():
            for tt in range(8):
                P.dma('sp', lambda e, tt=tt: e.dma_start(out=h[:, tt, :], in_=x.ap()[512 + tt * 128:512 + (tt + 1) * 128, :]), writes=[T_h[tt]])

        if stage >= 1:
            for r in (R1, R2, R3, R4):
                r.reset()
            aT = R1.alloc("aT", [128, 16, 1536], BF16); T_aT = P.tile("aT")
            yaT = R2.alloc("yaT", [128, 8, 1024], BF16); T_yaT = P.tile("yaT")
            ybT = R2.alloc("ybT", [128, 8, 1024], BF16); T_ybT = P.tile("ybT")
            mT = R3.alloc("mT", [128, 16, 1024], BF16); T_mT = P.tile("mT")
            RS = Region(11 * KB, 75 * KB)
            with ExitStack() as S:
                RS.reset(); R4.reset()
                load_gb(0)
                xt = [RS.alloc("xt", [128, 2048]) for i in range(2)]; T_xt = P.tiles_n("xt", 2)

                def src_fn(tt):
                    s = tt % 2
                    P.dma('sp', lambda e, tt=tt, s=s: e.dma_start(out=xt[s][:], in_=x.ap()[tt * 128:(tt + 1) * 128, :]), writes=[T_xt[s]])
                    return xt[s][:], T_xt[s]
                rmsnorm_T(S, RS, src_fn, 12, aT, T_aT, 0)
                P.flush()
            with ExitStack() as S:
                RS.reset(); R4.reset()
                RB = Region(155 * KB, 187 * KB)
                gv = RS.alloc("gv", [128, 8, 1024]); T_gv = P.tiles_n("gv", 8)
                gu = RS.alloc("gu", [128, 8, 1024], BF16); T_gu = P.tiles_n("gu", 8)
                lngb = RS.alloc("lngb", [128, 1024]); lnbb = RS.alloc("lnbb", [128, 1024])
                T_ln = P.tile("ln"); T_ln2 = P.tile("ln2")
                wsf = RS.alloc("wsf", [128, 4, 128]); wsb = RS.alloc("wsb", [128, 4, 128], BF16); T_ws = P.tile("ws")
                bs = RS.alloc("bs", [128, 4]); T_bs = P.tile("bs")
                P.dma('sp', lambda e: e.dma_start(out=lngb[:], in_=bass.AP(lng, 0, [[0, 128], [1, 1024]])), writes=[T_ln])
                P.dma('sp', lambda e: e.dma_start(out=lnbb[:], in_=bass.AP(lnb, 0, [[0, 128], [1, 1024]])), writes=[T_ln2])
                P.dma('sp', lambda e: e.dma_start(out=wsf[:], in_=wsT.ap()), writes=[T_ws])
                P.dma('sp', lambda e: e.dma_start(out=bs[:], in_=bsT.ap()), writes=[T_bs])
                P.op('dve', lambda e: e.memset(wsf[64:128, :, 0:64], 0.0), reads=[T_ws], writes=[T_ws])
                P.op('dve', lambda e: e.tensor_copy(wsb[:], wsf[:]), reads=[T_ws], writes=[T_ws])

                def evac_uv(pn, tt, ps, T_ps):
                    if pn < 2:
                        P.op('act', lambda e, pn=pn, tt=tt, ps=ps: e.activation(out=gu[:, tt, pn * 512:(pn + 1) * 512], in_=ps[:], func=AF.Gelu),
                             reads=[T_ps], writes=[T_gu[tt]])
                    else:
                        P.op('act', lambda e, pn=pn, tt=tt, ps=ps: e.activation(out=gv[:, tt, (pn - 2) * 512:(pn - 1) * 512], in_=ps[:], func=AF.Gelu),
                             reads=[T_ps], writes=[T_gv[tt]])
                proj_tok(S, RB, w_in, 9216, 16, 0, 4, aT, T_aT, 512, 8, evac_uv)
                stt_ = R4.alloc("lnstat", [128, 8, 8]); T_lst = P.tiles_n("lnst", 8)
                tmp = R4.alloc("lnt", [128, 1024]); T_tmp = P.tile("lnt")
                tmp2 = R4.alloc("lnu", [128, 1024]); T_tmp2 = P.tile("lnu")
                junk2 = R4.alloc("junk2", [128, 1024], BF16); T_j2 = P.tile("junk2")
                vln = [R4.alloc("vln", [128, 1024], BF16) for i in range(2)]; T_vln = P.tiles_n("vln", 2)
                ya = [R4.alloc("ya", [128, 1024], BF16) for i in range(2)]; T_ya = P.tiles_n("ya", 2)
                sp_ = [PS(S, "sgp%d" % i, [128, 1024]) for i in range(2)]; T_sp = P.tiles_n("sgp", 2)
                ytp = PS(S, "ytp", [128, 8, 128], BF16); T_ytp = P.tile("ytp")
                for tt in range(8):
                    s = tt % 2
                    P.op('dve', lambda e, tt=tt: e.tensor_reduce(out=stt_[:, tt, 0:1], in_=gv[:, tt, :], axis=mybir.AxisListType.X, op=ALU.add),
                         reads=[T_gv[tt]], writes=[T_lst[tt]])
                    P.op('dve', lambda e, tt=tt: e.tensor_scalar(out=stt_[:, tt, 1:2], in0=stt_[:, tt, 0:1], scalar1=1.0 / 1024, scalar2=None, op0=ALU.mult),
                         reads=[T_lst[tt]], writes=[T_lst[tt]])
                    P.op('dve', lambda e, tt=tt: e.tensor_scalar(out=tmp[:], in0=gv[:, tt, :], scalar1=stt_[:, tt, 1:2], scalar2=None, op0=ALU.subtract),
                         reads=[T_gv[tt], T_lst[tt]], writes=[T_tmp])
                    P.op('act', lambda e, tt=tt: e.activation(out=junk2[:], in_=tmp[:], func=AF.Square, accum_out=stt_[:, tt, 2:3]),
                         reads=[T_tmp], writes=[T_j2, T_lst[tt]])
                    P.op('act', lambda e, tt=tt: e.activation(out=stt_[:, tt, 3:4], in_=stt_[:, tt, 2:3], func=AF.Sqrt, scale=1.0 / 1024, bias=EPS),
                         reads=[T_lst[tt]], writes=[T_lst[tt]])
                    P.op('dve', lambda e, tt=tt: e.reciprocal(stt_[:, tt, 4:5], stt_[:, tt, 3:4]), reads=[T_lst[tt]], writes=[T_lst[tt]])
                    P.op('dve', lambda e, tt=tt: e.scalar_tensor_tensor(out=tmp2[:], in0=tmp[:], scalar=stt_[:, tt, 4:5], in1=lngb[:], op0=ALU.mult, op1=ALU.mult),
                         reads=[T_tmp, T_lst[tt], T_ln], writes=[T_tmp2])
                    P.op('dve', lambda e, s=s: e.tensor_tensor(out=vln[s][:], in0=tmp2[:], in1=lnbb[:], op=ALU.add),
                         reads=[T_tmp2, T_ln2], writes=[T_vln[s]])
                    for g in range(4):
                        P.op('pe', lambda e, s=s, g=g: e.matmul(sp_[s][:, g * 256:(g + 1) * 256], lhsT=wsb[:, g, :], rhs=vln[s][:, g * 256:(g + 1) * 256], start=True, stop=True),
                             reads=[T_vln[s], T_ws], writes=[T_sp[s]], inc=(g == 3))
                    for g in range(4):
                        P.op('dve', lambda e, s=s, g=g, tt=tt: e.scalar_tensor_tensor(out=ya[s][:, g * 256:(g + 1) * 256], in0=sp_[s][:, g * 256:(g + 1) * 256], scalar=bs[:, g:g + 1],
                                                                               in1=gu[:, tt, g * 256:(g + 1) * 256], op0=ALU.add, op1=ALU.mult),
                             reads=[T_sp[s], T_bs, T_gu[tt]], writes=[T_ya[s]])
                    for k in range(8):
                        P.op('pe', lambda e, s=s, k=k: e.transpose(out=ytp[:, k, :], in_=ya[s][:, k * 128:(k + 1) * 128], identity=identb[:]),
                             reads=[T_ya[s], T_c2], writes=[T_ytp], inc=(k == 7))
                    P.op('act', lambda e, tt=tt: e.activation(out=yaT[:, :, tt * 128:(tt + 1) * 128], in_=ytp[:], func=AF.Copy), reads=[T_ytp], writes=[T_yaT])
                P.flush()
            with ExitStack() as S:
                RS.reset(); R4.reset()
                wq = [RS.alloc("wqkv", [128, 3, 16, 128], BF16) for i in range(2)]; T_wq = P.tiles_n("wqkv", 2)
                bt = [RS.alloc("bt", [128, 5, 128]) for i in range(2)]; T_bt = P.tiles_n("bt", 2)
                km = RS.alloc("km", [128, 12]); T_km = P.tile("km")
                P.dma('sp', lambda e: e.dma_start(out=km[:], in_=keymask.ap()), writes=[T_km])
                qT = [RS.alloc("qT", [128, 1024], BF16) for i in range(2)]; T_qT = P.tiles_n("qT", 2)
                kT = [RS.alloc("kT", [128, 1536], BF16) for i in range(2)]; T_kT = P.tiles_n("kT", 2)
                vh = [RS.alloc("vh", [128, 12, 128], BF16) for i in range(2)]; T_vh = P.tiles_n("vh", 2)
                pT = [RS.alloc("pT", [128, 5, 128], BF16) for i in range(2)]; T_pT = P.tiles_n("pT", 2)
                rr = [RS.alloc("rr", [128, 128]) for i in range(2)]; T_rr = P.tiles_n("rr", 2)
                pj = [PS(S, "pj%d" % i, [128, 512]) for i in range(2)]; T_pj = P.tiles_n("pj", 2)
                scA = PS(S, "scA", [128, 4, 128]); T_scA = P.tile("scA")
                scB = PS(S, "scB", [128, 4, 128]); T_scB = P.tile("scB")
                po = [PS(S, "po%d" % i, [128, 4, 128]) for i in range(2)]; T_po = P.tiles_n("po", 2)
                cnt = 0
                for hd in range(8):
                    s = hd % 2
                    for i3, cbase in enumerate((2048, 3072, 4096)):
                        P.dma('pool', lambda e, s=s, i3=i3, cbase=cbase, hd=hd: e.dma_start(out=wq[s][:, i3, :, :], in_=wpanel(w_in, 9216, 16, cbase + hd * 128, 128)),
                              writes=[T_wq[s]], part=(i3 > 0))
                    P.dma('sp', lambda e, s=s, hd=hd: e.dma_start(out=bt[s][:], in_=BTd.ap()[:, hd, :, :]), writes=[T_bt[s]])
                    for hf in range(2):
                        b = cnt % 2; cnt += 1
                        for kc in range(16):
                            P.op('pe', lambda e, s=s, kc=kc, b=b, hf=hf: e.matmul(pj[b][:], lhsT=wq[s][:, 0, kc, :], rhs=aT[:, kc, 512 + hf * 512:1024 + hf * 512], start=(kc == 0), stop=(kc == 15)),
                                 reads=[T_wq[s], T_aT], writes=[T_pj[b]], inc=(kc == 15))
                        P.op('act', lambda e, s=s, b=b, hf=hf: e.activation(out=qT[s][:, hf * 512:(hf + 1) * 512], in_=pj[b][:], func=AF.Copy, scale=128.0 ** -0.5),
                             reads=[T_pj[b]], writes=[T_qT[s]])
                    for hf in range(3):
                        b = cnt % 2; cnt += 1
                        for kc in range(16):
                            P.op('pe', lambda e, s=s, kc=kc, b=b, hf=hf: e.matmul(pj[b][:], lhsT=wq[s][:, 1, kc, :], rhs=aT[:, kc, hf * 512:(hf + 1) * 512], start=(kc == 0), stop=(kc == 15)),
                                 reads=[T_wq[s], T_aT], writes=[T_pj[b]], inc=(kc == 15))
                        P.op('dve', lambda e, s=s, b=b, hf=hf: e.tensor_copy(kT[s][:, hf * 512:(hf + 1) * 512], pj[b][:]),
                             reads=[T_pj[b]], writes=[T_kT[s]])
                    for g4 in range(3):
                        b = cnt % 2; cnt += 1
                        for kb in range(4):
                            blk = g4 * 4 + kb
                            for kc in range(16):
                                P.op('pe', lambda e, s=s, kc=kc, b=b, kb=kb, blk=blk: e.matmul(pj[b][:, kb * 128:(kb + 1) * 128], lhsT=aT[:, kc, blk * 128:(blk + 1) * 128], rhs=wq[s][:, 2, kc, :],
                                                                                     start=(kc == 0), stop=(kc == 15)),
                                     reads=[T_wq[s], T_aT], writes=[T_pj[b]], inc=(kc == 15 and kb == 3))
                        P.op('act', lambda e, s=s, b=b, g4=g4: e.activation(out=vh[s][:, g4 * 4:(g4 + 1) * 4, :], in_=pj[b][:].rearrange("p (a b) -> p a b", b=128), func=AF.Copy),
                             reads=[T_pj[b]], writes=[T_vh[s]])
                    for m in range(8):
                        u = m % 2
                        for ki in range(5):
                            dst = scA[:, ki, :] if ki < 4 else scB[:, 0, :]
                            Tsc = T_scA if ki < 4 else T_scB
                            P.op('pe', lambda e, s=s, ki=ki, m=m, dst=dst: e.matmul(dst, lhsT=kT[s][:, (m + ki) * 128:(m + ki + 1) * 128], rhs=qT[s][:, m * 128:(m + 1) * 128], start=True, stop=False),
                                 reads=[T_kT[s], T_qT[s]], writes=[Tsc], inc=False)
                            P.op('pe', lambda e, s=s, ki=ki, dst=dst: e.matmul(dst, lhsT=identf[:], rhs=bt[s][:, ki, :], start=False, stop=True),
                                 reads=[T_bt[s], T_c], writes=[Tsc], inc=True)
                            P.op('act', lambda e, u=u, ki=ki, m=m, dst=dst: e.activation(out=pT[u][:, ki, :], in_=dst, func=AF.Exp, bias=km[:, m + ki:m + ki + 1], scale=1.0),
                                 reads=[Tsc, T_km], writes=[T_pT[u]])
                        for ki in range(5):
                            P.op('pe', lambda e, s=s, u=u, ki=ki, m=m: e.matmul(po[u][:, 0, :], lhsT=vh[s][:, m + ki, :], rhs=pT[u][:, ki, :], start=(ki == 0), stop=(ki == 4)),
                                 reads=[T_vh[s], T_pT[u]], writes=[T_po[u]], inc=False)
                        for ki in range(5):
                            P.op('pe', lambda e, u=u, ki=ki: e.matmul(po[u][:, 1, :], lhsT=onesb[:], rhs=pT[u][:, ki, :], start=(ki == 0), stop=(ki == 4), skip_group_check=True),
                                 reads=[T_c4, T_pT[u]], writes=[T_po[u]], inc=(ki == 4))
                        P.op('dve', lambda e, u=u: e.reciprocal(rr[u][:], po[u][:, 1, :]), reads=[T_po[u]], writes=[T_rr[u]])
                        P.op('dve', lambda e, u=u, hd=hd, m=m: e.tensor_tensor(out=ybT[:, hd, m * 128:(m + 1) * 128], in0=po[u][:, 0, :], in1=rr[u][:], op=ALU.mult),
                             reads=[T_po[u], T_rr[u]], writes=[T_ybT])
                P.flush()
            with ExitStack() as S:
                RS.reset(); R4.reset()
                wg = [RS.alloc("wg", [128, 2, 16, 128], BF16) for i in range(2)]; T_wg = P.tiles_n("wg", 2)
                wu = [RS.alloc("wu", [128, 2, 8, 128], BF16) for i in range(2)]; T_wu = P.tiles_n("wu", 2)
                sg_ = [RS.alloc("sg", [128, 512]) for i in range(2)]; T_sg = P.tiles_n("sg", 2)
                m1 = [RS.alloc("m1", [128, 512]) for i in range(2)]; T_m1 = P.tiles_n("m1", 2)
                pg = [PS(S, "pg%d" % i, [128, 512]) for i in range(4)]; T_pg = P.tiles_n("pg", 4)
                pu_ = [PS(S, "pu%d" % i, [128, 512]) for i in range(4)]; T_pu = P.tiles_n("pu", 4)
                cnt = 0
                for jb in range(16):
                    s = jb % 2
                    P.dma('pool', lambda e, s=s, jb=jb: e.dma_start(out=wg[s][:, 0, :, :], in_=wpanel(w_in, 9216, 16, 5120 + jb * 128, 128)), writes=[T_wg[s]])
                    P.dma('pool', lambda e, s=s, jb=jb: e.dma_start(out=wg[s][:, 1, :, :], in_=wpanel(w_in, 9216, 16, 7168 + jb * 128, 128)), writes=[T_wg[s]], part=True)
                    P.dma('pool', lambda e, s=s, jb=jb: e.dma_start(out=wu[s][:, 0, :, :], in_=wpanel(w_up_a, 2048, 8, jb * 128, 128)), writes=[T_wu[s]])
                    P.dma('pool', lambda e, s=s, jb=jb: e.dma_start(out=wu[s][:, 1, :, :], in_=wpanel(w_up_b, 2048, 8, jb * 128, 128)), writes=[T_wu[s]], part=True)
                    for hf in range(2):
                        for br in range(2):
                            b = cnt % 4; cnt += 1
                            srcY, T_srcY = (yaT, T_yaT) if br == 0 else (ybT, T_ybT)
                            for kc in range(16):
                                P.op('pe', lambda e, s=s, br=br, kc=kc, b=b, hf=hf: e.matmul(pg[b][:], lhsT=wg[s][:, br, kc, :], rhs=aT[:, kc, 512 + hf * 512:1024 + hf * 512], start=(kc == 0), stop=(kc == 15)),
                                     reads=[T_wg[s], T_aT], writes=[T_pg[b]], inc=(kc == 15))
                            for kc in range(8):
                                P.op('pe', lambda e, s=s, br=br, kc=kc, b=b, hf=hf, srcY=srcY: e.matmul(pu_[b][:], lhsT=wu[s][:, br, kc, :], rhs=srcY[:, kc, hf * 512:(hf + 1) * 512], start=(kc == 0), stop=(kc == 7)),
                                     reads=[T_wu[s], T_srcY], writes=[T_pu[b]], inc=(kc == 7))
                            P.op('act', lambda e, br=br, b=b: e.activation(out=sg_[br][:], in_=pg[b][:], func=AF.Sigmoid), reads=[T_pg[b]], writes=[T_sg[br]])
                            P.op('dve', lambda e, b=b, br=br: e.tensor_tensor(out=m1[br][:], in0=sg_[br][:], in1=pu_[b][:], op=ALU.mult), reads=[T_sg[br], T_pu[b]], writes=[T_m1[br]])
                            if br == 1:
                                P.op('dve', lambda e, jb=jb, hf=hf: e.tensor_tensor(out=mT[:, jb, hf * 512:(hf + 1) * 512], in0=m1[0][:], in1=m1[1][:], op=ALU.add),
                                     reads=[T_m1[0], T_m1[1]], writes=[T_mT])
                P.flush()
            with ExitStack() as S:
                R1.reset()
                load_x_into_h()
                proj_tok(S, R1, w_out, 2048, 16, 0, 4, mT, T_mT, 0, 8, add_to_h)
                P.flush()
        else:
            load_x_into_h()
            P.flush()

        if stage >= 2:
            for r in (R1, R2, R3, R4):
                r.reset()
            a2T = R1.alloc("a2T", [128, 16, 1024], BF16); T_a2T = P.tile("a2T")
            mnT = R4.alloc("mnT", [128, 16, 256], BF16); T_mnT = P.tile("mnT")
            kmT = R4.alloc("kmT", [128, 16, 256], BF16); T_kmT = P.tile("kmT")
            RX = Region(75 * KB + 32 * KB, 123 * KB)
            vm = RX.alloc("vm", [128, 2, 2048], BF16); T_vm = P.tile("vm")
            with ExitStack() as S:
                R2.reset()
                load_gb(1)
                rmsnorm_T(S, R2, lambda tt: (h[:, tt, :], T_h[tt]), 8, a2T, T_a2T, 0)
                P.flush()
            with ExitStack() as S:
                R2.reset()
                load_gb(2)
                mt = [R2.alloc("mt", [128, 2048]) for i in range(2)]; T_mt = P.tiles_n("mt", 2)

                def src_m(tt):
                    P.dma('sp', lambda e, tt=tt: e.dma_start(out=mt[tt][:], in_=memx.ap()[tt * 128:(tt + 1) * 128, :]), writes=[T_mt[tt]])
                    return mt[tt][:], T_mt[tt]
                R3.reset()
                rmsnorm_T(S, R3, src_m, 2, mnT, T_mnT, 0)
                P.flush()
            with ExitStack() as S:
                R3.reset()
                proj_feat(S, R3, mem_w_kv, 4096, 16, 0, 16, mnT, T_mnT, 0, 256, copy_evac(lambda oc, t0, n: kmT[:, oc, t0:t0 + n], T_kmT))
                P.flush()
            with ExitStack() as S:
                R3.reset()

                def ev_vm(pn, tt, ps, T_ps):
                    P.op('act', lambda e, pn=pn, tt=tt, ps=ps: e.activation(out=vm[:, tt, pn * 512:(pn + 1) * 512], in_=ps[:], func=AF.Copy), reads=[T_ps], writes=[T_vm])
                proj_tok(S, R3, mem_w_kv, 4096, 16, 2048, 4, mnT, T_mnT, 0, 2, ev_vm)
                P.flush()
            R2.reset()
            q2T = R2.alloc("q2T", [128, 16, 1024], BF16); T_q2T = P.tile("q2T")
            with ExitStack() as S:
                R3.reset()
                proj_feat(S, R3, mem_w_q, 2048, 16, 0, 16, a2T, T_a2T, 0, 1024, copy_evac(lambda oc, t0, n: q2T[:, oc, t0:t0 + n], T_q2T))
                P.flush()
            onT = a2T; T_onT = P.tile("onT")
            with ExitStack() as S:
                R3.reset()
                pT2 = [R3.alloc("pT2", [128, 2, 512], BF16) for i in range(2)]; T_pT2 = P.tiles_n("pT2", 2)
                r2 = [R3.alloc("r2", [128, 512]) for i in range(2)]; T_r2 = P.tiles_n("r2", 2)
                sc2 = [PS(S, "sc2%d" % i, [128, 512]) for i in range(2)]; T_sc2 = P.tiles_n("sc2", 2)
                rs2 = PS(S, "rs2", [128, 512]); T_rs2 = P.tile("rs2")
                o2 = [PS(S, "o2%d" % i, [128, 512]) for i in range(2)]; T_o2 = P.tiles_n("o2", 2)
                it = 0
                for hd in range(4):
                    for hf in range(2):
                        u = it % 2; it += 1
                        for mb in range(2):
                            for kc in range(4):
                                P.op('pe', lambda e, hd=hd, hf=hf, mb=mb, kc=kc: e.matmul(sc2[mb][:], lhsT=kmT[:, hd * 4 + kc, mb * 128:(mb + 1) * 128], rhs=q2T[:, hd * 4 + kc, hf * 512:(hf + 1) * 512],
                                                                                    start=(kc == 0), stop=(kc == 3)),
                                     reads=[T_kmT, T_q2T], writes=[T_sc2[mb]], inc=(kc == 3))
                            P.op('act', lambda e, u=u, mb=mb: e.activation(out=pT2[u][:, mb, :], in_=sc2[mb][:], func=AF.Exp, scale=512.0 ** -0.5),
                                 reads=[T_sc2[mb]], writes=[T_pT2[u]])
                        for mb in range(2):
                            P.op('pe', lambda e, u=u, mb=mb: e.matmul(rs2[:], lhsT=onesb[:], rhs=pT2[u][:, mb, :], start=(mb == 0), stop=(mb == 1)),
                                 reads=[T_c4, T_pT2[u]], writes=[T_rs2], inc=(mb == 1))
                        P.op('dve', lambda e, u=u: e.reciprocal(r2[u][:], rs2[:]), reads=[T_rs2], writes=[T_r2[u]])
                        for oc in range(4):
                            ob = oc % 2
                            for mb in range(2):
                                P.op('pe', lambda e, u=u, mb=mb, hd=hd, oc=oc, ob=ob: e.matmul(o2[ob][:], lhsT=vm[:, mb, hd * 512 + oc * 128:hd * 512 + (oc + 1) * 128], rhs=pT2[u][:, mb, :],
                                                                                         start=(mb == 0), stop=(mb == 1)),
                                     reads=[T_vm, T_pT2[u]], writes=[T_o2[ob]], inc=(mb == 1))
                            P.op('dve', lambda e, u=u, hd=hd, oc=oc, ob=ob, hf=hf: e.tensor_tensor(out=onT[:, hd * 4 + oc, hf * 512:(hf + 1) * 512], in0=o2[ob][:], in1=r2[u][:], op=ALU.mult),
                                 reads=[T_o2[ob], T_r2[u]], writes=[T_onT])
                P.flush()
            with ExitStack() as S:
                R2.reset()
                proj_tok(S, R2, mem_w_o, 2048, 16, 0, 4, onT, T_onT, 0, 8, add_to_h)
                P.flush()

        if stage >= 3:
            for r in (R1, R2, R3, R4):
                r.reset()
            a3T = R1.alloc("a3T", [128, 16, 1024], BF16); T_a3T = P.tile("a3T")
            RX = Region(75 * KB + 32 * KB, 123 * KB)
            qT3 = R2.alloc("qT3", [128, 32, 2, 2, 128], BF16); T_qT3 = P.tile("qT3")
            with ExitStack() as S:
                R3.reset()
                load_gb(3)
                rmsnorm_T(S, R3, lambda tt: (h[:, tt, :], T_h[tt]), 8, a3T, T_a3T, 0)
                P.flush()
            with ExitStack() as S:
                R3.reset()
                def ev_q3(oc, t0, n, ps, T_ps, cnt):
                    h_, pp = oc // 2, oc % 2
                    hh, hl = h_ // 4, h_ % 4
                    dst = bass.AP(qT3, hh * 256 + pp * 128 + hl * 32 + (t0 // 32) * 512, [[16384, 128], [512, n // 32], [1, 32]])
                    src = ps[:, 0:n].rearrange("p (a b) -> p a b", b=32)
                    if cnt % 2 == 0:
                        P.op('act', lambda e, dst=dst, src=src: e.activation(out=dst, in_=src, func=AF.Copy), reads=[T_ps], writes=[T_qT3])
                    else:
                        P.op('dve', lambda e, dst=dst, src=src: e.tensor_copy(dst, src), reads=[T_ps], writes=[T_qT3])
                proj_feat(S, R3, peer_w_q, 2048, 16, 0, 16, a3T, T_a3T, 0, 1024, ev_q3)
                P.flush()
            with ExitStack() as S:
                R3.reset(); R4.reset(); RX.reset()
                keysb = RX.alloc("keysb", [128, 2, 128], BF16); T_keys = P.tile("keys")
                P.dma('pool', lambda e: e.dma_start(out=keysb[:], in_=keysTd.ap()), writes=[T_keys])
                s_sb = [RX.alloc("s_sb", [128, 2, 128]) for i in range(8)]; T_s = P.tiles_n("s_sb", 8)
                s1p = [RX.alloc("s1p", [128, 128]) for i in range(8)]; T_s1p = P.tiles_n("s1p", 8)
                t16 = [R4.alloc("t16", [128, 2, 16]) for i in range(8)]; T_t16 = P.tiles_n("t16", 8)
                c16 = [R4.alloc("c16", [128, 16]) for i in range(8)]; T_c16 = P.tiles_n("c16", 8)
                scal = [R4.alloc("scal", [128, 8]) for i in range(8)]; T_scal = P.tiles_n("scal", 8)
                tmpk = R4.alloc("tmpk", [128, 128]); T_tmpk = P.tile("tmpk")
                cand = R4.alloc("cand", [128, 256]); T_cand = P.tile("cand")
                tmpc = R4.alloc("tmpc", [128, 256]); T_tmpc = P.tile("tmpc")
                j16 = R4.alloc("j16", [128, 16]); T_j16 = P.tile("j16")
                Dd = [R3.alloc("Dd", [128, 1024]) for i in range(4)]; T_D = P.tiles_n("Dd", 4)
                Ee = [R3.alloc("Ee", [128, 1024], BF16) for i in range(3)]; T_E = P.tiles_n("Ee", 3)
                Gm = [R3.alloc("Gm", [128, 1024], BF16) for i in range(3)]; T_Gm = P.tiles_n("Gm", 3)
                Mm = [R3.alloc("Mm", [128, 1024], BF16) for i in range(2)]; T_M = P.tiles_n("Mm", 2)
                Go = [R4.alloc("Go", [128, 1024], BF16) for i in range(2)]; T_Go = P.tiles_n("Go", 2)
                sps = PS(S, "sps", [128, 4, 128]); T_sps = P.tile("sps")
                gps = [PS(S, "gps%d" % i, [128, 2, 512]) for i in range(2)]; T_gps = P.tiles_n("gps", 2)
                T_Gd = P.tile("Gd")
                for st_ in range(8):
                    tb = st_ * 128
                    for ti in range(8):
                        q, hh = ti // 2, ti % 2
                        for pp in range(2):
                            lh = qT3[:, st_ * 4 + q, hh, pp, :]
                            P.op('pe', lambda e, pp=pp, lh=lh: e.matmul(sps[:, pp, :], lhsT=lh, rhs=keysb[:, pp, :], start=True, stop=True),
                                 reads=[T_qT3, T_keys], writes=[T_sps], inc=(pp == 1))
                        P.op('act', lambda e, ti=ti: e.activation(out=s_sb[ti][:], in_=sps[:, 0:2, :], func=AF.Copy), reads=[T_sps], writes=[T_s[ti]])
                        for pp in range(2):
                            P.op('dve', lambda e, ti=ti, pp=pp: e.max(out=t16[ti][:, pp, 0:8], in_=s_sb[ti][:, pp, :]), reads=[T_s[ti]], writes=[T_t16[ti]])
                            P.op('dve', lambda e, ti=ti, pp=pp: e.match_replace(out=tmpk[:], in_to_replace=t16[ti][:, pp, 0:8], in_values=s_sb[ti][:, pp, :], imm_value=NEG),
                                 reads=[T_s[ti], T_t16[ti]], writes=[T_tmpk])
                            P.op('dve', lambda e, ti=ti, pp=pp: e.max(out=t16[ti][:, pp, 8:16], in_=tmpk[:]), reads=[T_tmpk], writes=[T_t16[ti]])
                        P.op('dve', lambda e, ti=ti: e.tensor_tensor(out=cand[:].rearrange("p (a b) -> p a b", b=16), in0=bass.AP(t16[ti], 0, [[32, 128], [1, 16], [0, 16]]),
                                                                  in1=bass.AP(t16[ti], 16, [[32, 128], [0, 16], [1, 16]]), op=ALU.add),
                             reads=[T_t16[ti]], writes=[T_cand])
                        P.op('dve', lambda e, ti=ti: e.max(out=c16[ti][:, 0:8], in_=cand[:]), reads=[T_cand], writes=[T_c16[ti]])
                        P.op('dve', lambda e, ti=ti: e.match_replace(out=tmpc[:], in_to_replace=c16[ti][:, 0:8], in_values=cand[:], imm_value=NEG),
                             reads=[T_cand, T_c16[ti]], writes=[T_tmpc])
                        P.op('dve', lambda e, ti=ti: e.max(out=c16[ti][:, 8:16], in_=tmpc[:]), reads=[T_tmpc], writes=[T_c16[ti]])
                        P.op('dve', lambda e, ti=ti: e.tensor_scalar(out=scal[ti][:, 0:1], in0=c16[ti][:, 0:1], scalar1=-1.0, scalar2=None, op0=ALU.mult),
                             reads=[T_c16[ti]], writes=[T_scal[ti]])
                        P.op('act', lambda e, ti=ti: e.activation(out=j16[:], in_=c16[ti][:], func=AF.Exp, bias=scal[ti][:, 0:1], scale=1.0, accum_out=scal[ti][:, 1:2]),
                             reads=[T_c16[ti], T_scal[ti]], writes=[T_j16, T_scal[ti]])
                        P.op('act', lambda e, ti=ti: e.activation(out=scal[ti][:, 2:3], in_=scal[ti][:, 1:2], func=AF.Ln), reads=[T_scal[ti]], writes=[T_scal[ti]])
                        P.op('dve', lambda e, ti=ti: e.scalar_tensor_tensor(out=scal[ti][:, 3:4], in0=c16[ti][:, 15:16], scalar=scal[ti][:, 0:1], in1=scal[ti][:, 2:3], op0=ALU.add, op1=ALU.subtract),
                             reads=[T_c16[ti], T_scal[ti]], writes=[T_scal[ti]])
                        P.op('dve', lambda e, ti=ti: e.tensor_scalar(out=s1p[ti][:], in0=s_sb[ti][:, 0, :], scalar1=c16[ti][:, 15:16], scalar2=None, op0=ALU.subtract),
                             reads=[T_s[ti], T_c16[ti]], writes=[T_s1p[ti]])
                    NIT = 128
                    for k in range(NIT + 2):
                        if k < NIT:
                            eb, ti = k // 8, k % 8
                            u = k % 4
                            deng = 'dve' if (k % 5 in (1, 3)) else 'pool'
                            P.op(deng, lambda e, ti=ti, eb=eb, u=u: e.tensor_tensor(out=Dd[u][:].rearrange("p (a b) -> p a b", b=128), in0=bass.AP(s1p[ti], eb * 8, [[128, 128], [1, 8], [0, 128]]),
                                                                                 in1=bass.AP(s_sb[ti], 128, [[256, 128], [0, 8], [1, 128]]), op=ALU.add),
                                 reads=[T_s1p[ti], T_s[ti]], writes=[T_D[u]])
                        k1 = k - 1
                        if 0 <= k1 < NIT:
                            ti = k1 % 8
                            P.op('act', lambda e, ti=ti, k1=k1: e.activation(out=Ee[k1 % 3][:], in_=Dd[k1 % 4][:], func=AF.Exp, bias=scal[ti][:, 3:4], scale=1.0),
                                 reads=[T_D[k1 % 4], T_scal[ti]], writes=[T_E[k1 % 3]])
                        k2 = k - 2
                        if 0 <= k2 < NIT:
                            eb, ti = k2 // 8, k2 % 8
                            q, hh = ti // 2, ti % 2
                            gsl = eb % 2
                            P.op('dve', lambda e, k2=k2: e.tensor_scalar(out=Mm[k2 % 2][:], in0=Dd[k2 % 4][:], scalar1=-1e-5, scalar2=None, op0=ALU.is_ge),
                                 reads=[T_D[k2 % 4]], writes=[T_M[k2 % 2]])
                            P.op('dve', lambda e, k2=k2: e.tensor_tensor(out=Gm[k2 % 3][:], in0=Ee[k2 % 3][:], in1=Mm[k2 % 2][:], op=ALU.mult),
                                 reads=[T_M[k2 % 2], T_E[k2 % 3]], writes=[T_Gm[k2 % 3]])
                            for h2 in range(2):
                                P.op('pe', lambda e, k2=k2, h2=h2, q=q, hh=hh, gsl=gsl: e.matmul(gps[gsl][32 * q:32 * q + 32, h2, :], lhsT=sel32b[:], rhs=Gm[k2 % 3][:, h2 * 512:(h2 + 1) * 512],
                                                                                           start=(hh == 0), stop=(hh == 1), tile_position=(0, 32 * q), skip_group_check=True),
                                     reads=[T_Gm[k2 % 3], T_c3], writes=[T_gps[gsl]], inc=(h2 == 1))
                            if ti == 7:
                                P.op('act', lambda e, gsl=gsl: e.activation(out=Go[gsl][:], in_=gps[gsl][:].rearrange("p a b -> p (a b)"), func=AF.Copy), reads=[T_gps[gsl]], writes=[T_Go[gsl]])
                                P.dma('sp', lambda e, gsl=gsl, tb=tb, eb=eb: e.dma_start(out=Gd.ap()[tb:tb + 128, eb * 1024:(eb + 1) * 1024], in_=Go[gsl][:]), reads=[T_Go[gsl]], writes=[T_Gd], part=True)
                P.flush()
            with ExitStack() as S:
                R2.reset(); R3.reset(); R4.reset(); RX.reset()
                RP = Region(123 * KB, 207 * KB)
                GI = 4
                ust = [RP.alloc("ust", [128, 2048], BF16) for i in range(2)]; T_ust = P.tiles_n("ust", 2)
                UT = [RP.alloc("UT", [128, 16, 128], BF16) for i in range(2)]; T_UT = P.tiles_n("UT", 2)
                vg = [RP.alloc("vg", [128, GI, 2048], BF16) for i in range(2)]; T_vg = P.tiles_n("vg", 2)
                gg = [RP.alloc("gg", [128, 8, GI * 128], BF16) for i in range(2)]; T_gg = P.tiles_n("gg", 2)
                actT = [RP.alloc("actT", [128, GI, 1024], BF16) for i in range(2)]; T_actT = P.tiles_n("actT", 2)
                gl = [RP.alloc("gl", [128, 512], BF16) for i in range(2)]; T_gl = P.tiles_n("gl", 2)
                utp = PS(S, "utp", [128, 8, 128], BF16); T_utp = P.tile("utp")
                gtp = PS(S, "gtp", [128, 8, 128], BF16); T_gtp = P.tile("gtp")
                pps = [PS(S, "pps%d" % i, [128, 512]) for i in range(2)]; T_pps = P.tiles_n("pps", 2)
                ops_ = [PS(S, "ops%d" % i, [128, 2, 512]) for i in range(2)]; T_ops = P.tiles_n("ops", 2)
                ci = 0
                oi = 0
                for g in range(16384 // (128 * GI)):
                    gs = g % 2
                    P.dma('pool', lambda e, g=g, gs=gs: e.dma_start(out=vg[gs][:], in_=bass.AP(pv, g * GI * 128 * 2048, [[2048, 128], [128 * 2048, GI], [1, 2048]])), writes=[T_vg[gs]])
                    P.dma('sp', lambda e, g=g, gs=gs: e.dma_start(out=gg[gs][:], in_=bass.AP(Gd, g * GI * 128, [[16384, 128], [128 * 16384, 8], [1, GI * 128]])), writes=[T_gg[gs]])
                    for c in range(GI):
                        us = ci % 2; ci += 1
                        ch = g * GI + c
                        P.dma('pool', lambda e, ch=ch, us=us: e.dma_start(out=ust[us][:], in_=pu.ap()[ch * 128:(ch + 1) * 128, :]), writes=[T_ust[us]])
                        for hf in range(2):
                            for k in range(8):
                                dc = hf * 8 + k
                                P.op('pe', lambda e, us=us, k=k, dc=dc: e.transpose(out=utp[:, k, :], in_=ust[us][:, dc * 128:(dc + 1) * 128], identity=identb[:]),
                                     reads=[T_ust[us], T_c2], writes=[T_utp], inc=(k == 7))
                            if hf == 0:
                                P.op('act', lambda e, us=us: e.activation(out=UT[us][:, 0:8, :], in_=utp[:], func=AF.Copy), reads=[T_utp], writes=[T_UT[us]])
                            else:
                                P.op('dve', lambda e, us=us: e.tensor_copy(UT[us][:, 8:16, :], utp[:]), reads=[T_utp], writes=[T_UT[us]])
                        for tt in range(8):
                            P.op('pe', lambda e, gs=gs, tt=tt, c=c: e.transpose(out=gtp[:, tt, :], in_=gg[gs][:, tt, c * 128:(c + 1) * 128], identity=identb[:]),
                                 reads=[T_gg[gs], T_c2], writes=[T_gtp], inc=(tt == 7))
                        for hf in range(2):
                            for dc in range(16):
                                P.op('pe', lambda e, us=us, dc=dc, hf=hf: e.matmul(pps[hf][:], lhsT=UT[us][:, dc, :], rhs=a3T[:, dc, hf * 512:(hf + 1) * 512], start=(dc == 0), stop=(dc == 15)),
                                     reads=[T_UT[us], T_a3T], writes=[T_pps[hf]], inc=(dc == 15))
                            P.op('act', lambda e, hf=hf: e.activation(out=gl[hf][:], in_=pps[hf][:], func=AF.Gelu), reads=[T_pps[hf]], writes=[T_gl[hf]])
                            P.op('dve', lambda e, hf=hf, gs=gs, c=c: e.tensor_tensor(out=actT[gs][:, c, hf * 512:(hf + 1) * 512].rearrange("p (a b) -> p a b", b=128),
                                                                                  in0=gl[hf][:].rearrange("p (a b) -> p a b", b=128), in1=gtp[:, hf * 4:(hf + 1) * 4, :], op=ALU.mult),
                                 reads=[T_gl[hf], T_gtp], writes=[T_actT[gs]])
                    for tt in range(8):
                        for dh in range(2):
                            ob = oi % 2; oi += 1
                            for d2 in range(2):
                                db = dh * 2 + d2
                                for c in range(GI):
                                    P.op('pe', lambda e, gs=gs, c=c, tt=tt, db=db, d2=d2, ob=ob: e.matmul(ops_[ob][:, d2, :], lhsT=actT[gs][:, c, tt * 128:(tt + 1) * 128], rhs=vg[gs][:, c, db * 512:(db + 1) * 512],
                                                                                                    start=(c == 0), stop=(c == GI - 1)),
                                         reads=[T_actT[gs], T_vg[gs]], writes=[T_ops[ob]], inc=(c == GI - 1 and d2 == 1))
                            P.op('dve', lambda e, tt=tt, dh=dh, ob=ob: e.tensor_tensor(out=h[:, tt, dh * 1024:(dh + 1) * 1024], in0=h[:, tt, dh * 1024:(dh + 1) * 1024],
                                                                                  in1=ops_[ob][:].rearrange("p a b -> p (a b)"), op=ALU.add),
                                 reads=[T_ops[ob], T_h[tt]], writes=[T_h[tt]])
                P.flush()

        with ExitStack() as S:
            R1.reset()
            load_gb(4)
            ot = [R1.alloc("ot", [128, 2048]) for i in range(2)]; T_ot = P.tiles_n("ot", 2)
            junk = R1.alloc("junkf", [128, 2048], BF16); T_j = P.tile("junkf")
            stat = R1.alloc("statf", [128, 24]); T_st = P.tiles_n("stf", 8)
            T_out = P.tile("out")
            for tt in range(8):
                s = tt % 2
                c = 3 * tt
                P.op('act', lambda e, tt=tt, c=c: e.activation(out=junk[:], in_=h[:, tt, :], func=AF.Square, accum_out=stat[:, c:c + 1]), reads=[T_h[tt]], writes=[T_j, T_st[tt]])
                P.op('act', lambda e, c=c: e.activation(out=stat[:, c + 1:c + 2], in_=stat[:, c:c + 1], func=AF.Sqrt, scale=1.0 / 2048, bias=EPS), reads=[T_st[tt]], writes=[T_st[tt]])
                P.op('dve', lambda e, c=c: e.reciprocal(stat[:, c + 2:c + 3], stat[:, c + 1:c + 2]), reads=[T_st[tt]], writes=[T_st[tt]])
                P.op('dve', lambda e, tt=tt, c=c, s=s: e.scalar_tensor_tensor(out=ot[s][:], in0=h[:, tt, :], scalar=stat[:, c + 2:c + 3], in1=gb[:], op0=ALU.mult, op1=ALU.mult),
                     reads=[T_h[tt], T_st[tt], T_gb], writes=[T_ot[s]])
                P.dma('sp', lambda e, tt=tt, s=s: e.dma_start(out=out.ap()[tt * 128:(tt + 1) * 128, :], in_=ot[s][:]), reads=[T_ot[s]], writes=[T_out], part=True)
            P.flush()
        print("bass instructions emitted:", P.n_instr, "dma sems:", P.nsem)
    return nc


_CACHE = {}


def _host_consts(att_rel_bias):
    kj = np.arange(640)[:, None]
    qi = np.arange(128)[None, :]
    dist = 512 + qi - kj
    idx = np.clip(dist, -128, 128) + 128
    valid = (qi // 64 <= kj // 64) & (kj // 64 <= 8 + qi // 64)
    rb = att_rel_bias[0]
    bt = rb[:, idx]
    bt = np.where(valid[None], bt, np.float32(NEG)).astype(np.float32)
    bt = bt.reshape(8, 5, 128, 128).transpose(2, 0, 1, 3)
    return np.ascontiguousarray(bt)


def kernel(x, mem, g_mix, w_in, sg_ln_g, sg_ln_b, sg_w_s, sg_b_s, att_rel_bias, w_up_a, w_up_b, w_out,
           g_mem_q, g_mem_kv, mem_w_q, mem_w_kv, mem_w_o, g_ffn, peer_w_q, peer_sub_keys, peer_u, peer_v, g_final,
           _stage=9):
    f = lambda a: np.ascontiguousarray(np.asarray(a, dtype=np.float32))
    x = f(x); mem = f(mem)
    nc = build(_stage)
    shared = {
        "gvecs": f(np.stack([np.asarray(g_mix)[0], np.asarray(g_mem_q)[0], np.asarray(g_mem_kv)[0], np.asarray(g_ffn)[0], np.asarray(g_final)])),
        "w_in": f(np.asarray(w_in)[0]), "sg_ln_g": f(np.asarray(sg_ln_g)[0]), "sg_ln_b": f(np.asarray(sg_ln_b)[0]),
        "sg_wsT": f(np.asarray(sg_w_s)[0].transpose(2, 0, 1)),
        "sg_bsT": f(np.asarray(sg_b_s)[0].T),
        "att_bt": _host_consts(f(att_rel_bias)),
        "w_up_a": f(np.asarray(w_up_a)[0]), "w_up_b": f(np.asarray(w_up_b)[0]), "w_out": f(np.asarray(w_out)[0]),
        "mem_w_q": f(np.asarray(mem_w_q)[0]), "mem_w_kv": f(np.asarray(mem_w_kv)[0]), "mem_w_o": f(np.asarray(mem_w_o)[0]),
        "peer_w_q": f(np.asarray(peer_w_q)[0]),
        "peer_keysT": f(np.asarray(peer_sub_keys)[0].transpose(2, 0, 1)),
        "peer_u": f(np.asarray(peer_u)[0]), "peer_v": f(np.asarray(peer_v)[0]),
        "ident": np.eye(128, dtype=np.float32),
        "sel32": np.ascontiguousarray(np.tile(np.eye(32, dtype=np.float32), (4, 1))),
    }
    in_maps = []
    for k in range(8):
        b, j = k // 4, k % 4
        xc = np.zeros((1536, 2048), np.float32)
        xc[512:] = x[b, j * 1024:(j + 1) * 1024]
        km = np.zeros((128, 12), np.float32)
        if j == 0:
            km[:, 0:4] = NEG
        else:
            xc[:512] = x[b, j * 1024 - 512:j * 1024]
        m = dict(shared)
        m["x"] = xc
        m["keymask"] = km
        m["mem"] = mem[b]
        in_maps.append(m)
    res = run_bass_kernel_spmd(nc, in_maps, core_ids=list(range(8)))
    outp = np.empty((2, 4096, 2048), np.float32)
    for k in range(8):
        b, j = k // 4, k % 4
        outp[b, j * 1024:(j + 1) * 1024] = res.results[k]["out"]
    return outp
```

```python
ENG = ('pe', 'act', 'dve', 'pool', 'sp')


class Tile:
    def __init__(self, prog, name):
        self.name = name
        self.w = {}
        self.r = {}
        self.dsem = None
        prog.tiles.append(self)


class Prog:
    def __init__(self, nc, block, stack):
        self.nc = nc
        self.block = block
        self.stack = stack
        self.bfn = {'pe': block.tensor, 'act': block.scalar, 'dve': block.vector, 'pool': block.gpsimd, 'sp': block.sync}
        self.sem = {e: stack.enter_context(nc.semaphore('s_' + e)) for e in ENG}
        self.ops = {e: [] for e in ENG}
        self.base = {e: 0 for e in ENG}
        self.waited = {e: {} for e in ENG}
        self.tiles = []
        self.dsems = []
        self.nsem = 0
        self.n_instr = 0

    def tile(self, name):
        return Tile(self, name)

    def tiles_n(self, name, n):
        return [Tile(self, '%s%d' % (name, i)) for i in range(n)]

    def _dsem(self, t):
        if t.dsem is None:
            s = self.stack.enter_context(self.nc.semaphore('d%d' % self.nsem))
            self.nsem += 1
            t.dsem = [s, 0]
            self.dsems.append(t.dsem)
        return t.dsem

    @staticmethod
    def _key(d):
        return d[1] if d[0] == 'e' else id(d[1])

    def _collect(self, eng, reads, writes):
        waits = self._collect0(eng, reads, writes)
        cur = {id(d[0]): d[1] for d in self.dsems}
        return [d if d[0] == 'e' else ('d', d[1], cur[id(d[1])]) for d in waits]

    def _collect0(self, eng, reads, writes):
        waits = []
        for t in reads:
            for k, d in t.w.items():
                if k == eng and eng == 'pe':
                    continue
                waits.append(d)
        for t in writes:
            for k, d in t.w.items():
                if k == eng:
                    continue
                waits.append(d)
            for k, d in t.r.items():
                if k == eng:
                    continue
                waits.append(d)
        return waits

    def op(self, eng, fn, reads=(), writes=(), inc=None):
        if inc is None:
            inc = (eng != 'pe')
        idx = len(self.ops[eng])
        waits = self._collect(eng, reads, writes)
        me = ('e', eng, idx)
        self.ops[eng].append({'fn': fn, 'waits': waits, 'inc': inc, 'dma': None})
        for t in reads:
            t.r[eng] = me
        for t in writes:
            t.w = {eng: me}
            t.r = {}
        return me

    def dma(self, eng, fn, reads=(), writes=(), part=False):
        assert len(writes) == 1
        wt = writes[0]
        waits = self._collect('dma', reads, () if part else writes)
        ds = self._dsem(wt)
        ds[1] += 16
        me = ('d', ds[0], ds[1])
        self.ops[eng].append({'fn': fn, 'waits': waits, 'inc': False, 'dma': ds[0]})
        for t in reads:
            t.r[id(ds[0])] = me
        wt.w = {id(ds[0]): me}
        wt.r = {}
        return me

    def _resolve_prepare(self):
        for e in ENG:
            for o in self.ops[e]:
                for d in o['waits']:
                    if d[0] == 'e':
                        lst = self.ops[d[1]]
                        found = False
                        for j in range(d[2], len(lst)):
                            if lst[j]['inc']:
                                found = True
                                break
                        if not found:
                            j = len(lst) - 1
                            while j >= d[2] and (lst[j]['dma'] is not None or lst[j]['fn'] is None):
                                j -= 1
                            assert j >= d[2], "no compute op to carry inc"
                            lst[j]['inc'] = True

    def flush(self, barrier=True):
        if barrier:
            self._add_barrier()
        self._resolve_prepare()
        val = {}
        for e in ENG:
            c = self.base[e]
            arr = []
            for o in self.ops[e]:
                if o['inc']:
                    c += 1
                arr.append(c)
            need = [None] * len(arr)
            nxt = None
            for j in range(len(arr) - 1, -1, -1):
                if self.ops[e][j]['inc']:
                    nxt = arr[j]
                need[j] = nxt
            val[e] = need
        prog = self
        for e in ENG:
            ops = self.ops[e]
            if not ops:
                continue

            def body(engine, ops=ops, waited=self.waited[e], semh=self.sem[e]):
                for o in ops:
                    for d in o['waits']:
                        if d[0] == 'e':
                            s = prog.sem[d[1]]
                            v = val[d[1]][d[2]]
                            assert v is not None
                        else:
                            s, v = d[1], d[2]
                        k = id(s)
                        if waited.get(k, 0) >= v:
                            continue
                        waited[k] = v
                        engine.wait_ge(s, v)
                        prog.n_instr += 1
                    if o['fn'] is None:
                        continue
                    ins = o['fn'](engine)
                    prog.n_instr += 1
                    if o['dma'] is not None:
                        ins.then_inc(o['dma'], 16)
                    elif o['inc']:
                        ins.then_inc(semh, 1)
            self.bfn[e](body)
        for e in ENG:
            self.base[e] += sum(1 for o in self.ops[e] if o['inc'])
            self.ops[e] = []
        if barrier:
            for t in self.tiles:
                t.w = {}
                t.r = {}

    def _add_barrier(self):
        lasts = []
        for e in ENG:
            lst = self.ops[e]
            j = len(lst) - 1
            while j >= 0 and (lst[j]['dma'] is not None or lst[j]['fn'] is None):
                j -= 1
            if j >= 0:
                lst[j]['inc'] = True
                lasts.append(('e', e, j))
        dm = [('d', s[0], s[1]) for s in self.dsems if s[1] > 0]
        for e in ENG:
            self.ops[e].append({'fn': None, 'waits': [d for d in lasts if d[1] != e] + dm, 'inc': False, 'dma': None})

import numpy as np
from contextlib import ExitStack
import concourse.bass as bass
import concourse.mybir as mybir
from concourse.bass_utils import run_bass_kernel_spmd

F32 = mybir.dt.float32
BF16 = mybir.dt.bfloat16
AF = mybir.ActivationFunctionType
ALU = mybir.AluOpType
NEG = -1.0e30
EPS = 1e-6


def build(stage=9):
    nc = bass.Bass("TRN2", target_bir_lowering=False)

    def DI(name, shape, dt=F32):
        return nc.dram_tensor(name, shape, dt, kind="ExternalInput")
    x = DI("x", [1536, 2048]); keymask = DI("keymask", [128, 12]); memx = DI("mem", [256, 2048])
    gvecs = DI("gvecs", [5, 2048])
    w_in = DI("w_in", [2048, 9216]); lng = DI("sg_ln_g", [1024]); lnb = DI("sg_ln_b", [1024])
    wsT = DI("sg_wsT", [128, 4, 128]); bsT = DI("sg_bsT", [128, 4]); BTd = DI("att_bt", [128, 8, 5, 128])
    w_up_a = DI("w_up_a", [1024, 2048]); w_up_b = DI("w_up_b", [1024, 2048]); w_out = DI("w_out", [2048, 2048])
    mem_w_q = DI("mem_w_q", [2048, 2048]); mem_w_kv = DI("mem_w_kv", [2048, 4096]); mem_w_o = DI("mem_w_o", [2048, 2048])
    peer_w_q = DI("peer_w_q", [2048, 2048]); keysTd = DI("peer_keysT", [128, 2, 128])
    pu = DI("peer_u", [16384, 2048]); pv = DI("peer_v", [16384, 2048])
    identd = DI("ident", [128, 128]); sel32d = DI("sel32", [128, 32])
    out = nc.dram_tensor("out", [1024, 2048], F32, kind="ExternalOutput")
    Gd = nc.dram_tensor("g_scratch", [1024, 16384], BF16, kind="Internal")

    def wpanel(w, ncols_total, kchunks, c0, ncols):
        return bass.AP(w, c0, [[ncols_total, 128], [128 * ncols_total, kchunks], [1, ncols]])

    KB = 1024
    with ExitStack() as G:
        block = G.enter_context(nc.Block())
        P = Prog(nc, block, G)
        base0 = (nc.sbuf_base + 63) // 64 * 64
        ARENA = 207 * KB
        G.enter_context(nc.sbuf_tensor("arena", [128, ARENA + 64], mybir.dt.uint8))
        uid = [0]

        class Region:
            def __init__(self, start, end):
                self.start, self.end, self.p = start, end, start

            def reset(self):
                self.p = self.start

            def alloc(self, name, shape, dt=F32):
                n = 4 if dt == F32 else 2
                for d in shape[1:]:
                    n *= d
                n = (n + 63) // 64 * 64
                assert self.p + n <= self.end, "region overflow %s need %d have %d" % (name, n, self.end - self.p)
                uid[0] += 1
                t = nc.alloc_sbuf_tensor_at("%s_%d" % (name, uid[0]), shape, dt, offset=base0 + self.p)
                self.p += n
                return t

        PS = lambda st, name, shape, dt=F32: st.enter_context(nc.psum_tensor(name, shape, dt))
        RG = Region(0, 11 * KB)
        RH = Region(11 * KB, 75 * KB)
        R1 = Region(75 * KB, 123 * KB)
        R2 = Region(123 * KB, 155 * KB)
        R3 = Region(155 * KB, 187 * KB)
        R4 = Region(187 * KB, 207 * KB)

        h = RH.alloc("h", [128, 8, 2048]); T_h = P.tiles_n("h", 8)
        identf = RG.alloc("identf", [128, 128]); identb = RG.alloc("identb", [128, 128], BF16)
        onesb = RG.alloc("onesb", [128, 128], BF16); sel32b = RG.alloc("sel32b", [128, 32], BF16)
        gb = RG.alloc("gb", [128, 2048]); T_gb = P.tile("gb")
        T_c = P.tile("consts"); T_c2 = P.tile("consts2"); T_c3 = P.tile("consts3"); T_c4 = P.tile("consts4")
        P.dma('sp', lambda e: e.dma_start(out=identf[:], in_=identd.ap()), writes=[T_c])
        P.dma('pool', lambda e: e.dma_start(out=identb[:], in_=identd.ap()), writes=[T_c2])
        P.dma('pool', lambda e: e.dma_start(out=sel32b[:], in_=sel32d.ap()), writes=[T_c3])
        P.op('dve', lambda e: e.memset(onesb[:], 1.0), writes=[T_c4])
        P.flush()

        def load_gb(i):
            P.dma('sp', lambda e: e.dma_start(out=gb[:], in_=bass.AP(gvecs, i * 2048, [[0, 128], [1, 2048]])), writes=[T_gb])

        def rmsnorm_T(st, R, src_fn, ntiles, dstT, T_dst, col0):
            junk = R.alloc("junk", [128, 2048], BF16); T_j = P.tile("junk")
            stat = R.alloc("stat", [128, 3 * ntiles]); T_st = P.tiles_n("st", ntiles)
            xs = [R.alloc("xs", [128, 2048], BF16) for i in range(2)]; T_xs = P.tiles_n("xs", 2)
            tp = [PS(st, "tp%d_%d" % (i, uid[0]), [128, 8, 128], BF16) for i in range(2)]; T_tp = P.tiles_n("tp", 2)
            for tt in range(ntiles):
                s = tt % 2
                src, Ts = src_fn(tt)
                c = 3 * tt
                P.op('act', lambda e, src=src, c=c: e.activation(out=junk[:], in_=src, func=AF.Square, accum_out=stat[:, c:c + 1]),
                     reads=[Ts], writes=[T_j, T_st[tt]])
                P.op('act', lambda e, c=c: e.activation(out=stat[:, c + 1:c + 2], in_=stat[:, c:c + 1], func=AF.Sqrt, scale=1.0 / 2048, bias=EPS),
                     reads=[T_st[tt]], writes=[T_st[tt]])
                P.op('dve', lambda e, c=c: e.reciprocal(stat[:, c + 2:c + 3], stat[:, c + 1:c + 2]), reads=[T_st[tt]], writes=[T_st[tt]])
                P.op('dve', lambda e, src=src, c=c, s=s: e.scalar_tensor_tensor(out=xs[s][:], in0=src, scalar=stat[:, c + 2:c + 3], in1=gb[:],
                                                                               op0=ALU.mult, op1=ALU.mult),
                     reads=[Ts, T_st[tt], T_gb], writes=[T_xs[s]])
                for hf in range(2):
                    for k in range(8):
                        dc = hf * 8 + k
                        P.op('pe', lambda e, s=s, hf=hf, k=k, dc=dc: e.transpose(out=tp[hf][:, k, :], in_=xs[s][:, dc * 128:(dc + 1) * 128], identity=identb[:]),
                             reads=[T_xs[s], T_c2], writes=[T_tp[hf]], inc=(k == 7))
                    c0 = col0 + tt * 128
                    if hf == 0:
                        P.op('act', lambda e, hf=hf, c0=c0: e.activation(out=dstT[:, hf * 8:(hf + 1) * 8, c0:c0 + 128], in_=tp[hf][:], func=AF.Copy),
                             reads=[T_tp[hf]], writes=[T_dst])
                    else:
                        P.op('dve', lambda e, hf=hf, c0=c0: e.tensor_copy(dstT[:, hf * 8:(hf + 1) * 8, c0:c0 + 128], tp[hf][:]),
                             reads=[T_tp[hf]], writes=[T_dst])

        def proj_feat(st, R, w, wcols, kch, c0, nout_chunks, srcT, T_src, tok0, ntok, evac):
            npan = (nout_chunks + 3) // 4
            wp = [R.alloc("wp", [128, kch, 512], BF16) for i in range(2)]; T_wp = P.tiles_n("wp", 2)
            ps = [PS(st, "pp%d_%d" % (i, uid[0]), [128, 512]) for i in range(2)]; T_ps = P.tiles_n("pp", 2)
            cnt = 0
            for pn in range(npan):
                s = pn % 2
                ncl = min(4, nout_chunks - pn * 4)
                P.dma('pool', lambda e, s=s, pn=pn, ncl=ncl: e.dma_start(out=wp[s][:, :, 0:ncl * 128], in_=wpanel(w, wcols, kch, c0 + pn * 512, ncl * 128)),
                      writes=[T_wp[s]])
                for ocl in range(ncl):
                    oc = pn * 4 + ocl
                    t0 = 0
                    while t0 < ntok:
                        n = min(512, ntok - t0)
                        b = cnt % 2
                        cnt += 1
                        for kc in range(kch):
                            P.op('pe', lambda e, s=s, ocl=ocl, kc=kc, b=b, t0=t0, n=n: e.matmul(ps[b][:, 0:n], lhsT=wp[s][:, kc, ocl * 128:(ocl + 1) * 128],
                                                                                           rhs=srcT[:, kc, tok0 + t0:tok0 + t0 + n], start=(kc == 0), stop=(kc == kch - 1)),
                                 reads=[T_wp[s], T_src], writes=[T_ps[b]], inc=(kc == kch - 1))
                        evac(oc, t0, n, ps[b], T_ps[b], cnt)
                        t0 += n

        def proj_tok(st, R, w, wcols, kch, c0, npan, srcT, T_src, tok0, ntiles, evac):
            wp = [R.alloc("wq", [128, kch, 512], BF16) for i in range(2)]; T_wp = P.tiles_n("wq", 2)
            ps = [PS(st, "pq%d_%d" % (i, uid[0]), [128, 512]) for i in range(2)]; T_ps = P.tiles_n("pq", 2)
            cnt = 0
            for pn in range(npan):
                s = pn % 2
                P.dma('pool', lambda e, s=s, pn=pn: e.dma_start(out=wp[s][:], in_=wpanel(w, wcols, kch, c0 + pn * 512, 512)), writes=[T_wp[s]])
                for tt in range(ntiles):
                    b = cnt % 2
                    cnt += 1
                    for kc in range(kch):
                        P.op('pe', lambda e, s=s, kc=kc, b=b, tt=tt: e.matmul(ps[b][:], lhsT=srcT[:, kc, tok0 + tt * 128:tok0 + (tt + 1) * 128], rhs=wp[s][:, kc, :],
                                                                        start=(kc == 0), stop=(kc == kch - 1)),
                             reads=[T_wp[s], T_src], writes=[T_ps[b]], inc=(kc == kch - 1))
                    evac(pn, tt, ps[b], T_ps[b])

        def add_to_h(pn, tt, ps, T_ps):
            P.op('dve', lambda e, pn=pn, tt=tt, ps=ps: e.tensor_tensor(out=h[:, tt, pn * 512:(pn + 1) * 512], in0=h[:, tt, pn * 512:(pn + 1) * 512], in1=ps[:], op=ALU.add),
                 reads=[T_ps, T_h[tt]], writes=[T_h[tt]])

        def copy_evac(dst_fn, T_dst):
            def ev(oc, t0, n, ps, T_ps, cnt):
                if cnt % 2 == 0:
                    P.op('act', lambda e, oc=oc, t0=t0, n=n, ps=ps: e.activation(out=dst_fn(oc, t0, n), in_=ps[:, 0:n], func=AF.Copy), reads=[T_ps], writes=[T_dst])
                else:
                    P.op('dve', lambda e, oc=oc, t0=t0, n=n, ps=ps: e.tensor_copy(dst_fn(oc, t0, n), ps[:, 0:n]), reads=[T_ps], writes=[T_dst])
            return ev

        def load_x_into_h():
            for tt in range(8):
                P.dma('sp', lambda e, tt=tt: e.dma_start(out=h[:, tt, :], in_=x.ap()[512 + tt * 128:512 + (tt + 1) * 128, :]), writes=[T_h[tt]])

        if stage >= 1:
            for r in (R1, R2, R3, R4):
                r.reset()
            aT = R1.alloc("aT", [128, 16, 1536], BF16); T_aT = P.tile("aT")
            yaT = R2.alloc("yaT", [128, 8, 1024], BF16); T_yaT = P.tile("yaT")
            ybT = R2.alloc("ybT", [128, 8, 1024], BF16); T_ybT = P.tile("ybT")
            mT = R3.alloc("mT", [128, 16, 1024], BF16); T_mT = P.tile("mT")
            RS = Region(11 * KB, 75 * KB)
            with ExitStack() as S:
                RS.reset(); R4.reset()
                load_gb(0)
                xt = [RS.alloc("xt", [128, 2048]) for i in range(2)]; T_xt = P.tiles_n("xt", 2)

                def src_fn(tt):
                    s = tt % 2
                    P.dma('sp', lambda e, tt=tt, s=s: e.dma_start(out=xt[s][:], in_=x.ap()[tt * 128:(tt + 1) * 128, :]), writes=[T_xt[s]])
                    return xt[s][:], T_xt[s]
                rmsnorm_T(S, RS, src_fn, 12, aT, T_aT, 0)
                P.flush()
            with ExitStack() as S:
                RS.reset(); R4.reset()
                RB = Region(155 * KB, 187 * KB)
                gv = RS.alloc("gv", [128, 8, 1024]); T_gv = P.tiles_n("gv", 8)
                gu = RS.alloc("gu", [128, 8, 1024], BF16); T_gu = P.tiles_n("gu", 8)
                lngb = RS.alloc("lngb", [128, 1024]); lnbb = RS.alloc("lnbb", [128, 1024])
                T_ln = P.tile("ln"); T_ln2 = P.tile("ln2")
                wsf = RS.alloc("wsf", [128, 4, 128]); wsb = RS.alloc("wsb", [128, 4, 128], BF16); T_ws = P.tile("ws")
                bs = RS.alloc("bs", [128, 4]); T_bs = P.tile("bs")
                P.dma('sp', lambda e: e.dma_start(out=lngb[:], in_=bass.AP(lng, 0, [[0, 128], [1, 1024]])), writes=[T_ln])
                P.dma('sp', lambda e: e.dma_start(out=lnbb[:], in_=bass.AP(lnb, 0, [[0, 128], [1, 1024]])), writes=[T_ln2])
                P.dma('sp', lambda e: e.dma_start(out=wsf[:], in_=wsT.ap()), writes=[T_ws])
                P.dma('sp', lambda e: e.dma_start(out=bs[:], in_=bsT.ap()), writes=[T_bs])
                P.op('dve', lambda e: e.memset(wsf[64:128, :, 0:64], 0.0), reads=[T_ws], writes=[T_ws])
                P.op('dve', lambda e: e.tensor_copy(wsb[:], wsf[:]), reads=[T_ws], writes=[T_ws])

                def evac_uv(pn, tt, ps, T_ps):
                    if pn < 2:
                        P.op('act', lambda e, pn=pn, tt=tt, ps=ps: e.activation(out=gu[:, tt, pn * 512:(pn + 1) * 512], in_=ps[:], func=AF.Gelu),
                             reads=[T_ps], writes=[T_gu[tt]])
                    else:
                        P.op('act', lambda e, pn=pn, tt=tt, ps=ps: e.activation(out=gv[:, tt, (pn - 2) * 512:(pn - 1) * 512], in_=ps[:], func=AF.Gelu),
                             reads=[T_ps], writes=[T_gv[tt]])
                proj_tok(S, RB, w_in, 9216, 16, 0, 4, aT, T_aT, 512, 8, evac_uv)
                stt_ = R4.alloc("lnstat", [128, 8, 8]); T_lst = P.tiles_n("lnst", 8)
                tmp = R4.alloc("lnt", [128, 1024]); T_tmp = P.tile("lnt")
                tmp2 = R4.alloc("lnu", [128, 1024]); T_tmp2 = P.tile("lnu")
                junk2 = R4.alloc("junk2", [128, 1024], BF16); T_j2 = P.tile("junk2")
                vln = [R4.alloc("vln", [128, 1024], BF16) for i in range(2)]; T_vln = P.tiles_n("vln", 2)
                ya = [R4.alloc("ya", [128, 1024], BF16) for i in range(2)]; T_ya = P.tiles_n("ya", 2)
                sp_ = [PS(S, "sgp%d" % i, [128, 1024]) for i in range(2)]; T_sp = P.tiles_n("sgp", 2)
                ytp = PS(S, "ytp", [128, 8, 128], BF16); T_ytp = P.tile("ytp")
                for tt in range(8):
                    s = tt % 2
                    P.op('dve', lambda e, tt=tt: e.tensor_reduce(out=stt_[:, tt, 0:1], in_=gv[:, tt, :], axis=mybir.AxisListType.X, op=ALU.add),
                         reads=[T_gv[tt]], writes=[T_lst[tt]])
                    P.op('dve', lambda e, tt=tt: e.tensor_scalar(out=stt_[:, tt, 1:2], in0=stt_[:, tt, 0:1], scalar1=1.0 / 1024, scalar2=None, op0=ALU.mult),
                         reads=[T_lst[tt]], writes=[T_lst[tt]])
                    P.op('dve', lambda e, tt=tt: e.tensor_scalar(out=tmp[:], in0=gv[:, tt, :], scalar1=stt_[:, tt, 1:2], scalar2=None, op0=ALU.subtract),
                         reads=[T_gv[tt], T_lst[tt]], writes=[T_tmp])
                    P.op('act', lambda e, tt=tt: e.activation(out=junk2[:], in_=tmp[:], func=AF.Square, accum_out=stt_[:, tt, 2:3]),
                         reads=[T_tmp], writes=[T_j2, T_lst[tt]])
                    P.op('act', lambda e, tt=tt: e.activation(out=stt_[:, tt, 3:4], in_=stt_[:, tt, 2:3], func=AF.Sqrt, scale=1.0 / 1024, bias=EPS),
                         reads=[T_lst[tt]], writes=[T_lst[tt]])
                    P.op('dve', lambda e, tt=tt: e.reciprocal(stt_[:, tt, 4:5], stt_[:, tt, 3:4]), reads=[T_lst[tt]], writes=[T_lst[tt]])
                    P.op('dve', lambda e, tt=tt: e.scalar_tensor_tensor(out=tmp2[:], in0=tmp[:], scalar=stt_[:, tt, 4:5], in1=lngb[:], op0=ALU.mult, op1=ALU.mult),
                         reads=[T_tmp, T_lst[tt], T_ln], writes=[T_tmp2])
                    P.op('dve', lambda e, s=s: e.tensor_tensor(out=vln[s][:], in0=tmp2[:], in1=lnbb[:], op=ALU.add),
                         reads=[T_tmp2, T_ln2], writes=[T_vln[s]])
                    for g in range(4):
                        P.op('pe', lambda e, s=s, g=g: e.matmul(sp_[s][:, g * 256:(g + 1) * 256], lhsT=wsb[:, g, :], rhs=vln[s][:, g * 256:(g + 1) * 256], start=True, stop=True),
                             reads=[T_vln[s], T_ws], writes=[T_sp[s]], inc=(g == 3))
                    for g in range(4):
                        P.op('dve', lambda e, s=s, g=g, tt=tt: e.scalar_tensor_tensor(out=ya[s][:, g * 256:(g + 1) * 256], in0=sp_[s][:, g * 256:(g + 1) * 256], scalar=bs[:, g:g + 1],
                                                                               in1=gu[:, tt, g * 256:(g + 1) * 256], op0=ALU.add, op1=ALU.mult),
                             reads=[T_sp[s], T_bs, T_gu[tt]], writes=[T_ya[s]])
                    for k in range(8):
                        P.op('pe', lambda e, s=s, k=k: e.transpose(out=ytp[:, k, :], in_=ya[s][:, k * 128:(k + 1) * 128], identity=identb[:]),
                             reads=[T_ya[s], T_c2], writes=[T_ytp], inc=(k == 7))
                    P.op('act', lambda e, tt=tt: e.activation(out=yaT[:, :, tt * 128:(tt + 1) * 128], in_=ytp[:], func=AF.Copy), reads=[T_ytp], writes=[T_yaT])
                P.flush()
            with ExitStack() as S:
                RS.reset(); R4.reset()
                wq = [RS.alloc("wqkv", [128, 3, 16, 128], BF16) for i in range(2)]; T_wq = P.tiles_n("wqkv", 2)
                bt = [RS.alloc("bt", [128, 5, 128]) for i in range(2)]; T_bt = P.tiles_n("bt", 2)
                km = RS.alloc("km", [128, 12]); T_km = P.tile("km")
                P.dma('sp', lambda e: e.dma_start(out=km[:], in_=keymask.ap()), writes=[T_km])
                qT = [RS.alloc("qT", [128, 1024], BF16) for i in range(2)]; T_qT = P.tiles_n("qT", 2)
                kT = [RS.alloc("kT", [128, 1536], BF16) for i in range(2)]; T_kT = P.tiles_n("kT", 2)
                vh = [RS.alloc("vh", [128, 12, 128], BF16) for i in range(2)]; T_vh = P.tiles_n("vh", 2)
                pT = [RS.alloc("pT", [128, 5, 128], BF16) for i in range(2)]; T_pT = P.tiles_n("pT", 2)
                rr = [RS.alloc("rr", [128, 128]) for i in range(2)]; T_rr = P.tiles_n("rr", 2)
                pj = [PS(S, "pj%d" % i, [128, 512]) for i in range(2)]; T_pj = P.tiles_n("pj", 2)
                scA = PS(S, "scA", [128, 4, 128]); T_scA = P.tile("scA")
                scB = PS(S, "scB", [128, 4, 128]); T_scB = P.tile("scB")
                po = [PS(S, "po%d" % i, [128, 4, 128]) for i in range(2)]; T_po = P.tiles_n("po", 2)
                cnt = 0
                for hd in range(8):
                    s = hd % 2
                    for i3, cbase in enumerate((2048, 3072, 4096)):
                        P.dma('pool', lambda e, s=s, i3=i3, cbase=cbase, hd=hd: e.dma_start(out=wq[s][:, i3, :, :], in_=wpanel(w_in, 9216, 16, cbase + hd * 128, 128)),
                              writes=[T_wq[s]], part=(i3 > 0))
                    P.dma('sp', lambda e, s=s, hd=hd: e.dma_start(out=bt[s][:], in_=BTd.ap()[:, hd, :, :]), writes=[T_bt[s]])
                    for hf in range(2):
                        b = cnt % 2; cnt += 1
                        for kc in range(16):
                            P.op('pe', lambda e, s=s, kc=kc, b=b, hf=hf: e.matmul(pj[b][:], lhsT=wq[s][:, 0, kc, :], rhs=aT[:, kc, 512 + hf * 512:1024 + hf * 512], start=(kc == 0), stop=(kc == 15)),
                                 reads=[T_wq[s], T_aT], writes=[T_pj[b]], inc=(kc == 15))
                        P.op('act', lambda e, s=s, b=b, hf=hf: e.activation(out=qT[s][:, hf * 512:(hf + 1) * 512], in_=pj[b][:], func=AF.Copy, scale=128.0 ** -0.5),
                             reads=[T_pj[b]], writes=[T_qT[s]])
                    for hf in range(3):
                        b = cnt % 2; cnt += 1
                        for kc in range(16):
                            P.op('pe', lambda e, s=s, kc=kc, b=b, hf=hf: e.matmul(pj[b][:], lhsT=wq[s][:, 1, kc, :], rhs=aT[:, kc, hf * 512:(hf + 1) * 512], start=(kc == 0), stop=(kc == 15)),
                                 reads=[T_wq[s], T_aT], writes=[T_pj[b]], inc=(kc == 15))
                        P.op('dve', lambda e, s=s, b=b, hf=hf: e.tensor_copy(kT[s][:, hf * 512:(hf + 1) * 512], pj[b][:]),
                             reads=[T_pj[b]], writes=[T_kT[s]])
                    for g4 in range(3):
                        b = cnt % 2; cnt += 1
                        for kb in range(4):
                            blk = g4 * 4 + kb
                            for kc in range(16):
                                P.op('pe', lambda e, s=s, kc=kc, b=b, kb=kb, blk=blk: e.matmul(pj[b][:, kb * 128:(kb + 1) * 128], lhsT=aT[:, kc, blk * 128:(blk + 1) * 128], rhs=wq[s][:, 2, kc, :],
                                                                                     start=(kc == 0), stop=(kc == 15)),
                                     reads=[T_wq[s], T_aT], writes=[T_pj[b]], inc=(kc == 15 and kb == 3))
                        P.op('act', lambda e, s=s, b=b, g4=g4: e.activation(out=vh[s][:, g4 * 4:(g4 + 1) * 4, :], in_=pj[b][:].rearrange("p (a b) -> p a b", b=128), func=AF.Copy),
                             reads=[T_pj[b]], writes=[T_vh[s]])
                    for m in range(8):
                        u = m % 2
                        for ki in range(5):
                            dst = scA[:, ki, :] if ki < 4 else scB[:, 0, :]
                            Tsc = T_scA if ki < 4 else T_scB
                            P.op('pe', lambda e, s=s, ki=ki, m=m, dst=dst: e.matmul(dst, lhsT=kT[s][:, (m + ki) * 128:(m + ki + 1) * 128], rhs=qT[s][:, m * 128:(m + 1) * 128], start=True, stop=False),
                                 reads=[T_kT[s], T_qT[s]], writes=[Tsc], inc=False)
                            P.op('pe', lambda e, s=s, ki=ki, dst=dst: e.matmul(dst, lhsT=identf[:], rhs=bt[s][:, ki, :], start=False, stop=True),
                                 reads=[T_bt[s], T_c], writes=[Tsc], inc=True)
                            P.op('act', lambda e, u=u, ki=ki, m=m, dst=dst: e.activation(out=pT[u][:, ki, :], in_=dst, func=AF.Exp, bias=km[:, m + ki:m + ki + 1], scale=1.0),
                                 reads=[Tsc, T_km], writes=[T_pT[u]])
                        for ki in range(5):
                            P.op('pe', lambda e, s=s, u=u, ki=ki, m=m: e.matmul(po[u][:, 0, :], lhsT=vh[s][:, m + ki, :], rhs=pT[u][:, ki, :], start=(ki == 0), stop=(ki == 4)),
                                 reads=[T_vh[s], T_pT[u]], writes=[T_po[u]], inc=False)
                        for ki in range(5):
                            P.op('pe', lambda e, u=u, ki=ki: e.matmul(po[u][:, 1, :], lhsT=onesb[:], rhs=pT[u][:, ki, :], start=(ki == 0), stop=(ki == 4), skip_group_check=True),
                                 reads=[T_c4, T_pT[u]], writes=[T_po[u]], inc=(ki == 4))
                        P.op('dve', lambda e, u=u: e.reciprocal(rr[u][:], po[u][:, 1, :]), reads=[T_po[u]], writes=[T_rr[u]])
                        P.op('dve', lambda e, u=u, hd=hd, m=m: e.tensor_tensor(out=ybT[:, hd, m * 128:(m + 1) * 128], in0=po[u][:, 0, :], in1=rr[u][:], op=ALU.mult),
                             reads=[T_po[u], T_rr[u]], writes=[T_ybT])
                P.flush()
            with ExitStack() as S:
                RS.reset(); R4.reset()
                wg = [RS.alloc("wg", [128, 2, 16, 128], BF16) for i in range(2)]; T_wg = P.tiles_n("wg", 2)
                wu = [RS.alloc("wu", [128, 2, 8, 128], BF16) for i in range(2)]; T_wu = P.tiles_n("wu", 2)
                sg_ = [RS.alloc("sg", [128, 512]) for i in range(2)]; T_sg = P.tiles_n("sg", 2)
                m1 = [RS.alloc("m1", [128, 512]) for i in range(2)]; T_m1 = P.tiles_n("m1", 2)
                pg = [PS(S, "pg%d" % i, [128, 512]) for i in range(4)]; T_pg = P.tiles_n("pg", 4)
                pu_ = [PS(S, "pu%d" % i, [128, 512]) for i in range(4)]; T_pu = P.tiles_n("pu", 4)
                cnt = 0
                for jb in range(16):
                    s = jb % 2
                    P.dma('pool', lambda e, s=s, jb=jb: e.dma_start(out=wg[s][:, 0, :, :], in_=wpanel(w_in, 9216, 16, 5120 + jb * 128, 128)), writes=[T_wg[s]])
                    P.dma('pool', lambda e, s=s, jb=jb: e.dma_start(out=wg[s][:, 1, :, :], in_=wpanel(w_in, 9216, 16, 7168 + jb * 128, 128)), writes=[T_wg[s]], part=True)
                    P.dma('pool', lambda e, s=s, jb=jb: e.dma_start(out=wu[s][:, 0, :, :], in_=wpanel(w_up_a, 2048, 8, jb * 128, 128)), writes=[T_wu[s]])
                    P.dma('pool', lambda e, s=s, jb=jb: e.dma_start(out=wu[s][:, 1, :, :], in_=wpanel(w_up_b, 2048, 8, jb * 128, 128)), writes=[T_wu[s]], part=True)
                    for hf in range(2):
                        for br in range(2):
                            b = cnt % 4; cnt += 1
                            srcY, T_srcY = (yaT, T_yaT) if br == 0 else (ybT, T_ybT)
                            for kc in range(16):
                                P.op('pe', lambda e, s=s, br=br, kc=kc, b=b, hf=hf: e.matmul(pg[b][:], lhsT=wg[s][:, br, kc, :], rhs=aT[:, kc, 512 + hf * 512:1024 + hf * 512], start=(kc == 0), stop=(kc == 15)),
                                     reads=[T_wg[s], T_aT], writes=[T_pg[b]], inc=(kc == 15))
                            for kc in range(8):
                                P.op('pe', lambda e, s=s, br=br, kc=kc, b=b, hf=hf, srcY=srcY: e.matmul(pu_[b][:], lhsT=wu[s][:, br, kc, :], rhs=srcY[:, kc, hf * 512:(hf + 1) * 512], start=(kc == 0), stop=(kc == 7)),
                                     reads=[T_wu[s], T_srcY], writes=[T_pu[b]], inc=(kc == 7))
                            P.op('act', lambda e, br=br, b=b: e.activation(out=sg_[br][:], in_=pg[b][:], func=AF.Sigmoid), reads=[T_pg[b]], writes=[T_sg[br]])
                            P.op('dve', lambda e, b=b, br=br: e.tensor_tensor(out=m1[br][:], in0=sg_[br][:], in1=pu_[b][:], op=ALU.mult), reads=[T_sg[br], T_pu[b]], writes=[T_m1[br]])
                            if br == 1:
                                P.op('dve', lambda e, jb=jb, hf=hf: e.tensor_tensor(out=mT[:, jb, hf * 512:(hf + 1) * 512], in0=m1[0][:], in1=m1[1][:], op=ALU.add),
                                     reads=[T_m1[0], T_m1[1]], writes=[T_mT])
                P.flush()
            with ExitStack() as S:
                R1.reset()
                load_x_into_h()
                proj_tok(S, R1, w_out, 2048, 16, 0, 4, mT, T_mT, 0, 8, add_to_h)
                P.flush()
        else:
            load_x_into_h()
            P.flush()

        if stage >= 2:
            for r in (R1, R2, R3, R4):
                r.reset()
            a2T = R1.alloc("a2T", [128, 16, 1024], BF16); T_a2T = P.tile("a2T")
            mnT = R4.alloc("mnT", [128, 16, 256], BF16); T_mnT = P.tile("mnT")
            kmT = R4.alloc("kmT", [128, 16, 256], BF16); T_kmT = P.tile("kmT")
            RX = Region(75 * KB + 32 * KB, 123 * KB)
            vm = RX.alloc("vm", [128, 2, 2048], BF16); T_vm = P.tile("vm")
            with ExitStack() as S:
                R2.reset()
                load_gb(1)
                rmsnorm_T(S, R2, lambda tt: (h[:, tt, :], T_h[tt]), 8, a2T, T_a2T, 0)
                P.flush()
            with ExitStack() as S:
                R2.reset()
                load_gb(2)
                mt = [R2.alloc("mt", [128, 2048]) for i in range(2)]; T_mt = P.tiles_n("mt", 2)

                def src_m(tt):
                    P.dma('sp', lambda e, tt=tt: e.dma_start(out=mt[tt][:], in_=memx.ap()[tt * 128:(tt + 1) * 128, :]), writes=[T_mt[tt]])
                    return mt[tt][:], T_mt[tt]
                R3.reset()
                rmsnorm_T(S, R3, src_m, 2, mnT, T_mnT, 0)
                P.flush()
            with ExitStack() as S:
                R3.reset()
                proj_feat(S, R3, mem_w_kv, 4096, 16, 0, 16, mnT, T_mnT, 0, 256, copy_evac(lambda oc, t0, n: kmT[:, oc, t0:t0 + n], T_kmT))
                P.flush()
            with ExitStack() as S:
                R3.reset()

                def ev_vm(pn, tt, ps, T_ps):
                    P.op('act', lambda e, pn=pn, tt=tt, ps=ps: e.activation(out=vm[:, tt, pn * 512:(pn + 1) * 512], in_=ps[:], func=AF.Copy), reads=[T_ps], writes=[T_vm])
                proj_tok(S, R3, mem_w_kv, 4096, 16, 2048, 4, mnT, T_mnT, 0, 2, ev_vm)
                P.flush()
            R2.reset()
            q2T = R2.alloc("q2T", [128, 16, 1024], BF16); T_q2T = P.tile("q2T")
            with ExitStack() as S:
                R3.reset()
                proj_feat(S, R3, mem_w_q, 2048, 16, 0, 16, a2T, T_a2T, 0, 1024, copy_evac(lambda oc, t0, n: q2T[:, oc, t0:t0 + n], T_q2T))
                P.flush()
            onT = a2T; T_onT = P.tile("onT")
            with ExitStack() as S:
                R3.reset()
                pT2 = [R3.alloc("pT2", [128, 2, 512], BF16) for i in range(2)]; T_pT2 = P.tiles_n("pT2", 2)
                r2 = [R3.alloc("r2", [128, 512]) for i in range(2)]; T_r2 = P.tiles_n("r2", 2)
                sc2 = [PS(S, "sc2%d" % i, [128, 512]) for i in range(2)]; T_sc2 = P.tiles_n("sc2", 2)
                rs2 = PS(S, "rs2", [128, 512]); T_rs2 = P.tile("rs2")
                o2 = [PS(S, "o2%d" % i, [128, 512]) for i in range(2)]; T_o2 = P.tiles_n("o2", 2)
                it = 0
                for hd in range(4):
                    for hf in range(2):
                        u = it % 2; it += 1
                        for mb in range(2):
                            for kc in range(4):
                                P.op('pe', lambda e, hd=hd, hf=hf, mb=mb, kc=kc: e.matmul(sc2[mb][:], lhsT=kmT[:, hd * 4 + kc, mb * 128:(mb + 1) * 128], rhs=q2T[:, hd * 4 + kc, hf * 512:(hf + 1) * 512],
                                                                                    start=(kc == 0), stop=(kc == 3)),
                                     reads=[T_kmT, T_q2T], writes=[T_sc2[mb]], inc=(kc == 3))
                            P.op('act', lambda e, u=u, mb=mb: e.activation(out=pT2[u][:, mb, :], in_=sc2[mb][:], func=AF.Exp, scale=512.0 ** -0.5),
                                 reads=[T_sc2[mb]], writes=[T_pT2[u]])
                        for mb in range(2):
                            P.op('pe', lambda e, u=u, mb=mb: e.matmul(rs2[:], lhsT=onesb[:], rhs=pT2[u][:, mb, :], start=(mb == 0), stop=(mb == 1)),
                                 reads=[T_c4, T_pT2[u]], writes=[T_rs2], inc=(mb == 1))
                        P.op('dve', lambda e, u=u: e.reciprocal(r2[u][:], rs2[:]), reads=[T_rs2], writes=[T_r2[u]])
                        for oc in range(4):
                            ob = oc % 2
                            for mb in range(2):
                                P.op('pe', lambda e, u=u, mb=mb, hd=hd, oc=oc, ob=ob: e.matmul(o2[ob][:], lhsT=vm[:, mb, hd * 512 + oc * 128:hd * 512 + (oc + 1) * 128], rhs=pT2[u][:, mb, :],
                                                                                         start=(mb == 0), stop=(mb == 1)),
                                     reads=[T_vm, T_pT2[u]], writes=[T_o2[ob]], inc=(mb == 1))
                            P.op('dve', lambda e, u=u, hd=hd, oc=oc, ob=ob, hf=hf: e.tensor_tensor(out=onT[:, hd * 4 + oc, hf * 512:(hf + 1) * 512], in0=o2[ob][:], in1=r2[u][:], op=ALU.mult),
                                 reads=[T_o2[ob], T_r2[u]], writes=[T_onT])
                P.flush()
            with ExitStack() as S:
                R2.reset()
                proj_tok(S, R2, mem_w_o, 2048, 16, 0, 4, onT, T_onT, 0, 8, add_to_h)
                P.flush()

        if stage >= 3:
            for r in (R1, R2, R3, R4):
                r.reset()
            a3T = R1.alloc("a3T", [128, 16, 1024], BF16); T_a3T = P.tile("a3T")
            RX = Region(75 * KB + 32 * KB, 123 * KB)
            qT3 = R2.alloc("qT3", [128, 32, 2, 2, 128], BF16); T_qT3 = P.tile("qT3")
            with ExitStack() as S:
                R3.reset()
                load_gb(3)
                rmsnorm_T(S, R3, lambda tt: (h[:, tt, :], T_h[tt]), 8, a3T, T_a3T, 0)
                P.flush()
            with ExitStack() as S:
                R3.reset()
                def ev_q3(oc, t0, n, ps, T_ps, cnt):
                    h_, pp = oc // 2, oc % 2
                    hh, hl = h_ // 4, h_ % 4
                    dst = bass.AP(qT3, hh * 256 + pp * 128 + hl * 32 + (t0 // 32) * 512, [[16384, 128], [512, n // 32], [1, 32]])
                    src = ps[:, 0:n].rearrange("p (a b) -> p a b", b=32)
                    if cnt % 2 == 0:
                        P.op('act', lambda e, dst=dst, src=src: e.activation(out=dst, in_=src, func=AF.Copy), reads=[T_ps], writes=[T_qT3])
                    else:
                        P.op('dve', lambda e, dst=dst, src=src: e.tensor_copy(dst, src), reads=[T_ps], writes=[T_qT3])
                proj_feat(S, R3, peer_w_q, 2048, 16, 0, 16, a3T, T_a3T, 0, 1024, ev_q3)
                P.flush()
            with ExitStack() as S:
                R3.reset(); R4.reset(); RX.reset()
                keysb = RX.alloc("keysb", [128, 2, 128], BF16); T_keys = P.tile("keys")
                P.dma('pool', lambda e: e.dma_start(out=keysb[:], in_=keysTd.ap()), writes=[T_keys])
                s_sb = [RX.alloc("s_sb", [128, 2, 128]) for i in range(8)]; T_s = P.tiles_n("s_sb", 8)
                s1p = [RX.alloc("s1p", [128, 128]) for i in range(8)]; T_s1p = P.tiles_n("s1p", 8)
                t16 = [R4.alloc("t16", [128, 2, 16]) for i in range(8)]; T_t16 = P.tiles_n("t16", 8)
                c16 = [R4.alloc("c16", [128, 16]) for i in range(8)]; T_c16 = P.tiles_n("c16", 8)
                scal = [R4.alloc("scal", [128, 8]) for i in range(8)]; T_scal = P.tiles_n("scal", 8)
                tmpk = R4.alloc("tmpk", [128, 128]); T_tmpk = P.tile("tmpk")
                cand = R4.alloc("cand", [128, 256]); T_cand = P.tile("cand")
                tmpc = R4.alloc("tmpc", [128, 256]); T_tmpc = P.tile("tmpc")
                j16 = R4.alloc("j16", [128, 16]); T_j16 = P.tile("j16")
                Dd = [R3.alloc("Dd", [128, 1024]) for i in range(4)]; T_D = P.tiles_n("Dd", 4)
                Ee = [R3.alloc("Ee", [128, 1024], BF16) for i in range(3)]; T_E = P.tiles_n("Ee", 3)
                Gm = [R3.alloc("Gm", [128, 1024], BF16) for i in range(3)]; T_Gm = P.tiles_n("Gm", 3)
                Go = [R3.alloc("Go", [128, 1024], BF16) for i in range(2)]; T_Go = P.tiles_n("Go", 2)
                sps = PS(S, "sps", [128, 4, 128]); T_sps = P.tile("sps")
                gps = [PS(S, "gps%d" % i, [128, 2, 512]) for i in range(2)]; T_gps = P.tiles_n("gps", 2)
                T_Gd = P.tile("Gd")
                for st_ in range(8):
                    tb = st_ * 128
                    for ti in range(8):
                        q, hh = ti // 2, ti % 2
                        for pp in range(2):
                            lh = qT3[:, st_ * 4 + q, hh, pp, :]
                            P.op('pe', lambda e, pp=pp, lh=lh: e.matmul(sps[:, pp, :], lhsT=lh, rhs=keysb[:, pp, :], start=True, stop=True),
                                 reads=[T_qT3, T_keys], writes=[T_sps], inc=(pp == 1))
                        P.op('act', lambda e, ti=ti: e.activation(out=s_sb[ti][:], in_=sps[:, 0:2, :], func=AF.Copy), reads=[T_sps], writes=[T_s[ti]])
                        for pp in range(2):
                            P.op('dve', lambda e, ti=ti, pp=pp: e.max(out=t16[ti][:, pp, 0:8], in_=s_sb[ti][:, pp, :]), reads=[T_s[ti]], writes=[T_t16[ti]])
                            P.op('dve', lambda e, ti=ti, pp=pp: e.match_replace(out=tmpk[:], in_to_replace=t16[ti][:, pp, 0:8], in_values=s_sb[ti][:, pp, :], imm_value=NEG),
                                 reads=[T_s[ti], T_t16[ti]], writes=[T_tmpk])
                            P.op('dve', lambda e, ti=ti, pp=pp: e.max(out=t16[ti][:, pp, 8:16], in_=tmpk[:]), reads=[T_tmpk], writes=[T_t16[ti]])
                        P.op('dve', lambda e, ti=ti: e.tensor_tensor(out=cand[:].rearrange("p (a b) -> p a b", b=16), in0=bass.AP(t16[ti], 0, [[32, 128], [1, 16], [0, 16]]),
                                                                  in1=bass.AP(t16[ti], 16, [[32, 128], [0, 16], [1, 16]]), op=ALU.add),
                             reads=[T_t16[ti]], writes=[T_cand])
                        P.op('dve', lambda e, ti=ti: e.max(out=c16[ti][:, 0:8], in_=cand[:]), reads=[T_cand], writes=[T_c16[ti]])
                        P.op('dve', lambda e, ti=ti: e.match_replace(out=tmpc[:], in_to_replace=c16[ti][:, 0:8], in_values=cand[:], imm_value=NEG),
                             reads=[T_cand, T_c16[ti]], writes=[T_tmpc])
                        P.op('dve', lambda e, ti=ti: e.max(out=c16[ti][:, 8:16], in_=tmpc[:]), reads=[T_tmpc], writes=[T_c16[ti]])
                        P.op('dve', lambda e, ti=ti: e.tensor_scalar(out=scal[ti][:, 0:1], in0=c16[ti][:, 0:1], scalar1=-1.0, scalar2=None, op0=ALU.mult),
                             reads=[T_c16[ti]], writes=[T_scal[ti]])
                        P.op('act', lambda e, ti=ti: e.activation(out=j16[:], in_=c16[ti][:], func=AF.Exp, bias=scal[ti][:, 0:1], scale=1.0, accum_out=scal[ti][:, 1:2]),
                             reads=[T_c16[ti], T_scal[ti]], writes=[T_j16, T_scal[ti]])
                        P.op('act', lambda e, ti=ti: e.activation(out=scal[ti][:, 2:3], in_=scal[ti][:, 1:2], func=AF.Ln), reads=[T_scal[ti]], writes=[T_scal[ti]])
                        P.op('dve', lambda e, ti=ti: e.scalar_tensor_tensor(out=scal[ti][:, 3:4], in0=c16[ti][:, 15:16], scalar=scal[ti][:, 0:1], in1=scal[ti][:, 2:3], op0=ALU.add, op1=ALU.subtract),
                             reads=[T_c16[ti], T_scal[ti]], writes=[T_scal[ti]])
                        P.op('dve', lambda e, ti=ti: e.tensor_scalar(out=s1p[ti][:], in0=s_sb[ti][:, 0, :], scalar1=c16[ti][:, 15:16], scalar2=None, op0=ALU.subtract),
                             reads=[T_s[ti], T_c16[ti]], writes=[T_s1p[ti]])
                    NIT = 128
                    for k in range(NIT + 2):
                        if k < NIT:
                            eb, ti = k // 8, k % 8
                            u = k % 4
                            deng = 'dve' if (k % 3 == 2) else 'pool'
                            P.op(deng, lambda e, ti=ti, eb=eb, u=u: e.tensor_tensor(out=Dd[u][:].rearrange("p (a b) -> p a b", b=128), in0=bass.AP(s1p[ti], eb * 8, [[128, 128], [1, 8], [0, 128]]),
                                                                                 in1=bass.AP(s_sb[ti], 128, [[256, 128], [0, 8], [1, 128]]), op=ALU.add),
                                 reads=[T_s1p[ti], T_s[ti]], writes=[T_D[u]])
                        k1 = k - 1
                        if 0 <= k1 < NIT:
                            ti = k1 % 8
                            P.op('act', lambda e, ti=ti, k1=k1: e.activation(out=Ee[k1 % 3][:], in_=Dd[k1 % 4][:], func=AF.Exp, bias=scal[ti][:, 3:4], scale=1.0),
                                 reads=[T_D[k1 % 4], T_scal[ti]], writes=[T_E[k1 % 3]])
                        k2 = k - 2
                        if 0 <= k2 < NIT:
                            eb, ti = k2 // 8, k2 % 8
                            q, hh = ti // 2, ti % 2
                            gsl = eb % 2
                            P.op('dve', lambda e, k2=k2: e.scalar_tensor_tensor(out=Gm[k2 % 3][:], in0=Dd[k2 % 4][:], scalar=-1e-5, in1=Ee[k2 % 3][:], op0=ALU.is_ge, op1=ALU.mult),
                                 reads=[T_D[k2 % 4], T_E[k2 % 3]], writes=[T_Gm[k2 % 3]])
                            for h2 in range(2):
                                P.op('pe', lambda e, k2=k2, h2=h2, q=q, hh=hh, gsl=gsl: e.matmul(gps[gsl][32 * q:32 * q + 32, h2, :], lhsT=sel32b[:], rhs=Gm[k2 % 3][:, h2 * 512:(h2 + 1) * 512],
                                                                                           start=(hh == 0), stop=(hh == 1), tile_position=(0, 32 * q), skip_group_check=True),
                                     reads=[T_Gm[k2 % 3], T_c3], writes=[T_gps[gsl]], inc=(h2 == 1))
                            if ti == 7:
                                P.op('act', lambda e, gsl=gsl: e.activation(out=Go[gsl][:], in_=gps[gsl][:].rearrange("p a b -> p (a b)"), func=AF.Copy), reads=[T_gps[gsl]], writes=[T_Go[gsl]])
                                P.dma('sp', lambda e, gsl=gsl, tb=tb, eb=eb: e.dma_start(out=Gd.ap()[tb:tb + 128, eb * 1024:(eb + 1) * 1024], in_=Go[gsl][:]), reads=[T_Go[gsl]], writes=[T_Gd], part=True)
                P.flush()
            with ExitStack() as S:
                R2.reset(); R3.reset(); R4.reset(); RX.reset()
                RP = Region(123 * KB, 207 * KB)
                GI = 4
                ust = [RP.alloc("ust", [128, 2048], BF16) for i in range(2)]; T_ust = P.tiles_n("ust", 2)
                UT = [RP.alloc("UT", [128, 16, 128], BF16) for i in range(2)]; T_UT = P.tiles_n("UT", 2)
                vg = [RP.alloc("vg", [128, GI, 2048], BF16) for i in range(2)]; T_vg = P.tiles_n("vg", 2)
                gg = [RP.alloc("gg", [128, 8, GI * 128], BF16) for i in range(2)]; T_gg = P.tiles_n("gg", 2)
                actT = [RP.alloc("actT", [128, GI, 1024], BF16) for i in range(2)]; T_actT = P.tiles_n("actT", 2)
                gl = [RP.alloc("gl", [128, 512], BF16) for i in range(2)]; T_gl = P.tiles_n("gl", 2)
                utp = PS(S, "utp", [128, 8, 128], BF16); T_utp = P.tile("utp")
                gtp = PS(S, "gtp", [128, 8, 128], BF16); T_gtp = P.tile("gtp")
                pps = [PS(S, "pps%d" % i, [128, 512]) for i in range(2)]; T_pps = P.tiles_n("pps", 2)
                ops_ = [PS(S, "ops%d" % i, [128, 2, 512]) for i in range(2)]; T_ops = P.tiles_n("ops", 2)
                ci = 0
                oi = 0
                for g in range(16384 // (128 * GI)):
                    gs = g % 2
                    P.dma('pool', lambda e, g=g, gs=gs: e.dma_start(out=vg[gs][:], in_=bass.AP(pv, g * GI * 128 * 2048, [[2048, 128], [128 * 2048, GI], [1, 2048]])), writes=[T_vg[gs]])
                    P.dma('sp', lambda e, g=g, gs=gs: e.dma_start(out=gg[gs][:], in_=bass.AP(Gd, g * GI * 128, [[16384, 128], [128 * 16384, 8], [1, GI * 128]])), writes=[T_gg[gs]])
                    for c in range(GI):
                        us = ci % 2; ci += 1
                        ch = g * GI + c
                        P.dma('pool', lambda e, ch=ch, us=us: e.dma_start(out=ust[us][:], in_=pu.ap()[ch * 128:(ch + 1) * 128, :]), writes=[T_ust[us]])
                        for hf in range(2):
                            for k in range(8):
                                dc = hf * 8 + k
                                P.op('pe', lambda e, us=us, k=k, dc=dc: e.transpose(out=utp[:, k, :], in_=ust[us][:, dc * 128:(dc + 1) * 128], identity=identb[:]),
                                     reads=[T_ust[us], T_c2], writes=[T_utp], inc=(k == 7))
                            if hf == 0:
                                P.op('act', lambda e, us=us: e.activation(out=UT[us][:, 0:8, :], in_=utp[:], func=AF.Copy), reads=[T_utp], writes=[T_UT[us]])
                            else:
                                P.op('dve', lambda e, us=us: e.tensor_copy(UT[us][:, 8:16, :], utp[:]), reads=[T_utp], writes=[T_UT[us]])
                        for tt in range(8):
                            P.op('pe', lambda e, gs=gs, tt=tt, c=c: e.transpose(out=gtp[:, tt, :], in_=gg[gs][:, tt, c * 128:(c + 1) * 128], identity=identb[:]),
                                 reads=[T_gg[gs], T_c2], writes=[T_gtp], inc=(tt == 7))
                        for hf in range(2):
                            for dc in range(16):
                                P.op('pe', lambda e, us=us, dc=dc, hf=hf: e.matmul(pps[hf][:], lhsT=UT[us][:, dc, :], rhs=a3T[:, dc, hf * 512:(hf + 1) * 512], start=(dc == 0), stop=(dc == 15)),
                                     reads=[T_UT[us], T_a3T], writes=[T_pps[hf]], inc=(dc == 15))
                            P.op('act', lambda e, hf=hf: e.activation(out=gl[hf][:], in_=pps[hf][:], func=AF.Gelu), reads=[T_pps[hf]], writes=[T_gl[hf]])
                            P.op('dve', lambda e, hf=hf, gs=gs, c=c: e.tensor_tensor(out=actT[gs][:, c, hf * 512:(hf + 1) * 512].rearrange("p (a b) -> p a b", b=128),
                                                                                  in0=gl[hf][:].rearrange("p (a b) -> p a b", b=128), in1=gtp[:, hf * 4:(hf + 1) * 4, :], op=ALU.mult),
                                 reads=[T_gl[hf], T_gtp], writes=[T_actT[gs]])
                    for tt in range(8):
                        for dh in range(2):
                            ob = oi % 2; oi += 1
                            for d2 in range(2):
                                db = dh * 2 + d2
                                for c in range(GI):
                                    P.op('pe', lambda e, gs=gs, c=c, tt=tt, db=db, d2=d2, ob=ob: e.matmul(ops_[ob][:, d2, :], lhsT=actT[gs][:, c, tt * 128:(tt + 1) * 128], rhs=vg[gs][:, c, db * 512:(db + 1) * 512],
                                                                                                    start=(c == 0), stop=(c == GI - 1)),
                                         reads=[T_actT[gs], T_vg[gs]], writes=[T_ops[ob]], inc=(c == GI - 1 and d2 == 1))
                            P.op('dve', lambda e, tt=tt, dh=dh, ob=ob: e.tensor_tensor(out=h[:, tt, dh * 1024:(dh + 1) * 1024], in0=h[:, tt, dh * 1024:(dh + 1) * 1024],
                                                                                  in1=ops_[ob][:].rearrange("p a b -> p (a b)"), op=ALU.add),
                                 reads=[T_ops[ob], T_h[tt]], writes=[T_h[tt]])
                P.flush()

        with ExitStack() as S:
            R1.reset()
            load_gb(4)
            ot = [R1.alloc("ot", [128, 2048]) for i in range(2)]; T_ot = P.tiles_n("ot", 2)
            junk = R1.alloc("junkf", [128, 2048], BF16); T_j = P.tile("junkf")
            stat = R1.alloc("statf", [128, 24]); T_st = P.tiles_n("stf", 8)
            T_out = P.tile("out")
            for tt in range(8):
                s = tt % 2
                c = 3 * tt
                P.op('act', lambda e, tt=tt, c=c: e.activation(out=junk[:], in_=h[:, tt, :], func=AF.Square, accum_out=stat[:, c:c + 1]), reads=[T_h[tt]], writes=[T_j, T_st[tt]])
                P.op('act', lambda e, c=c: e.activation(out=stat[:, c + 1:c + 2], in_=stat[:, c:c + 1], func=AF.Sqrt, scale=1.0 / 2048, bias=EPS), reads=[T_st[tt]], writes=[T_st[tt]])
                P.op('dve', lambda e, c=c: e.reciprocal(stat[:, c + 2:c + 3], stat[:, c + 1:c + 2]), reads=[T_st[tt]], writes=[T_st[tt]])
                P.op('dve', lambda e, tt=tt, c=c, s=s: e.scalar_tensor_tensor(out=ot[s][:], in0=h[:, tt, :], scalar=stat[:, c + 2:c + 3], in1=gb[:], op0=ALU.mult, op1=ALU.mult),
                     reads=[T_h[tt], T_st[tt], T_gb], writes=[T_ot[s]])
                P.dma('sp', lambda e, tt=tt, s=s: e.dma_start(out=out.ap()[tt * 128:(tt + 1) * 128, :], in_=ot[s][:]), reads=[T_ot[s]], writes=[T_out], part=True)
            P.flush()
        print("bass instructions emitted:", P.n_instr, "dma sems:", P.nsem)
    return nc


_CACHE = {}


def _host_consts(att_rel_bias):
    kj = np.arange(640)[:, None]
    qi = np.arange(128)[None, :]
    dist = 512 + qi - kj
    idx = np.clip(dist, -128, 128) + 128
    valid = (qi // 64 <= kj // 64) & (kj // 64 <= 8 + qi // 64)
    rb = att_rel_bias[0]
    bt = rb[:, idx]
    bt = np.where(valid[None], bt, np.float32(NEG)).astype(np.float32)
    bt = bt.reshape(8, 5, 128, 128).transpose(2, 0, 1, 3)
    return np.ascontiguousarray(bt)


def kernel(x, mem, g_mix, w_in, sg_ln_g, sg_ln_b, sg_w_s, sg_b_s, att_rel_bias, w_up_a, w_up_b, w_out,
           g_mem_q, g_mem_kv, mem_w_q, mem_w_kv, mem_w_o, g_ffn, peer_w_q, peer_sub_keys, peer_u, peer_v, g_final,
           _stage=9):
    f = lambda a: np.ascontiguousarray(np.asarray(a, dtype=np.float32))
    x = f(x); mem = f(mem)
    nc = build(_stage)
    shared = {
        "gvecs": f(np.stack([np.asarray(g_mix)[0], np.asarray(g_mem_q)[0], np.asarray(g_mem_kv)[0], np.asarray(g_ffn)[0], np.asarray(g_final)])),
        "w_in": f(np.asarray(w_in)[0]), "sg_ln_g": f(np.asarray(sg_ln_g)[0]), "sg_ln_b": f(np.asarray(sg_ln_b)[0]),
        "sg_wsT": f(np.asarray(sg_w_s)[0].transpose(2, 0, 1)),
        "sg_bsT": f(np.asarray(sg_b_s)[0].T),
        "att_bt": _host_consts(f(att_rel_bias)),
        "w_up_a": f(np.asarray(w_up_a)[0]), "w_up_b": f(np.asarray(w_up_b)[0]), "w_out": f(np.asarray(w_out)[0]),
        "mem_w_q": f(np.asarray(mem_w_q)[0]), "mem_w_kv": f(np.asarray(mem_w_kv)[0]), "mem_w_o": f(np.asarray(mem_w_o)[0]),
        "peer_w_q": f(np.asarray(peer_w_q)[0]),
        "peer_keysT": f(np.asarray(peer_sub_keys)[0].transpose(2, 0, 1)),
        "peer_u": f(np.asarray(peer_u)[0]), "peer_v": f(np.asarray(peer_v)[0]),
        "ident": np.eye(128, dtype=np.float32),
        "sel32": np.ascontiguousarray(np.tile(np.eye(32, dtype=np.float32), (4, 1))),
    }
    in_maps = []
    for k in range(8):
        b, j = k // 4, k % 4
        xc = np.zeros((1536, 2048), np.float32)
        xc[512:] = x[b, j * 1024:(j + 1) * 1024]
        km = np.zeros((128, 12), np.float32)
        if j == 0:
            km[:, 0:4] = NEG
        else:
            xc[:512] = x[b, j * 1024 - 512:j * 1024]
        m = dict(shared)
        m["x"] = xc
        m["keymask"] = km
        m["mem"] = mem[b]
        in_maps.append(m)
    res = run_bass_kernel_spmd(nc, in_maps, core_ids=list(range(8)))
    outp = np.empty((2, 4096, 2048), np.float32)
    for k in range(8):
        b, j = k // 4, k % 4
        outp[b, j * 1024:(j + 1) * 1024] = res.results[k]["out"]
    return outp
```

```python
ENG = ('pe', 'act', 'dve', 'pool', 'sp')


class Tile:
    def __init__(self, prog, name):
        self.name = name
        self.w = {}
        self.r = {}
        self.dsem = None
        prog.tiles.append(self)


class Prog:
    def __init__(self, nc, block, stack):
        self.nc = nc
        self.block = block
        self.stack = stack
        self.bfn = {'pe': block.tensor, 'act': block.scalar, 'dve': block.vector, 'pool': block.gpsimd, 'sp': block.sync}
        self.sem = {e: stack.enter_context(nc.semaphore('s_' + e)) for e in ENG}
        self.ops = {e: [] for e in ENG}
        self.base = {e: 0 for e in ENG}
        self.waited = {e: {} for e in ENG}
        self.tiles = []
        self.dsems = []
        self.nsem = 0
        self.n_instr = 0

    def tile(self, name):
        return Tile(self, name)

    def tiles_n(self, name, n):
        return [Tile(self, '%s%d' % (name, i)) for i in range(n)]

    def _dsem(self, t):
        if t.dsem is None:
            s = self.stack.enter_context(self.nc.semaphore('d%d' % self.nsem))
            self.nsem += 1
            t.dsem = [s, 0]
            self.dsems.append(t.dsem)
        return t.dsem

    @staticmethod
    def _key(d):
        return d[1] if d[0] == 'e' else id(d[1])

    def _collect(self, eng, reads, writes):
        waits = self._collect0(eng, reads, writes)
        cur = {id(d[0]): d[1] for d in self.dsems}
        return [d if d[0] == 'e' else ('d', d[1], cur[id(d[1])]) for d in waits]

    def _collect0(self, eng, reads, writes):
        waits = []
        for t in reads:
            for k, d in t.w.items():
                if k == eng and eng == 'pe':
                    continue
                waits.append(d)
        for t in writes:
            for k, d in t.w.items():
                if k == eng and eng == 'pe':
                    continue
                waits.append(d)
            for k, d in t.r.items():
                if k == eng and eng == 'pe':
                    continue
                waits.append(d)
        return waits

    def op(self, eng, fn, reads=(), writes=(), inc=None):
        if inc is None:
            inc = (eng != 'pe')
        idx = len(self.ops[eng])
        waits = self._collect(eng, reads, writes)
        me = ('e', eng, idx)
        self.ops[eng].append({'fn': fn, 'waits': waits, 'inc': inc, 'dma': None})
        for t in reads:
            t.r[eng] = me
        for t in writes:
            t.w = {eng: me}
            t.r = {}
        return me

    def dma(self, eng, fn, reads=(), writes=(), part=False):
        assert len(writes) == 1
        wt = writes[0]
        waits = self._collect('dma', reads, () if part else writes)
        ds = self._dsem(wt)
        ds[1] += 16
        me = ('d', ds[0], ds[1])
        self.ops[eng].append({'fn': fn, 'waits': waits, 'inc': False, 'dma': ds[0]})
        for t in reads:
            t.r[id(ds[0])] = me
        wt.w = {id(ds[0]): me}
        wt.r = {}
        return me

    def _resolve_prepare(self):
        for e in ENG:
            for o in self.ops[e]:
                for d in o['waits']:
                    if d[0] == 'e':
                        lst = self.ops[d[1]]
                        found = False
                        for j in range(d[2], len(lst)):
                            if lst[j]['inc']:
                                found = True
                                break
                        if not found:
                            j = len(lst) - 1
                            while j >= d[2] and (lst[j]['dma'] is not None or lst[j]['fn'] is None):
                                j -= 1
                            assert j >= d[2], "no compute op to carry inc"
                            lst[j]['inc'] = True

    def flush(self, barrier=True):
        if barrier:
            self._add_barrier()
        self._resolve_prepare()
        val = {}
        for e in ENG:
            c = self.base[e]
            arr = []
            for o in self.ops[e]:
                if o['inc']:
                    c += 1
                arr.append(c)
            need = [None] * len(arr)
            nxt = None
            for j in range(len(arr) - 1, -1, -1):
                if self.ops[e][j]['inc']:
                    nxt = arr[j]
                need[j] = nxt
            val[e] = need
        prog = self
        for e in ENG:
            ops = self.ops[e]
            if not ops:
                continue

            def body(engine, ops=ops, waited=self.waited[e], semh=self.sem[e]):
                for o in ops:
                    for d in o['waits']:
                        if d[0] == 'e':
                            s = prog.sem[d[1]]
                            v = val[d[1]][d[2]]
                            assert v is not None
                        else:
                            s, v = d[1], d[2]
                        k = id(s)
                        if waited.get(k, 0) >= v:
                            continue
                        waited[k] = v
                        engine.wait_ge(s, v)
                        prog.n_instr += 1
                    if o['fn'] is None:
                        continue
                    ins = o['fn'](engine)
                    prog.n_instr += 1
                    if o['dma'] is not None:
                        ins.then_inc(o['dma'], 16)
                    elif o['inc']:
                        ins.then_inc(semh, 1)
            self.bfn[e](body)
        for e in ENG:
            self.base[e] += sum(1 for o in self.ops[e] if o['inc'])
            self.ops[e] = []
        if barrier:
            for t in self.tiles:
                t.w = {}
                t.r = {}

    def _add_barrier(self):
        lasts = []
        for e in ENG:
            lst = self.ops[e]
            j = len(lst) - 1
            while j >= 0 and (lst[j]['dma'] is not None or lst[j]['fn'] is None):
                j -= 1
            if j >= 0:
                lst[j]['inc'] = True
                lasts.append(('e', e, j))
        dm = [('d', s[0], s[1]) for s in self.dsems if s[1] > 0]
        for e in ENG:
            self.ops[e].append({'fn': None, 'waits': [d for d in lasts if d[1] != e] + dm, 'inc': False, 'dma': None})


import numpy as np
from contextlib import ExitStack
import concourse.bass as bass
import concourse.mybir as mybir
from concourse.bass_utils import run_bass_kernel_spmd

F32 = mybir.dt.float32
BF16 = mybir.dt.bfloat16
AF = mybir.ActivationFunctionType
ALU = mybir.AluOpType
NEG = -1.0e30
EPS = 1e-6


def build(stage=9, skip=()):
    nc = bass.Bass("TRN2", target_bir_lowering=False)

    def DI(name, shape, dt=F32):
        return nc.dram_tensor(name, shape, dt, kind="ExternalInput")
    x = DI("x", [1536, 2048]); keymask = DI("keymask", [128, 12]); memx = DI("mem", [256, 2048])
    gvecs = DI("gvecs", [5, 2048])
    w_in = DI("w_in", [2048, 9216]); lng = DI("sg_ln_g", [1024]); lnb = DI("sg_ln_b", [1024])
    wsT = DI("sg_wsT", [128, 4, 128]); bsT = DI("sg_bsT", [128, 4]); BTd = DI("att_bt", [128, 8, 5, 128])
    w_up_a = DI("w_up_a", [1024, 2048]); w_up_b = DI("w_up_b", [1024, 2048]); w_out = DI("w_out", [2048, 2048])
    mem_w_q = DI("mem_w_q", [2048, 2048]); mem_w_kv = DI("mem_w_kv", [2048, 4096]); mem_w_o = DI("mem_w_o", [2048, 2048])
    peer_w_q = DI("peer_w_q", [2048, 2048]); keysTd = DI("peer_keysT", [128, 2, 128])
    pu = DI("peer_u", [16384, 2048]); pv = DI("peer_v", [16384, 2048])
    identd = DI("ident", [128, 128]); sel32d = DI("sel32", [128, 32])
    out = nc.dram_tensor("out", [1024, 2048], F32, kind="ExternalOutput")
    Gd = nc.dram_tensor("g_scratch", [1024, 16384], BF16, kind="Internal")

    def wpanel(w, ncols_total, kchunks, c0, ncols):
        return bass.AP(w, c0, [[ncols_total, 128], [128 * ncols_total, kchunks], [1, ncols]])

    KB = 1024
    with ExitStack() as G:
        block = G.enter_context(nc.Block())
        P = Prog(nc, block, G)
        base0 = (nc.sbuf_base + 63) // 64 * 64
        ARENA = 207 * KB
        G.enter_context(nc.sbuf_tensor("arena", [128, ARENA + 64], mybir.dt.uint8))
        uid = [0]

        class Region:
            def __init__(self, start, end):
                self.start, self.end, self.p = start, end, start

            def reset(self):
                self.p = self.start

            def alloc(self, name, shape, dt=F32):
                n = 4 if dt == F32 else 2
                for d in shape[1:]:
                    n *= d
                n = (n + 63) // 64 * 64
                assert self.p + n <= self.end, "region overflow %s need %d have %d" % (name, n, self.end - self.p)
                uid[0] += 1
                t = nc.alloc_sbuf_tensor_at("%s_%d" % (name, uid[0]), shape, dt, offset=base0 + self.p)
                self.p += n
                return t

        PS = lambda st, name, shape, dt=F32: st.enter_context(nc.psum_tensor(name, shape, dt))
        RG = Region(0, 11 * KB)
        RH = Region(11 * KB, 75 * KB)
        R1 = Region(75 * KB, 123 * KB)
        R2 = Region(123 * KB, 155 * KB)
        R3 = Region(155 * KB, 187 * KB)
        R4 = Region(187 * KB, 207 * KB)

        h = RH.alloc("h", [128, 8, 2048]); T_h = P.tiles_n("h", 8)
        identf = RG.alloc("identf", [128, 128]); identb = RG.alloc("identb", [128, 128], BF16)
        onesb = RG.alloc("onesb", [128, 128], BF16); sel32b = RG.alloc("sel32b", [128, 32], BF16)
        gb = RG.alloc("gb", [128, 2048]); T_gb = P.tile("gb")
        T_c = P.tile("consts"); T_c2 = P.tile("consts2"); T_c3 = P.tile("consts3"); T_c4 = P.tile("consts4")
        P.dma('sp', lambda e: e.dma_start(out=identf[:], in_=identd.ap()), writes=[T_c])
        P.dma('pool', lambda e: e.dma_start(out=identb[:], in_=identd.ap()), writes=[T_c2])
        P.dma('pool', lambda e: e.dma_start(out=sel32b[:], in_=sel32d.ap()), writes=[T_c3])
        P.op('dve', lambda e: e.memset(onesb[:], 1.0), writes=[T_c4])
        P.flush()

        def load_gb(i):
            P.dma('sp', lambda e: e.dma_start(out=gb[:], in_=bass.AP(gvecs, i * 2048, [[0, 128], [1, 2048]])), writes=[T_gb])

        def rmsnorm_T(st, R, src_fn, ntiles, dstT, T_dst, col0):
            junk = R.alloc("junk", [128, 2048], BF16); T_j = P.tile("junk")
            stat = R.alloc("stat", [128, 3 * ntiles]); T_st = P.tiles_n("st", ntiles)
            xs = [R.alloc("xs", [128, 2048], BF16) for i in range(2)]; T_xs = P.tiles_n("xs", 2)
            tp = [PS(st, "tp%d_%d" % (i, uid[0]), [128, 8, 128], BF16) for i in range(2)]; T_tp = P.tiles_n("tp", 2)
            for tt in range(ntiles):
                s = tt % 2
                src, Ts = src_fn(tt)
                c = 3 * tt
                P.op('act', lambda e, src=src, c=c: e.activation(out=junk[:], in_=src, func=AF.Square, accum_out=stat[:, c:c + 1]),
                     reads=[Ts], writes=[T_j, T_st[tt]])
                P.op('act', lambda e, c=c: e.activation(out=stat[:, c + 1:c + 2], in_=stat[:, c:c + 1], func=AF.Sqrt, scale=1.0 / 2048, bias=EPS),
                     reads=[T_st[tt]], writes=[T_st[tt]])
                P.op('dve', lambda e, c=c: e.reciprocal(stat[:, c + 2:c + 3], stat[:, c + 1:c + 2]), reads=[T_st[tt]], writes=[T_st[tt]])
                P.op('dve', lambda e, src=src, c=c, s=s: e.scalar_tensor_tensor(out=xs[s][:], in0=src, scalar=stat[:, c + 2:c + 3], in1=gb[:],
                                                                               op0=ALU.mult, op1=ALU.mult),
                     reads=[Ts, T_st[tt], T_gb], writes=[T_xs[s]])
                for hf in range(2):
                    for k in range(8):
                        dc = hf * 8 + k
                        P.op('pe', lambda e, s=s, hf=hf, k=k, dc=dc: e.transpose(out=tp[hf][:, k, :], in_=xs[s][:, dc * 128:(dc + 1) * 128], identity=identb[:]),
                             reads=[T_xs[s], T_c2], writes=[T_tp[hf]], inc=(k == 7))
                    c0 = col0 + tt * 128
                    if hf == 0:
                        P.op('act', lambda e, hf=hf, c0=c0: e.activation(out=dstT[:, hf * 8:(hf + 1) * 8, c0:c0 + 128], in_=tp[hf][:], func=AF.Copy),
                             reads=[T_tp[hf]], writes=[T_dst])
                    else:
                        P.op('dve', lambda e, hf=hf, c0=c0: e.tensor_copy(dstT[:, hf * 8:(hf + 1) * 8, c0:c0 + 128], tp[hf][:]),
                             reads=[T_tp[hf]], writes=[T_dst])

        def proj_feat(st, R, w, wcols, kch, c0, nout_chunks, srcT, T_src, tok0, ntok, evac):
            npan = (nout_chunks + 3) // 4
            wp = [R.alloc("wp", [128, kch, 512], BF16) for i in range(2)]; T_wp = P.tiles_n("wp", 2)
            ps = [PS(st, "pp%d_%d" % (i, uid[0]), [128, 512]) for i in range(2)]; T_ps = P.tiles_n("pp", 2)
            cnt = 0
            for pn in range(npan):
                s = pn % 2
                ncl = min(4, nout_chunks - pn * 4)
                P.dma('pool', lambda e, s=s, pn=pn, ncl=ncl: e.dma_start(out=wp[s][:, :, 0:ncl * 128], in_=wpanel(w, wcols, kch, c0 + pn * 512, ncl * 128)),
                      writes=[T_wp[s]])
                for ocl in range(ncl):
                    oc = pn * 4 + ocl
                    t0 = 0
                    while t0 < ntok:
                        n = min(512, ntok - t0)
                        b = cnt % 2
                        cnt += 1
                        for kc in range(kch):
                            P.op('pe', lambda e, s=s, ocl=ocl, kc=kc, b=b, t0=t0, n=n: e.matmul(ps[b][:, 0:n], lhsT=wp[s][:, kc, ocl * 128:(ocl + 1) * 128],
                                                                                           rhs=srcT[:, kc, tok0 + t0:tok0 + t0 + n], start=(kc == 0), stop=(kc == kch - 1)),
                                 reads=[T_wp[s], T_src], writes=[T_ps[b]], inc=(kc == kch - 1))
                        evac(oc, t0, n, ps[b], T_ps[b], cnt)
                        t0 += n

        def proj_tok(st, R, w, wcols, kch, c0, npan, srcT, T_src, tok0, ntiles, evac):
            wp = [R.alloc("wq", [128, kch, 512], BF16) for i in range(2)]; T_wp = P.tiles_n("wq", 2)
            ps = [PS(st, "pq%d_%d" % (i, uid[0]), [128, 512]) for i in range(2)]; T_ps = P.tiles_n("pq", 2)
            cnt = 0
            for pn in range(npan):
                s = pn % 2
                P.dma('pool', lambda e, s=s, pn=pn: e.dma_start(out=wp[s][:], in_=wpanel(w, wcols, kch, c0 + pn * 512, 512)), writes=[T_wp[s]])
                for tt in range(ntiles):
                    b = cnt % 2
                    cnt += 1
                    for kc in range(kch):
                        P.op('pe', lambda e, s=s, kc=kc, b=b, tt=tt: e.matmul(ps[b][:], lhsT=srcT[:, kc, tok0 + tt * 128:tok0 + (tt + 1) * 128], rhs=wp[s][:, kc, :],
                                                                        start=(kc == 0), stop=(kc == kch - 1)),
                             reads=[T_wp[s], T_src], writes=[T_ps[b]], inc=(kc == kch - 1))
                    evac(pn, tt, ps[b], T_ps[b])

        def add_to_h(pn, tt, ps, T_ps):
            P.op('dve', lambda e, pn=pn, tt=tt, ps=ps: e.tensor_tensor(out=h[:, tt, pn * 512:(pn + 1) * 512], in0=h[:, tt, pn * 512:(pn + 1) * 512], in1=ps[:], op=ALU.add),
                 reads=[T_ps, T_h[tt]], writes=[T_h[tt]])

        def copy_evac(dst_fn, T_dst):
            def ev(oc, t0, n, ps, T_ps, cnt):
                if cnt % 2 == 0:
                    P.op('act', lambda e, oc=oc, t0=t0, n=n, ps=ps: e.activation(out=dst_fn(oc, t0, n), in_=ps[:, 0:n], func=AF.Copy), reads=[T_ps], writes=[T_dst])
                else:
                    P.op('dve', lambda e, oc=oc, t0=t0, n=n, ps=ps: e.tensor_copy(dst_fn(oc, t0, n), ps[:, 0:n]), reads=[T_ps], writes=[T_dst])
            return ev

        def load_x_into_h():
            for tt in range(8):
                P.dma('sp', lambda e, tt=tt: e.dma_start(out=h[:, tt, :], in_=x.ap()[512 + tt * 128:512 + (tt + 1) * 128, :]), writes=[T_h[tt]])

        if stage >= 1 and 1 not in skip:
            for r in (R1, R2, R3, R4):
                r.reset()
            aT = R1.alloc("aT", [128, 16, 1536], BF16); T_aT = P.tile("aT")
            yaT = R2.alloc("yaT", [128, 8, 1024], BF16); T_yaT = P.tile("yaT")
            ybT = R2.alloc("ybT", [128, 8, 1024], BF16); T_ybT = P.tile("ybT")
            mT = R3.alloc("mT", [128, 16, 1024], BF16); T_mT = P.tile("mT")
            RS = Region(11 * KB, 75 * KB)
            with ExitStack() as S:
                RS.reset(); R4.reset()
                load_gb(0)
                xt = [RS.alloc("xt", [128, 2048]) for i in range(2)]; T_xt = P.tiles_n("xt", 2)

                def src_fn(tt):
                    s = tt % 2
                    P.dma('sp', lambda e, tt=tt, s=s: e.dma_start(out=xt[s][:], in_=x.ap()[tt * 128:(tt + 1) * 128, :]), writes=[T_xt[s]])
                    return xt[s][:], T_xt[s]
                rmsnorm_T(S, RS, src_fn, 12, aT, T_aT, 0)
                P.flush()
            with ExitStack() as S:
                RS.reset(); R4.reset()
                RB = Region(155 * KB, 187 * KB)
                gv = RS.alloc("gv", [128, 8, 1024]); T_gv = P.tiles_n("gv", 8)
                gu = RS.alloc("gu", [128, 8, 1024], BF16); T_gu = P.tiles_n("gu", 8)
                lngb = RS.alloc("lngb", [128, 1024]); lnbb = RS.alloc("lnbb", [128, 1024])
                T_ln = P.tile("ln"); T_ln2 = P.tile("ln2")
                wsf = RS.alloc("wsf", [128, 4, 128]); wsb = RS.alloc("wsb", [128, 4, 128], BF16); T_ws = P.tile("ws")
                bs = RS.alloc("bs", [128, 4]); T_bs = P.tile("bs")
                P.dma('sp', lambda e: e.dma_start(out=lngb[:], in_=bass.AP(lng, 0, [[0, 128], [1, 1024]])), writes=[T_ln])
                P.dma('sp', lambda e: e.dma_start(out=lnbb[:], in_=bass.AP(lnb, 0, [[0, 128], [1, 1024]])), writes=[T_ln2])
                P.dma('sp', lambda e: e.dma_start(out=wsf[:], in_=wsT.ap()), writes=[T_ws])
                P.dma('sp', lambda e: e.dma_start(out=bs[:], in_=bsT.ap()), writes=[T_bs])
                P.op('dve', lambda e: e.memset(wsf[64:128, :, 0:64], 0.0), reads=[T_ws], writes=[T_ws])
                P.op('dve', lambda e: e.tensor_copy(wsb[:], wsf[:]), reads=[T_ws], writes=[T_ws])

                def evac_uv(pn, tt, ps, T_ps):
                    if pn < 2:
                        P.op('act', lambda e, pn=pn, tt=tt, ps=ps: e.activation(out=gu[:, tt, pn * 512:(pn + 1) * 512], in_=ps[:], func=AF.Gelu),
                             reads=[T_ps], writes=[T_gu[tt]])
                    else:
                        P.op('act', lambda e, pn=pn, tt=tt, ps=ps: e.activation(out=gv[:, tt, (pn - 2) * 512:(pn - 1) * 512], in_=ps[:], func=AF.Gelu),
                             reads=[T_ps], writes=[T_gv[tt]])
                proj_tok(S, RB, w_in, 9216, 16, 0, 4, aT, T_aT, 512, 8, evac_uv)
                stt_ = R4.alloc("lnstat", [128, 8, 8]); T_lst = P.tiles_n("lnst", 8)
                tmp = R4.alloc("lnt", [128, 1024]); T_tmp = P.tile("lnt")
                tmp2 = R4.alloc("lnu", [128, 1024]); T_tmp2 = P.tile("lnu")
                junk2 = R4.alloc("junk2", [128, 1024], BF16); T_j2 = P.tile("junk2")
                vln = [R4.alloc("vln", [128, 1024], BF16) for i in range(2)]; T_vln = P.tiles_n("vln", 2)
                ya = [R4.alloc("ya", [128, 1024], BF16) for i in range(2)]; T_ya = P.tiles_n("ya", 2)
                sp_ = [PS(S, "sgp%d" % i, [128, 1024]) for i in range(2)]; T_sp = P.tiles_n("sgp", 2)
                ytp = PS(S, "ytp", [128, 8, 128], BF16); T_ytp = P.tile("ytp")
                for tt in range(8):
                    s = tt % 2
                    P.op('dve', lambda e, tt=tt: e.tensor_reduce(out=stt_[:, tt, 0:1], in_=gv[:, tt, :], axis=mybir.AxisListType.X, op=ALU.add),
                         reads=[T_gv[tt]], writes=[T_lst[tt]])
                    P.op('dve', lambda e, tt=tt: e.tensor_scalar(out=stt_[:, tt, 1:2], in0=stt_[:, tt, 0:1], scalar1=1.0 / 1024, scalar2=None, op0=ALU.mult),
                         reads=[T_lst[tt]], writes=[T_lst[tt]])
                    P.op('dve', lambda e, tt=tt: e.tensor_scalar(out=tmp[:], in0=gv[:, tt, :], scalar1=stt_[:, tt, 1:2], scalar2=None, op0=ALU.subtract),
                         reads=[T_gv[tt], T_lst[tt]], writes=[T_tmp])
                    P.op('act', lambda e, tt=tt: e.activation(out=junk2[:], in_=tmp[:], func=AF.Square, accum_out=stt_[:, tt, 2:3]),
                         reads=[T_tmp], writes=[T_j2, T_lst[tt]])
                    P.op('act', lambda e, tt=tt: e.activation(out=stt_[:, tt, 3:4], in_=stt_[:, tt, 2:3], func=AF.Sqrt, scale=1.0 / 1024, bias=EPS),
                         reads=[T_lst[tt]], writes=[T_lst[tt]])
                    P.op('dve', lambda e, tt=tt: e.reciprocal(stt_[:, tt, 4:5], stt_[:, tt, 3:4]), reads=[T_lst[tt]], writes=[T_lst[tt]])
                    P.op('dve', lambda e, tt=tt: e.scalar_tensor_tensor(out=tmp2[:], in0=tmp[:], scalar=stt_[:, tt, 4:5], in1=lngb[:], op0=ALU.mult, op1=ALU.mult),
                         reads=[T_tmp, T_lst[tt], T_ln], writes=[T_tmp2])
                    P.op('dve', lambda e, s=s: e.tensor_tensor(out=vln[s][:], in0=tmp2[:], in1=lnbb[:], op=ALU.add),
                         reads=[T_tmp2, T_ln2], writes=[T_vln[s]])
                    for g in range(4):
                        P.op('pe', lambda e, s=s, g=g: e.matmul(sp_[s][:, g * 256:(g + 1) * 256], lhsT=wsb[:, g, :], rhs=vln[s][:, g * 256:(g + 1) * 256], start=True, stop=True),
                             reads=[T_vln[s], T_ws], writes=[T_sp[s]], inc=(g == 3))
                    for g in range(4):
                        P.op('dve', lambda e, s=s, g=g, tt=tt: e.scalar_tensor_tensor(out=ya[s][:, g * 256:(g + 1) * 256], in0=sp_[s][:, g * 256:(g + 1) * 256], scalar=bs[:, g:g + 1],
                                                                               in1=gu[:, tt, g * 256:(g + 1) * 256], op0=ALU.add, op1=ALU.mult),
                             reads=[T_sp[s], T_bs, T_gu[tt]], writes=[T_ya[s]])
                    for k in range(8):
                        P.op('pe', lambda e, s=s, k=k: e.transpose(out=ytp[:, k, :], in_=ya[s][:, k * 128:(k + 1) * 128], identity=identb[:]),
                             reads=[T_ya[s], T_c2], writes=[T_ytp], inc=(k == 7))
                    P.op('act', lambda e, tt=tt: e.activation(out=yaT[:, :, tt * 128:(tt + 1) * 128], in_=ytp[:], func=AF.Copy), reads=[T_ytp], writes=[T_yaT])
                P.flush()
            with ExitStack() as S:
                RS.reset(); R4.reset()
                wq = [RS.alloc("wqkv", [128, 3, 16, 128], BF16) for i in range(2)]; T_wq = P.tiles_n("wqkv", 2)
                bt = [RS.alloc("bt", [128, 5, 128]) for i in range(2)]; T_bt = P.tiles_n("bt", 2)
                km = RS.alloc("km", [128, 12]); T_km = P.tile("km")
                P.dma('sp', lambda e: e.dma_start(out=km[:], in_=keymask.ap()), writes=[T_km])
                qT = [RS.alloc("qT", [128, 1024], BF16) for i in range(2)]; T_qT = P.tiles_n("qT", 2)
                kT = [RS.alloc("kT", [128, 1536], BF16) for i in range(2)]; T_kT = P.tiles_n("kT", 2)
                vh = [RS.alloc("vh", [128, 12, 128], BF16) for i in range(2)]; T_vh = P.tiles_n("vh", 2)
                pT = [RS.alloc("pT", [128, 5, 128], BF16) for i in range(2)]; T_pT = P.tiles_n("pT", 2)
                rr = [RS.alloc("rr", [128, 128]) for i in range(2)]; T_rr = P.tiles_n("rr", 2)
                pj = [PS(S, "pj%d" % i, [128, 512]) for i in range(2)]; T_pj = P.tiles_n("pj", 2)
                scA = PS(S, "scA", [128, 4, 128]); T_scA = P.tile("scA")
                scB = PS(S, "scB", [128, 4, 128]); T_scB = P.tile("scB")
                po = [PS(S, "po%d" % i, [128, 4, 128]) for i in range(2)]; T_po = P.tiles_n("po", 2)
                cnt = 0
                for hd in range(8):
                    s = hd % 2
                    for i3, cbase in enumerate((2048, 3072, 4096)):
                        P.dma('pool', lambda e, s=s, i3=i3, cbase=cbase, hd=hd: e.dma_start(out=wq[s][:, i3, :, :], in_=wpanel(w_in, 9216, 16, cbase + hd * 128, 128)),
                              writes=[T_wq[s]], part=(i3 > 0))
                    P.dma('sp', lambda e, s=s, hd=hd: e.dma_start(out=bt[s][:], in_=BTd.ap()[:, hd, :, :]), writes=[T_bt[s]])
                    for hf in range(2):
                        b = cnt % 2; cnt += 1
                        for kc in range(16):
                            P.op('pe', lambda e, s=s, kc=kc, b=b, hf=hf: e.matmul(pj[b][:], lhsT=wq[s][:, 0, kc, :], rhs=aT[:, kc, 512 + hf * 512:1024 + hf * 512], start=(kc == 0), stop=(kc == 15)),
                                 reads=[T_wq[s], T_aT], writes=[T_pj[b]], inc=(kc == 15))
                        P.op('act', lambda e, s=s, b=b, hf=hf: e.activation(out=qT[s][:, hf * 512:(hf + 1) * 512], in_=pj[b][:], func=AF.Copy, scale=128.0 ** -0.5),
                             reads=[T_pj[b]], writes=[T_qT[s]])
                    for hf in range(3):
                        b = cnt % 2; cnt += 1
                        for kc in range(16):
                            P.op('pe', lambda e, s=s, kc=kc, b=b, hf=hf: e.matmul(pj[b][:], lhsT=wq[s][:, 1, kc, :], rhs=aT[:, kc, hf * 512:(hf + 1) * 512], start=(kc == 0), stop=(kc == 15)),
                                 reads=[T_wq[s], T_aT], writes=[T_pj[b]], inc=(kc == 15))
                        P.op('dve', lambda e, s=s, b=b, hf=hf: e.tensor_copy(kT[s][:, hf * 512:(hf + 1) * 512], pj[b][:]),
                             reads=[T_pj[b]], writes=[T_kT[s]])
                    for g4 in range(3):
                        b = cnt % 2; cnt += 1
                        for kb in range(4):
                            blk = g4 * 4 + kb
                            for kc in range(16):
                                P.op('pe', lambda e, s=s, kc=kc, b=b, kb=kb, blk=blk: e.matmul(pj[b][:, kb * 128:(kb + 1) * 128], lhsT=aT[:, kc, blk * 128:(blk + 1) * 128], rhs=wq[s][:, 2, kc, :],
                                                                                     start=(kc == 0), stop=(kc == 15)),
                                     reads=[T_wq[s], T_aT], writes=[T_pj[b]], inc=(kc == 15 and kb == 3))
                        P.op('act', lambda e, s=s, b=b, g4=g4: e.activation(out=vh[s][:, g4 * 4:(g4 + 1) * 4, :], in_=pj[b][:].rearrange("p (a b) -> p a b", b=128), func=AF.Copy),
                             reads=[T_pj[b]], writes=[T_vh[s]])
                    for m in range(8):
                        u = m % 2
                        for ki in range(5):
                            dst = scA[:, ki, :] if ki < 4 else scB[:, 0, :]
                            Tsc = T_scA if ki < 4 else T_scB
                            P.op('pe', lambda e, s=s, ki=ki, m=m, dst=dst: e.matmul(dst, lhsT=kT[s][:, (m + ki) * 128:(m + ki + 1) * 128], rhs=qT[s][:, m * 128:(m + 1) * 128], start=True, stop=False),
                                 reads=[T_kT[s], T_qT[s]], writes=[Tsc], inc=False)
                            P.op('pe', lambda e, s=s, ki=ki, dst=dst: e.matmul(dst, lhsT=identf[:], rhs=bt[s][:, ki, :], start=False, stop=True),
                                 reads=[T_bt[s], T_c], writes=[Tsc], inc=True)
                            P.op('act', lambda e, u=u, ki=ki, m=m, dst=dst: e.activation(out=pT[u][:, ki, :], in_=dst, func=AF.Exp, bias=km[:, m + ki:m + ki + 1], scale=1.0),
                                 reads=[Tsc, T_km], writes=[T_pT[u]])
                        for ki in range(5):
                            P.op('pe', lambda e, s=s, u=u, ki=ki, m=m: e.matmul(po[u][:, 0, :], lhsT=vh[s][:, m + ki, :], rhs=pT[u][:, ki, :], start=(ki == 0), stop=(ki == 4)),
                                 reads=[T_vh[s], T_pT[u]], writes=[T_po[u]], inc=False)
                        for ki in range(5):
                            P.op('pe', lambda e, u=u, ki=ki: e.matmul(po[u][:, 1, :], lhsT=onesb[:], rhs=pT[u][:, ki, :], start=(ki == 0), stop=(ki == 4), skip_group_check=True),
                                 reads=[T_c4, T_pT[u]], writes=[T_po[u]], inc=(ki == 4))
                        P.op('dve', lambda e, u=u: e.reciprocal(rr[u][:], po[u][:, 1, :]), reads=[T_po[u]], writes=[T_rr[u]])
                        P.op('dve', lambda e, u=u, hd=hd, m=m: e.tensor_tensor(out=ybT[:, hd, m * 128:(m + 1) * 128], in0=po[u][:, 0, :], in1=rr[u][:], op=ALU.mult),
                             reads=[T_po[u], T_rr[u]], writes=[T_ybT])
                P.flush()
            with ExitStack() as S:
                RS.reset(); R4.reset()
                wg = [RS.alloc("wg", [128, 2, 16, 128], BF16) for i in range(2)]; T_wg = P.tiles_n("wg", 2)
                wu = [RS.alloc("wu", [128, 2, 8, 128], BF16) for i in range(2)]; T_wu = P.tiles_n("wu", 2)
                sg_ = [RS.alloc("sg", [128, 512]) for i in range(2)]; T_sg = P.tiles_n("sg", 2)
                m1 = [RS.alloc("m1", [128, 512]) for i in range(2)]; T_m1 = P.tiles_n("m1", 2)
                pg = [PS(S, "pg%d" % i, [128, 512]) for i in range(4)]; T_pg = P.tiles_n("pg", 4)
                pu_ = [PS(S, "pu%d" % i, [128, 512]) for i in range(4)]; T_pu = P.tiles_n("pu", 4)
                cnt = 0
                for jb in range(16):
                    s = jb % 2
                    P.dma('pool', lambda e, s=s, jb=jb: e.dma_start(out=wg[s][:, 0, :, :], in_=wpanel(w_in, 9216, 16, 5120 + jb * 128, 128)), writes=[T_wg[s]])
                    P.dma('pool', lambda e, s=s, jb=jb: e.dma_start(out=wg[s][:, 1, :, :], in_=wpanel(w_in, 9216, 16, 7168 + jb * 128, 128)), writes=[T_wg[s]], part=True)
                    P.dma('pool', lambda e, s=s, jb=jb: e.dma_start(out=wu[s][:, 0, :, :], in_=wpanel(w_up_a, 2048, 8, jb * 128, 128)), writes=[T_wu[s]])
                    P.dma('pool', lambda e, s=s, jb=jb: e.dma_start(out=wu[s][:, 1, :, :], in_=wpanel(w_up_b, 2048, 8, jb * 128, 128)), writes=[T_wu[s]], part=True)
                    for hf in range(2):
                        for br in range(2):
                            b = cnt % 4; cnt += 1
                            srcY, T_srcY = (yaT, T_yaT) if br == 0 else (ybT, T_ybT)
                            for kc in range(16):
                                P.op('pe', lambda e, s=s, br=br, kc=kc, b=b, hf=hf: e.matmul(pg[b][:], lhsT=wg[s][:, br, kc, :], rhs=aT[:, kc, 512 + hf * 512:1024 + hf * 512], start=(kc == 0), stop=(kc == 15)),
                                     reads=[T_wg[s], T_aT], writes=[T_pg[b]], inc=(kc == 15))
                            for kc in range(8):
                                P.op('pe', lambda e, s=s, br=br, kc=kc, b=b, hf=hf, srcY=srcY: e.matmul(pu_[b][:], lhsT=wu[s][:, br, kc, :], rhs=srcY[:, kc, hf * 512:(hf + 1) * 512], start=(kc == 0), stop=(kc == 7)),
                                     reads=[T_wu[s], T_srcY], writes=[T_pu[b]], inc=(kc == 7))
                            P.op('act', lambda e, br=br, b=b: e.activation(out=sg_[br][:], in_=pg[b][:], func=AF.Sigmoid), reads=[T_pg[b]], writes=[T_sg[br]])
                            P.op('dve', lambda e, b=b, br=br: e.tensor_tensor(out=m1[br][:], in0=sg_[br][:], in1=pu_[b][:], op=ALU.mult), reads=[T_sg[br], T_pu[b]], writes=[T_m1[br]])
                            if br == 1:
                                P.op('dve', lambda e, jb=jb, hf=hf: e.tensor_tensor(out=mT[:, jb, hf * 512:(hf + 1) * 512], in0=m1[0][:], in1=m1[1][:], op=ALU.add),
                                     reads=[T_m1[0], T_m1[1]], writes=[T_mT])
                P.flush()
            with ExitStack() as S:
                R1.reset()
                load_x_into_h()
                proj_tok(S, R1, w_out, 2048, 16, 0, 4, mT, T_mT, 0, 8, add_to_h)
                P.flush()
        else:
            load_x_into_h()
            P.flush()

        if stage >= 2 and 2 not in skip:
            for r in (R1, R2, R3, R4):
                r.reset()
            a2T = R1.alloc("a2T", [128, 16, 1024], BF16); T_a2T = P.tile("a2T")
            mnT = R4.alloc("mnT", [128, 16, 256], BF16); T_mnT = P.tile("mnT")
            kmT = R4.alloc("kmT", [128, 16, 256], BF16); T_kmT = P.tile("kmT")
            RX = Region(75 * KB + 32 * KB, 123 * KB)
            vm = RX.alloc("vm", [128, 2, 2048], BF16); T_vm = P.tile("vm")
            with ExitStack() as S:
                R2.reset()
                load_gb(1)
                rmsnorm_T(S, R2, lambda tt: (h[:, tt, :], T_h[tt]), 8, a2T, T_a2T, 0)
                P.flush()
            with ExitStack() as S:
                R2.reset()
                load_gb(2)
                mt = [R2.alloc("mt", [128, 2048]) for i in range(2)]; T_mt = P.tiles_n("mt", 2)

                def src_m(tt):
                    P.dma('sp', lambda e, tt=tt: e.dma_start(out=mt[tt][:], in_=memx.ap()[tt * 128:(tt + 1) * 128, :]), writes=[T_mt[tt]])
                    return mt[tt][:], T_mt[tt]
                R3.reset()
                rmsnorm_T(S, R3, src_m, 2, mnT, T_mnT, 0)
                P.flush()
            with ExitStack() as S:
                R3.reset()
                proj_feat(S, R3, mem_w_kv, 4096, 16, 0, 16, mnT, T_mnT, 0, 256, copy_evac(lambda oc, t0, n: kmT[:, oc, t0:t0 + n], T_kmT))
                P.flush()
            with ExitStack() as S:
                R3.reset()

                def ev_vm(pn, tt, ps, T_ps):
                    P.op('act', lambda e, pn=pn, tt=tt, ps=ps: e.activation(out=vm[:, tt, pn * 512:(pn + 1) * 512], in_=ps[:], func=AF.Copy), reads=[T_ps], writes=[T_vm])
                proj_tok(S, R3, mem_w_kv, 4096, 16, 2048, 4, mnT, T_mnT, 0, 2, ev_vm)
                P.flush()
            R2.reset()
            q2T = R2.alloc("q2T", [128, 16, 1024], BF16); T_q2T = P.tile("q2T")
            with ExitStack() as S:
                R3.reset()
                proj_feat(S, R3, mem_w_q, 2048, 16, 0, 16, a2T, T_a2T, 0, 1024, copy_evac(lambda oc, t0, n: q2T[:, oc, t0:t0 + n], T_q2T))
                P.flush()
            onT = a2T; T_onT = P.tile("onT")
            with ExitStack() as S:
                R3.reset()
                pT2 = [R3.alloc("pT2", [128, 2, 512], BF16) for i in range(2)]; T_pT2 = P.tiles_n("pT2", 2)
                r2 = [R3.alloc("r2", [128, 512]) for i in range(2)]; T_r2 = P.tiles_n("r2", 2)
                sc2 = [PS(S, "sc2%d" % i, [128, 512]) for i in range(2)]; T_sc2 = P.tiles_n("sc2", 2)
                rs2 = PS(S, "rs2", [128, 512]); T_rs2 = P.tile("rs2")
                o2 = [PS(S, "o2%d" % i, [128, 512]) for i in range(2)]; T_o2 = P.tiles_n("o2", 2)
                it = 0
                for hd in range(4):
                    for hf in range(2):
                        u = it % 2; it += 1
                        for mb in range(2):
                            for kc in range(4):
                                P.op('pe', lambda e, hd=hd, hf=hf, mb=mb, kc=kc: e.matmul(sc2[mb][:], lhsT=kmT[:, hd * 4 + kc, mb * 128:(mb + 1) * 128], rhs=q2T[:, hd * 4 + kc, hf * 512:(hf + 1) * 512],
                                                                                    start=(kc == 0), stop=(kc == 3)),
                                     reads=[T_kmT, T_q2T], writes=[T_sc2[mb]], inc=(kc == 3))
                            P.op('act', lambda e, u=u, mb=mb: e.activation(out=pT2[u][:, mb, :], in_=sc2[mb][:], func=AF.Exp, scale=512.0 ** -0.5),
                                 reads=[T_sc2[mb]], writes=[T_pT2[u]])
                        for mb in range(2):
                            P.op('pe', lambda e, u=u, mb=mb: e.matmul(rs2[:], lhsT=onesb[:], rhs=pT2[u][:, mb, :], start=(mb == 0), stop=(mb == 1)),
                                 reads=[T_c4, T_pT2[u]], writes=[T_rs2], inc=(mb == 1))
                        P.op('dve', lambda e, u=u: e.reciprocal(r2[u][:], rs2[:]), reads=[T_rs2], writes=[T_r2[u]])
                        for oc in range(4):
                            ob = oc % 2
                            for mb in range(2):
                                P.op('pe', lambda e, u=u, mb=mb, hd=hd, oc=oc, ob=ob: e.matmul(o2[ob][:], lhsT=vm[:, mb, hd * 512 + oc * 128:hd * 512 + (oc + 1) * 128], rhs=pT2[u][:, mb, :],
                                                                                         start=(mb == 0), stop=(mb == 1)),
                                     reads=[T_vm, T_pT2[u]], writes=[T_o2[ob]], inc=(mb == 1))
                            P.op('dve', lambda e, u=u, hd=hd, oc=oc, ob=ob, hf=hf: e.tensor_tensor(out=onT[:, hd * 4 + oc, hf * 512:(hf + 1) * 512], in0=o2[ob][:], in1=r2[u][:], op=ALU.mult),
                                 reads=[T_o2[ob], T_r2[u]], writes=[T_onT])
                P.flush()
            with ExitStack() as S:
                R2.reset()
                proj_tok(S, R2, mem_w_o, 2048, 16, 0, 4, onT, T_onT, 0, 8, add_to_h)
                P.flush()

        if stage >= 3:
            for r in (R1, R2, R3, R4):
                r.reset()
            a3T = R1.alloc("a3T", [128, 16, 1024], BF16); T_a3T = P.tile("a3T")
            RX = Region(75 * KB + 32 * KB, 123 * KB)
            qT3 = R2.alloc("qT3", [128, 32, 2, 2, 128], BF16); T_qT3 = P.tile("qT3")
            with ExitStack() as S:
                R3.reset()
                load_gb(3)
                rmsnorm_T(S, R3, lambda tt: (h[:, tt, :], T_h[tt]), 8, a3T, T_a3T, 0)
                P.flush()
            with ExitStack() as S:
                R3.reset()
                def ev_q3(oc, t0, n, ps, T_ps, cnt):
                    h_, pp = oc // 2, oc % 2
                    hh, hl = h_ // 4, h_ % 4
                    dst = bass.AP(qT3, hh * 256 + pp * 128 + hl * 32 + (t0 // 32) * 512, [[16384, 128], [512, n // 32], [1, 32]])
                    src = ps[:, 0:n].rearrange("p (a b) -> p a b", b=32)
                    if cnt % 2 == 0:
                        P.op('act', lambda e, dst=dst, src=src: e.activation(out=dst, in_=src, func=AF.Copy), reads=[T_ps], writes=[T_qT3])
                    else:
                        P.op('dve', lambda e, dst=dst, src=src: e.tensor_copy(dst, src), reads=[T_ps], writes=[T_qT3])
                proj_feat(S, R3, peer_w_q, 2048, 16, 0, 16, a3T, T_a3T, 0, 1024, ev_q3)
                P.flush()
            with ExitStack() as S:
                R3.reset(); R4.reset(); RX.reset()
                keysb = RX.alloc("keysb", [128, 2, 128], BF16); T_keys = P.tile("keys")
                P.dma('pool', lambda e: e.dma_start(out=keysb[:], in_=keysTd.ap()), writes=[T_keys])
                s_sb = [RX.alloc("s_sb", [128, 2, 128]) for i in range(8)]; T_s = P.tiles_n("s_sb", 8)
                s1p = [RX.alloc("s1p", [128, 128]) for i in range(8)]; T_s1p = P.tiles_n("s1p", 8)
                t16 = [R4.alloc("t16", [128, 2, 16]) for i in range(8)]; T_t16 = P.tiles_n("t16", 8)
                c16 = [R4.alloc("c16", [128, 16]) for i in range(8)]; T_c16 = P.tiles_n("c16", 8)
                scal = [R4.alloc("scal", [128, 8]) for i in range(8)]; T_scal = P.tiles_n("scal", 8)
                tmpk = R4.alloc("tmpk", [128, 128]); T_tmpk = P.tile("tmpk")
                cand = R4.alloc("cand", [128, 256]); T_cand = P.tile("cand")
                tmpc = R4.alloc("tmpc", [128, 256]); T_tmpc = P.tile("tmpc")
                j16 = R4.alloc("j16", [128, 16]); T_j16 = P.tile("j16")
                Dd = [R3.alloc("Dd", [128, 1024]) for i in range(4)]; T_D = P.tiles_n("Dd", 4)
                Gm = [R3.alloc("Gm", [128, 1024], BF16) for i in range(3)]; T_Gm = P.tiles_n("Gm", 3)
                Go = [R3.alloc("Go", [128, 1024], BF16) for i in range(2)]; T_Go = P.tiles_n("Go", 2)
                sps = PS(S, "sps", [128, 4, 128]); T_sps = P.tile("sps")
                gps = [PS(S, "gps0", [128, 2, 512])] * 2; T_gps = [P.tile("gps")] * 2
                T_Gd = P.tile("Gd")
                Ee = [PS(S, "Eeps%d" % i, [128, 1024]) for i in range(2)]; T_E = P.tiles_n("Ee", 2)
                for st_ in range(8):
                    tb = st_ * 128
                    for ti in range(8):
                        q, hh = ti // 2, ti % 2
                        for pp in range(2):
                            lh = qT3[:, st_ * 4 + q, hh, pp, :]
                            P.op('pe', lambda e, pp=pp, lh=lh: e.matmul(sps[:, pp, :], lhsT=lh, rhs=keysb[:, pp, :], start=True, stop=True),
                                 reads=[T_qT3, T_keys], writes=[T_sps], inc=(pp == 1))
                        P.op('act', lambda e, ti=ti: e.activation(out=s_sb[ti][:], in_=sps[:, 0:2, :], func=AF.Copy), reads=[T_sps], writes=[T_s[ti]])
                        for pp in range(2):
                            P.op('dve', lambda e, ti=ti, pp=pp: e.max(out=t16[ti][:, pp, 0:8], in_=s_sb[ti][:, pp, :]), reads=[T_s[ti]], writes=[T_t16[ti]])
                            P.op('dve', lambda e, ti=ti, pp=pp: e.match_replace(out=tmpk[:], in_to_replace=t16[ti][:, pp, 0:8], in_values=s_sb[ti][:, pp, :], imm_value=NEG),
                                 reads=[T_s[ti], T_t16[ti]], writes=[T_tmpk])
                            P.op('dve', lambda e, ti=ti, pp=pp: e.max(out=t16[ti][:, pp, 8:16], in_=tmpk[:]), reads=[T_tmpk], writes=[T_t16[ti]])
                        P.op('dve', lambda e, ti=ti: e.tensor_tensor(out=cand[:].rearrange("p (a b) -> p a b", b=16), in0=bass.AP(t16[ti], 0, [[32, 128], [1, 16], [0, 16]]),
                                                                  in1=bass.AP(t16[ti], 16, [[32, 128], [0, 16], [1, 16]]), op=ALU.add),
                             reads=[T_t16[ti]], writes=[T_cand])
                        P.op('dve', lambda e, ti=ti: e.max(out=c16[ti][:, 0:8], in_=cand[:]), reads=[T_cand], writes=[T_c16[ti]])
                        P.op('dve', lambda e, ti=ti: e.match_replace(out=tmpc[:], in_to_replace=c16[ti][:, 0:8], in_values=cand[:], imm_value=NEG),
                             reads=[T_cand, T_c16[ti]], writes=[T_tmpc])
                        P.op('dve', lambda e, ti=ti: e.max(out=c16[ti][:, 8:16], in_=tmpc[:]), reads=[T_tmpc], writes=[T_c16[ti]])
                        P.op('dve', lambda e, ti=ti: e.tensor_scalar(out=scal[ti][:, 0:1], in0=c16[ti][:, 0:1], scalar1=-1.0, scalar2=None, op0=ALU.mult),
                             reads=[T_c16[ti]], writes=[T_scal[ti]])
                        P.op('act', lambda e, ti=ti: e.activation(out=j16[:], in_=c16[ti][:], func=AF.Exp, bias=scal[ti][:, 0:1], scale=1.0, accum_out=scal[ti][:, 1:2]),
                             reads=[T_c16[ti], T_scal[ti]], writes=[T_j16, T_scal[ti]])
                        P.op('act', lambda e, ti=ti: e.activation(out=scal[ti][:, 2:3], in_=scal[ti][:, 1:2], func=AF.Ln), reads=[T_scal[ti]], writes=[T_scal[ti]])
                        P.op('dve', lambda e, ti=ti: e.scalar_tensor_tensor(out=scal[ti][:, 3:4], in0=c16[ti][:, 15:16], scalar=scal[ti][:, 0:1], in1=scal[ti][:, 2:3], op0=ALU.add, op1=ALU.subtract),
                             reads=[T_c16[ti], T_scal[ti]], writes=[T_scal[ti]])
                        P.op('dve', lambda e, ti=ti: e.tensor_scalar(out=s1p[ti][:], in0=s_sb[ti][:, 0, :], scalar1=c16[ti][:, 15:16], scalar2=None, op0=ALU.subtract),
                             reads=[T_s[ti], T_c16[ti]], writes=[T_s1p[ti]])
                    NIT = 128
                    for k in range(NIT + 2):
                        if k < NIT:
                            eb, ti = k // 8, k % 8
                            u = k % 4
                            deng = 'dve' if (k % 2 == 1) else 'pool'
                            P.op(deng, lambda e, ti=ti, eb=eb, u=u: e.tensor_tensor(out=Dd[u][:].rearrange("p (a b) -> p a b", b=128), in0=bass.AP(s1p[ti], eb * 8, [[128, 128], [1, 8], [0, 128]]),
                                                                                 in1=bass.AP(s_sb[ti], 128, [[256, 128], [0, 8], [1, 128]]), op=ALU.add),
                                 reads=[T_s1p[ti], T_s[ti]], writes=[T_D[u]])
                        k1 = k - 1
                        if 0 <= k1 < NIT:
                            ti = k1 % 8
                            P.op('act', lambda e, ti=ti, k1=k1: e.activation(out=Ee[k1 % 2][:], in_=Dd[k1 % 4][:], func=AF.Exp, bias=scal[ti][:, 3:4], scale=1.0),
                                 reads=[T_D[k1 % 4], T_scal[ti]], writes=[T_E[k1 % 2]])
                        k2 = k - 2
                        if 0 <= k2 < NIT:
                            eb, ti = k2 // 8, k2 % 8
                            q, hh = ti // 2, ti % 2
                            gsl = eb % 2
                            P.op('dve', lambda e, k2=k2: e.scalar_tensor_tensor(out=Gm[k2 % 3][:], in0=Dd[k2 % 4][:], scalar=-1e-5, in1=Ee[k2 % 2][:], op0=ALU.is_ge, op1=ALU.mult),
                                 reads=[T_D[k2 % 4], T_E[k2 % 2]], writes=[T_Gm[k2 % 3]])
                            for h2 in range(2):
                                P.op('pe', lambda e, k2=k2, h2=h2, q=q, hh=hh, gsl=gsl: e.matmul(gps[gsl][32 * q:32 * q + 32, h2, :], lhsT=sel32b[:], rhs=Gm[k2 % 3][:, h2 * 512:(h2 + 1) * 512],
                                                                                           start=(hh == 0), stop=(hh == 1), tile_position=(0, 32 * q), skip_group_check=True),
                                     reads=[T_Gm[k2 % 3], T_c3], writes=[T_gps[gsl]], inc=(h2 == 1))
                            if ti == 7:
                                P.op('act', lambda e, gsl=gsl: e.activation(out=Go[gsl][:], in_=gps[gsl][:].rearrange("p a b -> p (a b)"), func=AF.Copy), reads=[T_gps[gsl]], writes=[T_Go[gsl]])
                                P.dma('sp', lambda e, gsl=gsl, tb=tb, eb=eb: e.dma_start(out=Gd.ap()[tb:tb + 128, eb * 1024:(eb + 1) * 1024], in_=Go[gsl][:]), reads=[T_Go[gsl]], writes=[T_Gd], part=True)
                P.flush()
            with ExitStack() as S:
                R2.reset(); R3.reset(); R4.reset(); RX.reset()
                RP = Region(123 * KB, 207 * KB)
                GI = 4
                ust = [RP.alloc("ust", [128, 2048], BF16) for i in range(2)]; T_ust = P.tiles_n("ust", 2)
                UT = [RP.alloc("UT", [128, 16, 128], BF16) for i in range(2)]; T_UT = P.tiles_n("UT", 2)
                vg = [RP.alloc("vg", [128, GI, 2048], BF16) for i in range(2)]; T_vg = P.tiles_n("vg", 2)
                gg = [RP.alloc("gg", [128, 8, GI * 128], BF16) for i in range(2)]; T_gg = P.tiles_n("gg", 2)
                actT = [RP.alloc("actT", [128, GI, 1024], BF16) for i in range(2)]; T_actT = P.tiles_n("actT", 2)
                gl = [RP.alloc("gl", [128, 512], BF16) for i in range(2)]; T_gl = P.tiles_n("gl", 2)
                utp = PS(S, "utp", [128, 8, 128], BF16); T_utp = P.tile("utp")
                gtp = PS(S, "gtp", [128, 8, 128], BF16); T_gtp = P.tile("gtp")
                pps = [PS(S, "pps%d" % i, [128, 512]) for i in range(2)]; T_pps = P.tiles_n("pps", 2)
                ops_ = [PS(S, "ops%d" % i, [128, 2, 512]) for i in range(2)]; T_ops = P.tiles_n("ops", 2)
                ci = 0
                oi = 0
                for g in range(16384 // (128 * GI)):
                    gs = g % 2
                    P.dma('pool', lambda e, g=g, gs=gs: e.dma_start(out=vg[gs][:], in_=bass.AP(pv, g * GI * 128 * 2048, [[2048, 128], [128 * 2048, GI], [1, 2048]])), writes=[T_vg[gs]])
                    P.dma('sp', lambda e, g=g, gs=gs: e.dma_start(out=gg[gs][:], in_=bass.AP(Gd, g * GI * 128, [[16384, 128], [128 * 16384, 8], [1, GI * 128]])), writes=[T_gg[gs]])
                    for c in range(GI):
                        us = ci % 2; ci += 1
                        ch = g * GI + c
                        P.dma('pool', lambda e, ch=ch, us=us: e.dma_start(out=ust[us][:], in_=pu.ap()[ch * 128:(ch + 1) * 128, :]), writes=[T_ust[us]])
                        for hf in range(2):
                            for k in range(8):
                                dc = hf * 8 + k
                                P.op('pe', lambda e, us=us, k=k, dc=dc: e.transpose(out=utp[:, k, :], in_=ust[us][:, dc * 128:(dc + 1) * 128], identity=identb[:]),
                                     reads=[T_ust[us], T_c2], writes=[T_utp], inc=(k == 7))
                            if hf == 0:
                                P.op('act', lambda e, us=us: e.activation(out=UT[us][:, 0:8, :], in_=utp[:], func=AF.Copy), reads=[T_utp], writes=[T_UT[us]])
                            else:
                                P.op('dve', lambda e, us=us: e.tensor_copy(UT[us][:, 8:16, :], utp[:]), reads=[T_utp], writes=[T_UT[us]])
                        for tt in range(8):
                            P.op('pe', lambda e, gs=gs, tt=tt, c=c: e.transpose(out=gtp[:, tt, :], in_=gg[gs][:, tt, c * 128:(c + 1) * 128], identity=identb[:]),
                                 reads=[T_gg[gs], T_c2], writes=[T_gtp], inc=(tt == 7))
                        for hf in range(2):
                            for dc in range(16):
                                P.op('pe', lambda e, us=us, dc=dc, hf=hf: e.matmul(pps[hf][:], lhsT=UT[us][:, dc, :], rhs=a3T[:, dc, hf * 512:(hf + 1) * 512], start=(dc == 0), stop=(dc == 15)),
                                     reads=[T_UT[us], T_a3T], writes=[T_pps[hf]], inc=(dc == 15))
                            P.op('act', lambda e, hf=hf: e.activation(out=gl[hf][:], in_=pps[hf][:], func=AF.Gelu), reads=[T_pps[hf]], writes=[T_gl[hf]])
                            P.op('dve', lambda e, hf=hf, gs=gs, c=c: e.tensor_tensor(out=actT[gs][:, c, hf * 512:(hf + 1) * 512].rearrange("p (a b) -> p a b", b=128),
                                                                                  in0=gl[hf][:].rearrange("p (a b) -> p a b", b=128), in1=gtp[:, hf * 4:(hf + 1) * 4, :], op=ALU.mult),
                                 reads=[T_gl[hf], T_gtp], writes=[T_actT[gs]])
                    for tt in range(8):
                        for dh in range(2):
                            ob = oi % 2; oi += 1
                            for d2 in range(2):
                                db = dh * 2 + d2
                                for c in range(GI):
                                    P.op('pe', lambda e, gs=gs, c=c, tt=tt, db=db, d2=d2, ob=ob: e.matmul(ops_[ob][:, d2, :], lhsT=actT[gs][:, c, tt * 128:(tt + 1) * 128], rhs=vg[gs][:, c, db * 512:(db + 1) * 512],
                                                                                                    start=(c == 0), stop=(c == GI - 1)),
                                         reads=[T_actT[gs], T_vg[gs]], writes=[T_ops[ob]], inc=(c == GI - 1 and d2 == 1))
                            P.op('dve', lambda e, tt=tt, dh=dh, ob=ob: e.tensor_tensor(out=h[:, tt, dh * 1024:(dh + 1) * 1024], in0=h[:, tt, dh * 1024:(dh + 1) * 1024],
                                                                                  in1=ops_[ob][:].rearrange("p a b -> p (a b)"), op=ALU.add),
                                 reads=[T_ops[ob], T_h[tt]], writes=[T_h[tt]])
                P.flush()

        with ExitStack() as S:
            R1.reset()
            load_gb(4)
            ot = [R1.alloc("ot", [128, 2048]) for i in range(2)]; T_ot = P.tiles_n("ot", 2)
            junk = R1.alloc("junkf", [128, 2048], BF16); T_j = P.tile("junkf")
            stat = R1.alloc("statf", [128, 24]); T_st = P.tiles_n("stf", 8)
            T_out = P.tile("out")
            for tt in range(8):
                s = tt % 2
                c = 3 * tt
                P.op('act', lambda e, tt=tt, c=c: e.activation(out=junk[:], in_=h[:, tt, :], func=AF.Square, accum_out=stat[:, c:c + 1]), reads=[T_h[tt]], writes=[T_j, T_st[tt]])
                P.op('act', lambda e, c=c: e.activation(out=stat[:, c + 1:c + 2], in_=stat[:, c:c + 1], func=AF.Sqrt, scale=1.0 / 2048, bias=EPS), reads=[T_st[tt]], writes=[T_st[tt]])
                P.op('dve', lambda e, c=c: e.reciprocal(stat[:, c + 2:c + 3], stat[:, c + 1:c + 2]), reads=[T_st[tt]], writes=[T_st[tt]])
                P.op('dve', lambda e, tt=tt, c=c, s=s: e.scalar_tensor_tensor(out=ot[s][:], in0=h[:, tt, :], scalar=stat[:, c + 2:c + 3], in1=gb[:], op0=ALU.mult, op1=ALU.mult),
                     reads=[T_h[tt], T_st[tt], T_gb], writes=[T_ot[s]])
                P.dma('sp', lambda e, tt=tt, s=s: e.dma_start(out=out.ap()[tt * 128:(tt + 1) * 128, :], in_=ot[s][:]), reads=[T_ot[s]], writes=[T_out], part=True)
            P.flush()
        print("bass instructions emitted:", P.n_instr, "dma sems:", P.nsem)
    return nc


_CACHE = {}


def _host_consts(att_rel_bias):
    kj = np.arange(640)[:, None]
    qi = np.arange(128)[None, :]
    dist = 512 + qi - kj
    idx = np.clip(dist, -128, 128) + 128
    valid = (qi // 64 <= kj // 64) & (kj // 64 <= 8 + qi // 64)
    rb = att_rel_bias[0]
    bt = rb[:, idx]
    bt = np.where(valid[None], bt, np.float32(NEG)).astype(np.float32)
    bt = bt.reshape(8, 5, 128, 128).transpose(2, 0, 1, 3)
    return np.ascontiguousarray(bt)


def kernel(x, mem, g_mix, w_in, sg_ln_g, sg_ln_b, sg_w_s, sg_b_s, att_rel_bias, w_up_a, w_up_b, w_out,
           g_mem_q, g_mem_kv, mem_w_q, mem_w_kv, mem_w_o, g_ffn, peer_w_q, peer_sub_keys, peer_u, peer_v, g_final,
           _stage=9):
    f = lambda a: np.ascontiguousarray(np.asarray(a, dtype=np.float32))
    x = f(x); mem = f(mem)
    nc = build(_stage)
    shared = {
        "gvecs": f(np.stack([np.asarray(g_mix)[0], np.asarray(g_mem_q)[0], np.asarray(g_mem_kv)[0], np.asarray(g_ffn)[0], np.asarray(g_final)])),
        "w_in": f(np.asarray(w_in)[0]), "sg_ln_g": f(np.asarray(sg_ln_g)[0]), "sg_ln_b": f(np.asarray(sg_ln_b)[0]),
        "sg_wsT": f(np.asarray(sg_w_s)[0].transpose(2, 0, 1)),
        "sg_bsT": f(np.asarray(sg_b_s)[0].T),
        "att_bt": _host_consts(f(att_rel_bias)),
        "w_up_a": f(np.asarray(w_up_a)[0]), "w_up_b": f(np.asarray(w_up_b)[0]), "w_out": f(np.asarray(w_out)[0]),
        "mem_w_q": f(np.asarray(mem_w_q)[0]), "mem_w_kv": f(np.asarray(mem_w_kv)[0]), "mem_w_o": f(np.asarray(mem_w_o)[0]),
        "peer_w_q": f(np.asarray(peer_w_q)[0]),
        "peer_keysT": f(np.asarray(peer_sub_keys)[0].transpose(2, 0, 1)),
        "peer_u": f(np.asarray(peer_u)[0]), "peer_v": f(np.asarray(peer_v)[0]),
        "ident": np.eye(128, dtype=np.float32),
        "sel32": np.ascontiguousarray(np.tile(np.eye(32, dtype=np.float32), (4, 1))),
    }
    in_maps = []
    for k in range(8):
        b, j = k // 4, k % 4
        xc = np.zeros((1536, 2048), np.float32)
        xc[512:] = x[b, j * 1024:(j + 1) * 1024]
        km = np.zeros((128, 12), np.float32)
        if j == 0:
            km[:, 0:4] = NEG
        else:
            xc[:512] = x[b, j * 1024 - 512:j * 1024]
        m = dict(shared)
        m["x"] = xc
        m["keymask"] = km
        m["mem"] = mem[b]
        in_maps.append(m)
    res = run_bass_kernel_spmd(nc, in_maps, core_ids=list(range(8)))
    outp = np.empty((2, 4096, 2048), np.float32)
    for k in range(8):
        b, j = k // 4, k % 4
        outp[b, j * 1024:(j + 1) * 1024] = res.results[k]["out"]
    return outp
```

```python
ENG = ('pe', 'act', 'dve', 'pool', 'sp')


class Tile:
    def __init__(self, prog, name):
        self.name = name
        self.w = {}
        self.r = {}
        self.dsem = None
        prog.tiles.append(self)


class Prog:
    def __init__(self, nc, block, stack):
        self.nc = nc
        self.block = block
        self.stack = stack
        self.bfn = {'pe': block.tensor, 'act': block.scalar, 'dve': block.vector, 'pool': block.gpsimd, 'sp': block.sync}
        self.sem = {e: stack.enter_context(nc.semaphore('s_' + e)) for e in ENG}
        self.ops = {e: [] for e in ENG}
        self.base = {e: 0 for e in ENG}
        self.waited = {e: {} for e in ENG}
        self.tiles = []
        self.dsems = []
        self.nsem = 0
        self.n_instr = 0

    def tile(self, name):
        return Tile(self, name)

    def tiles_n(self, name, n):
        return [Tile(self, '%s%d' % (name, i)) for i in range(n)]

    def _dsem(self, t):
        if t.dsem is None:
            s = self.stack.enter_context(self.nc.semaphore('d%d' % self.nsem))
            self.nsem += 1
            t.dsem = [s, 0]
            self.dsems.append(t.dsem)
        return t.dsem

    @staticmethod
    def _key(d):
        return d[1] if d[0] == 'e' else id(d[1])

    def _collect(self, eng, reads, writes):
        waits = self._collect0(eng, reads, writes)
        cur = {id(d[0]): d[1] for d in self.dsems}
        return [d if d[0] == 'e' else ('d', d[1], cur[id(d[1])]) for d in waits]

    def _collect0(self, eng, reads, writes):
        waits = []
        for t in reads:
            for k, d in t.w.items():
                if k == eng and eng == 'pe':
                    continue
                waits.append(d)
        for t in writes:
            for k, d in t.w.items():
                if k == eng and eng == 'pe':
                    continue
                waits.append(d)
            for k, d in t.r.items():
                if k == eng and eng == 'pe':
                    continue
                waits.append(d)
        return waits

    def op(self, eng, fn, reads=(), writes=(), inc=None):
        if inc is None:
            inc = (eng != 'pe')
        idx = len(self.ops[eng])
        waits = self._collect(eng, reads, writes)
        me = ('e', eng, idx)
        self.ops[eng].append({'fn': fn, 'waits': waits, 'inc': inc, 'dma': None})
        for t in reads:
            t.r[eng] = me
        for t in writes:
            t.w = {eng: me}
            t.r = {}
        return me

    def dma(self, eng, fn, reads=(), writes=(), part=False):
        assert len(writes) == 1
        wt = writes[0]
        waits = self._collect('dma', reads, () if part else writes)
        ds = self._dsem(wt)
        ds[1] += 16
        me = ('d', ds[0], ds[1])
        self.ops[eng].append({'fn': fn, 'waits': waits, 'inc': False, 'dma': ds[0]})
        for t in reads:
            t.r[id(ds[0])] = me
        wt.w = {id(ds[0]): me}
        wt.r = {}
        return me

    def _resolve_prepare(self):
        for e in ENG:
            for o in self.ops[e]:
                for d in o['waits']:
                    if d[0] == 'e':
                        lst = self.ops[d[1]]
                        found = False
                        for j in range(d[2], len(lst)):
                            if lst[j]['inc']:
                                found = True
                                break
                        if not found:
                            j = len(lst) - 1
                            while j >= d[2] and (lst[j]['dma'] is not None or lst[j]['fn'] is None):
                                j -= 1
                            assert j >= d[2], "no compute op to carry inc"
                            lst[j]['inc'] = True

    def flush(self, barrier=True):
        if barrier:
            self._add_barrier()
        self._resolve_prepare()
        val = {}
        for e in ENG:
            c = self.base[e]
            arr = []
            for o in self.ops[e]:
                if o['inc']:
                    c += 1
                arr.append(c)
            need = [None] * len(arr)
            nxt = None
            for j in range(len(arr) - 1, -1, -1):
                if self.ops[e][j]['inc']:
                    nxt = arr[j]
                need[j] = nxt
            val[e] = need
        prog = self
        for e in ENG:
            ops = self.ops[e]
            if not ops:
                continue

            def body(engine, ops=ops, waited=self.waited[e], semh=self.sem[e]):
                for o in ops:
                    for d in o['waits']:
                        if d[0] == 'e':
                            s = prog.sem[d[1]]
                            v = val[d[1]][d[2]]
                            assert v is not None
                        else:
                            s, v = d[1], d[2]
                        k = id(s)
                        if waited.get(k, 0) >= v:
                            continue
                        waited[k] = v
                        engine.wait_ge(s, v)
                        prog.n_instr += 1
                    if o['fn'] is None:
                        continue
                    ins = o['fn'](engine)
                    prog.n_instr += 1
                    if o['dma'] is not None:
                        ins.then_inc(o['dma'], 16)
                    elif o['inc']:
                        ins.then_inc(semh, 1)
            self.bfn[e](body)
        for e in ENG:
            self.base[e] += sum(1 for o in self.ops[e] if o['inc'])
            self.ops[e] = []
        if barrier:
            for t in self.tiles:
                t.w = {}
                t.r = {}

    def _add_barrier(self):
        lasts = []
        for e in ENG:
            lst = self.ops[e]
            j = len(lst) - 1
            while j >= 0 and (lst[j]['dma'] is not None or lst[j]['fn'] is None):
                j -= 1
            if j >= 0:
                lst[j]['inc'] = True
                lasts.append(('e', e, j))
        dm = [('d', s[0], s[1]) for s in self.dsems if s[1] > 0]
        for e in ENG:
            self.ops[e].append({'fn': None, 'waits': [d for d in lasts if d[1] != e] + dm, 'inc': False, 'dma': None})


import numpy as np
from contextlib import ExitStack
import concourse.bass as bass
import concourse.mybir as mybir
from concourse.bass_utils import run_bass_kernel_spmd

F32 = mybir.dt.float32
BF16 = mybir.dt.bfloat16
AF = mybir.ActivationFunctionType
ALU = mybir.AluOpType
NEG = -1.0e30
EPS = 1e-6


def build(stage=9, skip=()):
    nc = bass.Bass("TRN2", target_bir_lowering=False)

    def DI(name, shape, dt=F32):
        return nc.dram_tensor(name, shape, dt, kind="ExternalInput")
    x = DI("x", [1536, 2048]); keymask = DI("keymask", [128, 12]); memx = DI("mem", [256, 2048])
    gvecs = DI("gvecs", [5, 2048])
    w_in = DI("w_in", [2048, 9216]); lng = DI("sg_ln_g", [1024]); lnb = DI("sg_ln_b", [1024])
    wsT = DI("sg_wsT", [128, 4, 128]); bsT = DI("sg_bsT", [128, 4]); BTd = DI("att_bt", [128, 8, 5, 128])
    w_up_a = DI("w_up_a", [1024, 2048]); w_up_b = DI("w_up_b", [1024, 2048]); w_out = DI("w_out", [2048, 2048])
    mem_w_q = DI("mem_w_q", [2048, 2048]); mem_w_kv = DI("mem_w_kv", [2048, 4096]); mem_w_o = DI("mem_w_o", [2048, 2048])
    peer_w_q = DI("peer_w_q", [2048, 2048]); keysTd = DI("peer_keysT", [128, 2, 128])
    pu = DI("peer_u", [16384, 2048]); pv = DI("peer_v", [16384, 2048])
    identd = DI("ident", [128, 128]); sel32d = DI("sel32", [128, 32])
    out = nc.dram_tensor("out", [1024, 2048], F32, kind="ExternalOutput")
    Gd = nc.dram_tensor("g_scratch", [1024, 16384], BF16, kind="Internal")

    def wpanel(w, ncols_total, kchunks, c0, ncols):
        return bass.AP(w, c0, [[ncols_total, 128], [128 * ncols_total, kchunks], [1, ncols]])

    KB = 1024
    with ExitStack() as G:
        block = G.enter_context(nc.Block())
        P = Prog(nc, block, G)
        base0 = (nc.sbuf_base + 63) // 64 * 64
        ARENA = 207 * KB
        G.enter_context(nc.sbuf_tensor("arena", [128, ARENA + 64], mybir.dt.uint8))
        uid = [0]

        class Region:
            def __init__(self, start, end):
                self.start, self.end, self.p = start, end, start

            def reset(self):
                self.p = self.start

            def alloc(self, name, shape, dt=F32):
                n = 4 if dt == F32 else 2
                for d in shape[1:]:
                    n *= d
                n = (n + 63) // 64 * 64
                assert self.p + n <= self.end, "region overflow %s need %d have %d" % (name, n, self.end - self.p)
                uid[0] += 1
                t = nc.alloc_sbuf_tensor_at("%s_%d" % (name, uid[0]), shape, dt, offset=base0 + self.p)
                self.p += n
                return t

        PS = lambda st, name, shape, dt=F32: st.enter_context(nc.psum_tensor(name, shape, dt))
        RG = Region(0, 11 * KB)
        RH = Region(11 * KB, 75 * KB)
        R1 = Region(75 * KB, 123 * KB)
        R2 = Region(123 * KB, 155 * KB)
        R3 = Region(155 * KB, 187 * KB)
        R4 = Region(187 * KB, 207 * KB)

        h = RH.alloc("h", [128, 8, 2048]); T_h = P.tiles_n("h", 8)
        identf = RG.alloc("identf", [128, 128]); identb = RG.alloc("identb", [128, 128], BF16)
        onesb = RG.alloc("onesb", [128, 128], BF16); sel32b = RG.alloc("sel32b", [128, 32], BF16)
        gb = RG.alloc("gb", [128, 2048]); T_gb = P.tile("gb")
        T_c = P.tile("consts"); T_c2 = P.tile("consts2"); T_c3 = P.tile("consts3"); T_c4 = P.tile("consts4")
        P.dma('sp', lambda e: e.dma_start(out=identf[:], in_=identd.ap()), writes=[T_c])
        P.dma('pool', lambda e: e.dma_start(out=identb[:], in_=identd.ap()), writes=[T_c2])
        P.dma('pool', lambda e: e.dma_start(out=sel32b[:], in_=sel32d.ap()), writes=[T_c3])
        P.op('dve', lambda e: e.memset(onesb[:], 1.0), writes=[T_c4])
        P.flush()

        def load_gb(i):
            P.dma('sp', lambda e: e.dma_start(out=gb[:], in_=bass.AP(gvecs, i * 2048, [[0, 128], [1, 2048]])), writes=[T_gb])

        def rmsnorm_T(st, R, src_fn, ntiles, dstT, T_dst, col0):
            junk = R.alloc("junk", [128, 2048], BF16); T_j = P.tile("junk")
            stat = R.alloc("stat", [128, 3 * ntiles]); T_st = P.tiles_n("st", ntiles)
            xs = [R.alloc("xs", [128, 2048], BF16) for i in range(2)]; T_xs = P.tiles_n("xs", 2)
            tp = [PS(st, "tp%d_%d" % (i, uid[0]), [128, 8, 128], BF16) for i in range(2)]; T_tp = P.tiles_n("tp", 2)
            for tt in range(ntiles):
                s = tt % 2
                src, Ts = src_fn(tt)
                c = 3 * tt
                P.op('act', lambda e, src=src, c=c: e.activation(out=junk[:], in_=src, func=AF.Square, accum_out=stat[:, c:c + 1]),
                     reads=[Ts], writes=[T_j, T_st[tt]])
                P.op('act', lambda e, c=c: e.activation(out=stat[:, c + 1:c + 2], in_=stat[:, c:c + 1], func=AF.Sqrt, scale=1.0 / 2048, bias=EPS),
                     reads=[T_st[tt]], writes=[T_st[tt]])
                P.op('dve', lambda e, c=c: e.reciprocal(stat[:, c + 2:c + 3], stat[:, c + 1:c + 2]), reads=[T_st[tt]], writes=[T_st[tt]])
                P.op('dve', lambda e, src=src, c=c, s=s: e.scalar_tensor_tensor(out=xs[s][:], in0=src, scalar=stat[:, c + 2:c + 3], in1=gb[:],
                                                                               op0=ALU.mult, op1=ALU.mult),
                     reads=[Ts, T_st[tt], T_gb], writes=[T_xs[s]])
                for hf in range(2):
                    for k in range(8):
                        dc = hf * 8 + k
                        P.op('pe', lambda e, s=s, hf=hf, k=k, dc=dc: e.transpose(out=tp[hf][:, k, :], in_=xs[s][:, dc * 128:(dc + 1) * 128], identity=identb[:]),
                             reads=[T_xs[s], T_c2], writes=[T_tp[hf]], inc=(k == 7))
                    c0 = col0 + tt * 128
                    if hf == 0:
                        P.op('act', lambda e, hf=hf, c0=c0: e.activation(out=dstT[:, hf * 8:(hf + 1) * 8, c0:c0 + 128], in_=tp[hf][:], func=AF.Copy),
                             reads=[T_tp[hf]], writes=[T_dst])
                    else:
                        P.op('dve', lambda e, hf=hf, c0=c0: e.tensor_copy(dstT[:, hf * 8:(hf + 1) * 8, c0:c0 + 128], tp[hf][:]),
                             reads=[T_tp[hf]], writes=[T_dst])

        def proj_feat(st, R, w, wcols, kch, c0, nout_chunks, srcT, T_src, tok0, ntok, evac):
            npan = (nout_chunks + 3) // 4
            wp = [R.alloc("wp", [128, kch, 512], BF16) for i in range(2)]; T_wp = P.tiles_n("wp", 2)
            ps = [PS(st, "pp%d_%d" % (i, uid[0]), [128, 512]) for i in range(2)]; T_ps = P.tiles_n("pp", 2)
            cnt = 0
            for pn in range(npan):
                s = pn % 2
                ncl = min(4, nout_chunks - pn * 4)
                P.dma('pool', lambda e, s=s, pn=pn, ncl=ncl: e.dma_start(out=wp[s][:, :, 0:ncl * 128], in_=wpanel(w, wcols, kch, c0 + pn * 512, ncl * 128)),
                      writes=[T_wp[s]])
                for ocl in range(ncl):
                    oc = pn * 4 + ocl
                    t0 = 0
                    while t0 < ntok:
                        n = min(512, ntok - t0)
                        b = cnt % 2
                        cnt += 1
                        for kc in range(kch):
                            P.op('pe', lambda e, s=s, ocl=ocl, kc=kc, b=b, t0=t0, n=n: e.matmul(ps[b][:, 0:n], lhsT=wp[s][:, kc, ocl * 128:(ocl + 1) * 128],
                                                                                           rhs=srcT[:, kc, tok0 + t0:tok0 + t0 + n], start=(kc == 0), stop=(kc == kch - 1)),
                                 reads=[T_wp[s], T_src], writes=[T_ps[b]], inc=(kc == kch - 1))
                        evac(oc, t0, n, ps[b], T_ps[b], cnt)
                        t0 += n

        def proj_tok(st, R, w, wcols, kch, c0, npan, srcT, T_src, tok0, ntiles, evac):
            wp = [R.alloc("wq", [128, kch, 512], BF16) for i in range(2)]; T_wp = P.tiles_n("wq", 2)
            ps = [PS(st, "pq%d_%d" % (i, uid[0]), [128, 512]) for i in range(2)]; T_ps = P.tiles_n("pq", 2)
            cnt = 0
            for pn in range(npan):
                s = pn % 2
                P.dma('pool', lambda e, s=s, pn=pn: e.dma_start(out=wp[s][:], in_=wpanel(w, wcols, kch, c0 + pn * 512, 512)), writes=[T_wp[s]])
                for tt in range(ntiles):
                    b = cnt % 2
                    cnt += 1
                    for kc in range(kch):
                        P.op('pe', lambda e, s=s, kc=kc, b=b, tt=tt: e.matmul(ps[b][:], lhsT=srcT[:, kc, tok0 + tt * 128:tok0 + (tt + 1) * 128], rhs=wp[s][:, kc, :],
                                                                        start=(kc == 0), stop=(kc == kch - 1)),
                             reads=[T_wp[s], T_src], writes=[T_ps[b]], inc=(kc == kch - 1))
                    evac(pn, tt, ps[b], T_ps[b])

        def add_to_h(pn, tt, ps, T_ps):
            P.op('dve', lambda e, pn=pn, tt=tt, ps=ps: e.tensor_tensor(out=h[:, tt, pn * 512:(pn + 1) * 512], in0=h[:, tt, pn * 512:(pn + 1) * 512], in1=ps[:], op=ALU.add),
                 reads=[T_ps, T_h[tt]], writes=[T_h[tt]])

        def copy_evac(dst_fn, T_dst):
            def ev(oc, t0, n, ps, T_ps, cnt):
                if cnt % 2 == 0:
                    P.op('act', lambda e, oc=oc, t0=t0, n=n, ps=ps: e.activation(out=dst_fn(oc, t0, n), in_=ps[:, 0:n], func=AF.Copy), reads=[T_ps], writes=[T_dst])
                else:
                    P.op('dve', lambda e, oc=oc, t0=t0, n=n, ps=ps: e.tensor_copy(dst_fn(oc, t0, n), ps[:, 0:n]), reads=[T_ps], writes=[T_dst])
            return ev

        def load_x_into_h():
            for tt in range(8):
                P.dma('sp', lambda e, tt=tt: e.dma_start(out=h[:, tt, :], in_=x.ap()[512 + tt * 128:512 + (tt + 1) * 128, :]), writes=[T_h[tt]])

        if stage >= 1 and 1 not in skip:
            for r in (R1, R2, R3, R4):
                r.reset()
            aT = R1.alloc("aT", [128, 16, 1536], BF16); T_aT = P.tile("aT")
            yaT = R2.alloc("yaT", [128, 8, 1024], BF16); T_yaT = P.tile("yaT")
            ybT = R2.alloc("ybT", [128, 8, 1024], BF16); T_ybT = P.tile("ybT")
            mT = R3.alloc("mT", [128, 16, 1024], BF16); T_mT = P.tile("mT")
            RS = Region(11 * KB, 75 * KB)
            with ExitStack() as S:
                RS.reset(); R4.reset()
                load_gb(0)
                xt = [RS.alloc("xt", [128, 2048]) for i in range(2)]; T_xt = P.tiles_n("xt", 2)

                def src_fn(tt):
                    s = tt % 2
                    P.dma('sp', lambda e, tt=tt, s=s: e.dma_start(out=xt[s][:], in_=x.ap()[tt * 128:(tt + 1) * 128, :]), writes=[T_xt[s]])
                    return xt[s][:], T_xt[s]
                rmsnorm_T(S, RS, src_fn, 12, aT, T_aT, 0)
                P.flush()
            with ExitStack() as S:
                RS.reset(); R4.reset()
                RB = Region(155 * KB, 187 * KB)
                gv = RS.alloc("gv", [128, 8, 1024]); T_gv = P.tiles_n("gv", 8)
                gu = RS.alloc("gu", [128, 8, 1024], BF16); T_gu = P.tiles_n("gu", 8)
                lngb = RS.alloc("lngb", [128, 1024]); lnbb = RS.alloc("lnbb", [128, 1024])
                T_ln = P.tile("ln"); T_ln2 = P.tile("ln2")
                wsf = RS.alloc("wsf", [128, 4, 128]); wsb = RS.alloc("wsb", [128, 4, 128], BF16); T_ws = P.tile("ws")
                bs = RS.alloc("bs", [128, 4]); T_bs = P.tile("bs")
                P.dma('sp', lambda e: e.dma_start(out=lngb[:], in_=bass.AP(lng, 0, [[0, 128], [1, 1024]])), writes=[T_ln])
                P.dma('sp', lambda e: e.dma_start(out=lnbb[:], in_=bass.AP(lnb, 0, [[0, 128], [1, 1024]])), writes=[T_ln2])
                P.dma('sp', lambda e: e.dma_start(out=wsf[:], in_=wsT.ap()), writes=[T_ws])
                P.dma('sp', lambda e: e.dma_start(out=bs[:], in_=bsT.ap()), writes=[T_bs])
                P.op('dve', lambda e: e.memset(wsf[64:128, :, 0:64], 0.0), reads=[T_ws], writes=[T_ws])
                P.op('dve', lambda e: e.tensor_copy(wsb[:], wsf[:]), reads=[T_ws], writes=[T_ws])

                def evac_uv(pn, tt, ps, T_ps):
                    if pn < 2:
                        P.op('act', lambda e, pn=pn, tt=tt, ps=ps: e.activation(out=gu[:, tt, pn * 512:(pn + 1) * 512], in_=ps[:], func=AF.Gelu),
                             reads=[T_ps], writes=[T_gu[tt]])
                    else:
                        P.op('act', lambda e, pn=pn, tt=tt, ps=ps: e.activation(out=gv[:, tt, (pn - 2) * 512:(pn - 1) * 512], in_=ps[:], func=AF.Gelu),
                             reads=[T_ps], writes=[T_gv[tt]])
                proj_tok(S, RB, w_in, 9216, 16, 0, 4, aT, T_aT, 512, 8, evac_uv)
                stt_ = R4.alloc("lnstat", [128, 8, 8]); T_lst = P.tiles_n("lnst", 8)
                tmp = R4.alloc("lnt", [128, 1024]); T_tmp = P.tile("lnt")
                tmp2 = R4.alloc("lnu", [128, 1024]); T_tmp2 = P.tile("lnu")
                junk2 = R4.alloc("junk2", [128, 1024], BF16); T_j2 = P.tile("junk2")
                vln = [R4.alloc("vln", [128, 1024], BF16) for i in range(2)]; T_vln = P.tiles_n("vln", 2)
                ya = [R4.alloc("ya", [128, 1024], BF16) for i in range(2)]; T_ya = P.tiles_n("ya", 2)
                sp_ = [PS(S, "sgp%d" % i, [128, 1024]) for i in range(2)]; T_sp = P.tiles_n("sgp", 2)
                ytp = PS(S, "ytp", [128, 8, 128], BF16); T_ytp = P.tile("ytp")
                for tt in range(8):
                    s = tt % 2
                    P.op('dve', lambda e, tt=tt: e.tensor_reduce(out=stt_[:, tt, 0:1], in_=gv[:, tt, :], axis=mybir.AxisListType.X, op=ALU.add),
                         reads=[T_gv[tt]], writes=[T_lst[tt]])
                    P.op('dve', lambda e, tt=tt: e.tensor_scalar(out=stt_[:, tt, 1:2], in0=stt_[:, tt, 0:1], scalar1=1.0 / 1024, scalar2=None, op0=ALU.mult),
                         reads=[T_lst[tt]], writes=[T_lst[tt]])
                    P.op('dve', lambda e, tt=tt: e.tensor_scalar(out=tmp[:], in0=gv[:, tt, :], scalar1=stt_[:, tt, 1:2], scalar2=None, op0=ALU.subtract),
                         reads=[T_gv[tt], T_lst[tt]], writes=[T_tmp])
                    P.op('act', lambda e, tt=tt: e.activation(out=junk2[:], in_=tmp[:], func=AF.Square, accum_out=stt_[:, tt, 2:3]),
                         reads=[T_tmp], writes=[T_j2, T_lst[tt]])
                    P.op('act', lambda e, tt=tt: e.activation(out=stt_[:, tt, 3:4], in_=stt_[:, tt, 2:3], func=AF.Sqrt, scale=1.0 / 1024, bias=EPS),
                         reads=[T_lst[tt]], writes=[T_lst[tt]])
                    P.op('dve', lambda e, tt=tt: e.reciprocal(stt_[:, tt, 4:5], stt_[:, tt, 3:4]), reads=[T_lst[tt]], writes=[T_lst[tt]])
                    P.op('dve', lambda e, tt=tt: e.scalar_tensor_tensor(out=tmp2[:], in0=tmp[:], scalar=stt_[:, tt, 4:5], in1=lngb[:], op0=ALU.mult, op1=ALU.mult),
                         reads=[T_tmp, T_lst[tt], T_ln], writes=[T_tmp2])
                    P.op('dve', lambda e, s=s: e.tensor_tensor(out=vln[s][:], in0=tmp2[:], in1=lnbb[:], op=ALU.add),
                         reads=[T_tmp2, T_ln2], writes=[T_vln[s]])
                    for g in range(4):
                        P.op('pe', lambda e, s=s, g=g: e.matmul(sp_[s][:, g * 256:(g + 1) * 256], lhsT=wsb[:, g, :], rhs=vln[s][:, g * 256:(g + 1) * 256], start=True, stop=True),
                             reads=[T_vln[s], T_ws], writes=[T_sp[s]], inc=(g == 3))
                    for g in range(4):
                        P.op('dve', lambda e, s=s, g=g, tt=tt: e.scalar_tensor_tensor(out=ya[s][:, g * 256:(g + 1) * 256], in0=sp_[s][:, g * 256:(g + 1) * 256], scalar=bs[:, g:g + 1],
                                                                               in1=gu[:, tt, g * 256:(g + 1) * 256], op0=ALU.add, op1=ALU.mult),
                             reads=[T_sp[s], T_bs, T_gu[tt]], writes=[T_ya[s]])
                    for k in range(8):
                        P.op('pe', lambda e, s=s, k=k: e.transpose(out=ytp[:, k, :], in_=ya[s][:, k * 128:(k + 1) * 128], identity=identb[:]),
                             reads=[T_ya[s], T_c2], writes=[T_ytp], inc=(k == 7))
                    P.op('act', lambda e, tt=tt: e.activation(out=yaT[:, :, tt * 128:(tt + 1) * 128], in_=ytp[:], func=AF.Copy), reads=[T_ytp], writes=[T_yaT])
                P.flush()
            with ExitStack() as S:
                RS.reset(); R4.reset()
                wq = [RS.alloc("wqkv", [128, 3, 16, 128], BF16) for i in range(2)]; T_wq = P.tiles_n("wqkv", 2)
                bt = [RS.alloc("bt", [128, 5, 128]) for i in range(2)]; T_bt = P.tiles_n("bt", 2)
                km = RS.alloc("km", [128, 12]); T_km = P.tile("km")
                P.dma('sp', lambda e: e.dma_start(out=km[:], in_=keymask.ap()), writes=[T_km])
                qT = [RS.alloc("qT", [128, 1024], BF16) for i in range(2)]; T_qT = P.tiles_n("qT", 2)
                kT = [RS.alloc("kT", [128, 1536], BF16) for i in range(2)]; T_kT = P.tiles_n("kT", 2)
                vh = [RS.alloc("vh", [128, 12, 128], BF16) for i in range(2)]; T_vh = P.tiles_n("vh", 2)
                pT = [RS.alloc("pT", [128, 5, 128], BF16) for i in range(2)]; T_pT = P.tiles_n("pT", 2)
                rr = [RS.alloc("rr", [128, 128]) for i in range(2)]; T_rr = P.tiles_n("rr", 2)
                pj = [PS(S, "pj%d" % i, [128, 512]) for i in range(2)]; T_pj = P.tiles_n("pj", 2)
                scA = PS(S, "scA", [128, 4, 128]); T_scA = P.tile("scA")
                scB = PS(S, "scB", [128, 4, 128]); T_scB = P.tile("scB")
                po = [PS(S, "po%d" % i, [128, 4, 128]) for i in range(2)]; T_po = P.tiles_n("po", 2)
                cnt = 0
                for hd in range(8):
                    s = hd % 2
                    for i3, cbase in enumerate((2048, 3072, 4096)):
                        P.dma('pool', lambda e, s=s, i3=i3, cbase=cbase, hd=hd: e.dma_start(out=wq[s][:, i3, :, :], in_=wpanel(w_in, 9216, 16, cbase + hd * 128, 128)),
                              writes=[T_wq[s]], part=(i3 > 0))
                    P.dma('sp', lambda e, s=s, hd=hd: e.dma_start(out=bt[s][:], in_=BTd.ap()[:, hd, :, :]), writes=[T_bt[s]])
                    for hf in range(2):
                        b = cnt % 2; cnt += 1
                        for kc in range(16):
                            P.op('pe', lambda e, s=s, kc=kc, b=b, hf=hf: e.matmul(pj[b][:], lhsT=wq[s][:, 0, kc, :], rhs=aT[:, kc, 512 + hf * 512:1024 + hf * 512], start=(kc == 0), stop=(kc == 15)),
                                 reads=[T_wq[s], T_aT], writes=[T_pj[b]], inc=(kc == 15))
                        P.op('act', lambda e, s=s, b=b, hf=hf: e.activation(out=qT[s][:, hf * 512:(hf + 1) * 512], in_=pj[b][:], func=AF.Copy, scale=128.0 ** -0.5),
                             reads=[T_pj[b]], writes=[T_qT[s]])
                    for hf in range(3):
                        b = cnt % 2; cnt += 1
                        for kc in range(16):
                            P.op('pe', lambda e, s=s, kc=kc, b=b, hf=hf: e.matmul(pj[b][:], lhsT=wq[s][:, 1, kc, :], rhs=aT[:, kc, hf * 512:(hf + 1) * 512], start=(kc == 0), stop=(kc == 15)),
                                 reads=[T_wq[s], T_aT], writes=[T_pj[b]], inc=(kc == 15))
                        P.op('dve', lambda e, s=s, b=b, hf=hf: e.tensor_copy(kT[s][:, hf * 512:(hf + 1) * 512], pj[b][:]),
                             reads=[T_pj[b]], writes=[T_kT[s]])
                    for g4 in range(3):
                        b = cnt % 2; cnt += 1
                        for kb in range(4):
                            blk = g4 * 4 + kb
                            for kc in range(16):
                                P.op('pe', lambda e, s=s, kc=kc, b=b, kb=kb, blk=blk: e.matmul(pj[b][:, kb * 128:(kb + 1) * 128], lhsT=aT[:, kc, blk * 128:(blk + 1) * 128], rhs=wq[s][:, 2, kc, :],
                                                                                     start=(kc == 0), stop=(kc == 15)),
                                     reads=[T_wq[s], T_aT], writes=[T_pj[b]], inc=(kc == 15 and kb == 3))
                        P.op('act', lambda e, s=s, b=b, g4=g4: e.activation(out=vh[s][:, g4 * 4:(g4 + 1) * 4, :], in_=pj[b][:].rearrange("p (a b) -> p a b", b=128), func=AF.Copy),
                             reads=[T_pj[b]], writes=[T_vh[s]])
                    for m in range(8):
                        u = m % 2
                        for ki in range(5):
                            dst = scA[:, ki, :] if ki < 4 else scB[:, 0, :]
                            Tsc = T_scA if ki < 4 else T_scB
                            P.op('pe', lambda e, s=s, ki=ki, m=m, dst=dst: e.matmul(dst, lhsT=kT[s][:, (m + ki) * 128:(m + ki + 1) * 128], rhs=qT[s][:, m * 128:(m + 1) * 128], start=True, stop=False),
                                 reads=[T_kT[s], T_qT[s]], writes=[Tsc], inc=False)
                            P.op('pe', lambda e, s=s, ki=ki, dst=dst: e.matmul(dst, lhsT=identf[:], rhs=bt[s][:, ki, :], start=False, stop=True),
                                 reads=[T_bt[s], T_c], writes=[Tsc], inc=True)
                            P.op('act', lambda e, u=u, ki=ki, m=m, dst=dst: e.activation(out=pT[u][:, ki, :], in_=dst, func=AF.Exp, bias=km[:, m + ki:m + ki + 1], scale=1.0),
                                 reads=[Tsc, T_km], writes=[T_pT[u]])
                        for ki in range(5):
                            P.op('pe', lambda e, s=s, u=u, ki=ki, m=m: e.matmul(po[u][:, 0, :], lhsT=vh[s][:, m + ki, :], rhs=pT[u][:, ki, :], start=(ki == 0), stop=(ki == 4)),
                                 reads=[T_vh[s], T_pT[u]], writes=[T_po[u]], inc=False)
                        for ki in range(5):
                            P.op('pe', lambda e, u=u, ki=ki: e.matmul(po[u][:, 1, :], lhsT=onesb[:], rhs=pT[u][:, ki, :], start=(ki == 0), stop=(ki == 4), skip_group_check=True),
                                 reads=[T_c4, T_pT[u]], writes=[T_po[u]], inc=(ki == 4))
                        P.op('dve', lambda e, u=u: e.reciprocal(rr[u][:], po[u][:, 1, :]), reads=[T_po[u]], writes=[T_rr[u]])
                        P.op('dve', lambda e, u=u, hd=hd, m=m: e.tensor_tensor(out=ybT[:, hd, m * 128:(m + 1) * 128], in0=po[u][:, 0, :], in1=rr[u][:], op=ALU.mult),
                             reads=[T_po[u], T_rr[u]], writes=[T_ybT])
                P.flush()
            with ExitStack() as S:
                RS.reset(); R4.reset()
                wg = [RS.alloc("wg", [128, 2, 16, 128], BF16) for i in range(2)]; T_wg = P.tiles_n("wg", 2)
                wu = [RS.alloc("wu", [128, 2, 8, 128], BF16) for i in range(2)]; T_wu = P.tiles_n("wu", 2)
                sg_ = [RS.alloc("sg", [128, 512]) for i in range(2)]; T_sg = P.tiles_n("sg", 2)
                m1 = [RS.alloc("m1", [128, 512]) for i in range(2)]; T_m1 = P.tiles_n("m1", 2)
                pg = [PS(S, "pg%d" % i, [128, 512]) for i in range(4)]; T_pg = P.tiles_n("pg", 4)
                pu_ = [PS(S, "pu%d" % i, [128, 512]) for i in range(4)]; T_pu = P.tiles_n("pu", 4)
                cnt = 0
                for jb in range(16):
                    s = jb % 2
                    P.dma('pool', lambda e, s=s, jb=jb: e.dma_start(out=wg[s][:, 0, :, :], in_=wpanel(w_in, 9216, 16, 5120 + jb * 128, 128)), writes=[T_wg[s]])
                    P.dma('pool', lambda e, s=s, jb=jb: e.dma_start(out=wg[s][:, 1, :, :], in_=wpanel(w_in, 9216, 16, 7168 + jb * 128, 128)), writes=[T_wg[s]], part=True)
                    P.dma('pool', lambda e, s=s, jb=jb: e.dma_start(out=wu[s][:, 0, :, :], in_=wpanel(w_up_a, 2048, 8, jb * 128, 128)), writes=[T_wu[s]])
                    P.dma('pool', lambda e, s=s, jb=jb: e.dma_start(out=wu[s][:, 1, :, :], in_=wpanel(w_up_b, 2048, 8, jb * 128, 128)), writes=[T_wu[s]], part=True)
                    for hf in range(2):
                        for br in range(2):
                            b = cnt % 4; cnt += 1
                            srcY, T_srcY = (yaT, T_yaT) if br == 0 else (ybT, T_ybT)
                            for kc in range(16):
                                P.op('pe', lambda e, s=s, br=br, kc=kc, b=b, hf=hf: e.matmul(pg[b][:], lhsT=wg[s][:, br, kc, :], rhs=aT[:, kc, 512 + hf * 512:1024 + hf * 512], start=(kc == 0), stop=(kc == 15)),
                                     reads=[T_wg[s], T_aT], writes=[T_pg[b]], inc=(kc == 15))
                            for kc in range(8):
                                P.op('pe', lambda e, s=s, br=br, kc=kc, b=b, hf=hf, srcY=srcY: e.matmul(pu_[b][:], lhsT=wu[s][:, br, kc, :], rhs=srcY[:, kc, hf * 512:(hf + 1) * 512], start=(kc == 0), stop=(kc == 7)),
                                     reads=[T_wu[s], T_srcY], writes=[T_pu[b]], inc=(kc == 7))
                            P.op('act', lambda e, br=br, b=b: e.activation(out=sg_[br][:], in_=pg[b][:], func=AF.Sigmoid), reads=[T_pg[b]], writes=[T_sg[br]])
                            P.op('dve', lambda e, b=b, br=br: e.tensor_tensor(out=m1[br][:], in0=sg_[br][:], in1=pu_[b][:], op=ALU.mult), reads=[T_sg[br], T_pu[b]], writes=[T_m1[br]])
                            if br == 1:
                                P.op('dve', lambda e, jb=jb, hf=hf: e.tensor_tensor(out=mT[:, jb, hf * 512:(hf + 1) * 512], in0=m1[0][:], in1=m1[1][:], op=ALU.add),
                                     reads=[T_m1[0], T_m1[1]], writes=[T_mT])
                P.flush()
            with ExitStack() as S:
                R1.reset()
                load_x_into_h()
                proj_tok(S, R1, w_out, 2048, 16, 0, 4, mT, T_mT, 0, 8, add_to_h)
                P.flush()
        else:
            load_x_into_h()
            P.flush()

        if stage >= 2 and 2 not in skip:
            for r in (R1, R2, R3, R4):
                r.reset()
            a2T = R1.alloc("a2T", [128, 16, 1024], BF16); T_a2T = P.tile("a2T")
            mnT = R4.alloc("mnT", [128, 16, 256], BF16); T_mnT = P.tile("mnT")
            kmT = R4.alloc("kmT", [128, 16, 256], BF16); T_kmT = P.tile("kmT")
            RX = Region(75 * KB + 32 * KB, 123 * KB)
            vm = RX.alloc("vm", [128, 2, 2048], BF16); T_vm = P.tile("vm")
            with ExitStack() as S:
                R2.reset()
                load_gb(1)
                rmsnorm_T(S, R2, lambda tt: (h[:, tt, :], T_h[tt]), 8, a2T, T_a2T, 0)
                P.flush()
            with ExitStack() as S:
                R2.reset()
                load_gb(2)
                mt = [R2.alloc("mt", [128, 2048]) for i in range(2)]; T_mt = P.tiles_n("mt", 2)

                def src_m(tt):
                    P.dma('sp', lambda e, tt=tt: e.dma_start(out=mt[tt][:], in_=memx.ap()[tt * 128:(tt + 1) * 128, :]), writes=[T_mt[tt]])
                    return mt[tt][:], T_mt[tt]
                R3.reset()
                rmsnorm_T(S, R3, src_m, 2, mnT, T_mnT, 0)
                P.flush()
            with ExitStack() as S:
                R3.reset()
                proj_feat(S, R3, mem_w_kv, 4096, 16, 0, 16, mnT, T_mnT, 0, 256, copy_evac(lambda oc, t0, n: kmT[:, oc, t0:t0 + n], T_kmT))
                P.flush()
            with ExitStack() as S:
                R3.reset()

                def ev_vm(pn, tt, ps, T_ps):
                    P.op('act', lambda e, pn=pn, tt=tt, ps=ps: e.activation(out=vm[:, tt, pn * 512:(pn + 1) * 512], in_=ps[:], func=AF.Copy), reads=[T_ps], writes=[T_vm])
                proj_tok(S, R3, mem_w_kv, 4096, 16, 2048, 4, mnT, T_mnT, 0, 2, ev_vm)
                P.flush()
            R2.reset()
            q2T = R2.alloc("q2T", [128, 16, 1024], BF16); T_q2T = P.tile("q2T")
            with ExitStack() as S:
                R3.reset()
                proj_feat(S, R3, mem_w_q, 2048, 16, 0, 16, a2T, T_a2T, 0, 1024, copy_evac(lambda oc, t0, n: q2T[:, oc, t0:t0 + n], T_q2T))
                P.flush()
            onT = a2T; T_onT = P.tile("onT")
            with ExitStack() as S:
                R3.reset()
                pT2 = [R3.alloc("pT2", [128, 2, 512], BF16) for i in range(2)]; T_pT2 = P.tiles_n("pT2", 2)
                r2 = [R3.alloc("r2", [128, 512]) for i in range(2)]; T_r2 = P.tiles_n("r2", 2)
                sc2 = [PS(S, "sc2%d" % i, [128, 512]) for i in range(2)]; T_sc2 = P.tiles_n("sc2", 2)
                rs2 = PS(S, "rs2", [128, 512]); T_rs2 = P.tile("rs2")
                o2 = [PS(S, "o2%d" % i, [128, 512]) for i in range(2)]; T_o2 = P.tiles_n("o2", 2)
                it = 0
                for hd in range(4):
                    for hf in range(2):
                        u = it % 2; it += 1
                        for mb in range(2):
                            for kc in range(4):
                                P.op('pe', lambda e, hd=hd, hf=hf, mb=mb, kc=kc: e.matmul(sc2[mb][:], lhsT=kmT[:, hd * 4 + kc, mb * 128:(mb + 1) * 128], rhs=q2T[:, hd * 4 + kc, hf * 512:(hf + 1) * 512],
                                                                                    start=(kc == 0), stop=(kc == 3)),
                                     reads=[T_kmT, T_q2T], writes=[T_sc2[mb]], inc=(kc == 3))
                            P.op('act', lambda e, u=u, mb=mb: e.activation(out=pT2[u][:, mb, :], in_=sc2[mb][:], func=AF.Exp, scale=512.0 ** -0.5),
                                 reads=[T_sc2[mb]], writes=[T_pT2[u]])
                        for mb in range(2):
                            P.op('pe', lambda e, u=u, mb=mb: e.matmul(rs2[:], lhsT=onesb[:], rhs=pT2[u][:, mb, :], start=(mb == 0), stop=(mb == 1)),
                                 reads=[T_c4, T_pT2[u]], writes=[T_rs2], inc=(mb == 1))
                        P.op('dve', lambda e, u=u: e.reciprocal(r2[u][:], rs2[:]), reads=[T_rs2], writes=[T_r2[u]])
                        for oc in range(4):
                            ob = oc % 2
                            for mb in range(2):
                                P.op('pe', lambda e, u=u, mb=mb, hd=hd, oc=oc, ob=ob: e.matmul(o2[ob][:], lhsT=vm[:, mb, hd * 512 + oc * 128:hd * 512 + (oc + 1) * 128], rhs=pT2[u][:, mb, :],
                                                                                         start=(mb == 0), stop=(mb == 1)),
                                     reads=[T_vm, T_pT2[u]], writes=[T_o2[ob]], inc=(mb == 1))
                            P.op('dve', lambda e, u=u, hd=hd, oc=oc, ob=ob, hf=hf: e.tensor_tensor(out=onT[:, hd * 4 + oc, hf * 512:(hf + 1) * 512], in0=o2[ob][:], in1=r2[u][:], op=ALU.mult),
                                 reads=[T_o2[ob], T_r2[u]], writes=[T_onT])
                P.flush()
            with ExitStack() as S:
                R2.reset()
                proj_tok(S, R2, mem_w_o, 2048, 16, 0, 4, onT, T_onT, 0, 8, add_to_h)
                P.flush()

        if stage >= 3:
            for r in (R1, R2, R3, R4):
                r.reset()
            a3T = R1.alloc("a3T", [128, 16, 1024], BF16); T_a3T = P.tile("a3T")
            RX = Region(75 * KB + 32 * KB, 123 * KB)
            qT3 = R2.alloc("qT3", [128, 32, 2, 2, 128], BF16); T_qT3 = P.tile("qT3")
            with ExitStack() as S:
                R3.reset()
                load_gb(3)
                rmsnorm_T(S, R3, lambda tt: (h[:, tt, :], T_h[tt]), 8, a3T, T_a3T, 0)
                P.flush()
            with ExitStack() as S:
                R3.reset()
                def ev_q3(oc, t0, n, ps, T_ps, cnt):
                    h_, pp = oc // 2, oc % 2
                    hh, hl = h_ // 4, h_ % 4
                    dst = bass.AP(qT3, hh * 256 + pp * 128 + hl * 32 + (t0 // 32) * 512, [[16384, 128], [512, n // 32], [1, 32]])
                    src = ps[:, 0:n].rearrange("p (a b) -> p a b", b=32)
                    if cnt % 2 == 0:
                        P.op('act', lambda e, dst=dst, src=src: e.activation(out=dst, in_=src, func=AF.Copy), reads=[T_ps], writes=[T_qT3])
                    else:
                        P.op('dve', lambda e, dst=dst, src=src: e.tensor_copy(dst, src), reads=[T_ps], writes=[T_qT3])
                proj_feat(S, R3, peer_w_q, 2048, 16, 0, 16, a3T, T_a3T, 0, 1024, ev_q3)
                P.flush()
            with ExitStack() as S:
                R3.reset(); R4.reset(); RX.reset()
                keysb = RX.alloc("keysb", [128, 2, 128], BF16); T_keys = P.tile("keys")
                P.dma('pool', lambda e: e.dma_start(out=keysb[:], in_=keysTd.ap()), writes=[T_keys])
                s_sb = [RX.alloc("s_sb", [128, 2, 128]) for i in range(8)]; T_s = P.tiles_n("s_sb", 8)
                s1p = [RX.alloc("s1p", [128, 128]) for i in range(8)]; T_s1p = P.tiles_n("s1p", 8)
                t16 = [R4.alloc("t16", [128, 2, 16]) for i in range(8)]; T_t16 = P.tiles_n("t16", 8)
                c16 = [R4.alloc("c16", [128, 16]) for i in range(8)]; T_c16 = P.tiles_n("c16", 8)
                scal = [R4.alloc("scal", [128, 8]) for i in range(8)]; T_scal = P.tiles_n("scal", 8)
                tmpk = R4.alloc("tmpk", [128, 128]); T_tmpk = P.tile("tmpk")
                cand = R4.alloc("cand", [128, 256]); T_cand = P.tile("cand")
                tmpc = R4.alloc("tmpc", [128, 256]); T_tmpc = P.tile("tmpc")
                j16 = R4.alloc("j16", [128, 16]); T_j16 = P.tile("j16")
                Dd = [R3.alloc("Dd", [128, 1024]) for i in range(4)]; T_D = P.tiles_n("Dd", 4)
                Gm = [R3.alloc("Gm", [128, 1024], BF16) for i in range(3)]; T_Gm = P.tiles_n("Gm", 3)
                Go = [R3.alloc("Go", [128, 1024], BF16) for i in range(2)]; T_Go = P.tiles_n("Go", 2)
                sps = PS(S, "sps", [128, 4, 128]); T_sps = P.tile("sps")
                gps = [PS(S, "gps0", [128, 2, 512])] * 2; T_gps = [P.tile("gps")] * 2
                T_Gd = P.tile("Gd")
                Ee = [PS(S, "Eeps%d" % i, [128, 1024]) for i in range(2)]; T_E = P.tiles_n("Ee", 2)
                for st_ in range(8):
                    tb = st_ * 128
                    for ti in range(8):
                        q, hh = ti // 2, ti % 2
                        for pp in range(2):
                            lh = qT3[:, st_ * 4 + q, hh, pp, :]
                            P.op('pe', lambda e, pp=pp, lh=lh: e.matmul(sps[:, pp, :], lhsT=lh, rhs=keysb[:, pp, :], start=True, stop=True),
                                 reads=[T_qT3, T_keys], writes=[T_sps], inc=(pp == 1))
                        P.op('act', lambda e, ti=ti: e.activation(out=s_sb[ti][:], in_=sps[:, 0:2, :], func=AF.Copy), reads=[T_sps], writes=[T_s[ti]])
                        for pp in range(2):
                            P.op('dve', lambda e, ti=ti, pp=pp: e.max(out=t16[ti][:, pp, 0:8], in_=s_sb[ti][:, pp, :]), reads=[T_s[ti]], writes=[T_t16[ti]])
                            P.op('dve', lambda e, ti=ti, pp=pp: e.match_replace(out=tmpk[:], in_to_replace=t16[ti][:, pp, 0:8], in_values=s_sb[ti][:, pp, :], imm_value=NEG),
                                 reads=[T_s[ti], T_t16[ti]], writes=[T_tmpk])
                            P.op('dve', lambda e, ti=ti, pp=pp: e.max(out=t16[ti][:, pp, 8:16], in_=tmpk[:]), reads=[T_tmpk], writes=[T_t16[ti]])
                        P.op('dve', lambda e, ti=ti: e.tensor_tensor(out=cand[:].rearrange("p (a b) -> p a b", b=16), in0=bass.AP(t16[ti], 0, [[32, 128], [1, 16], [0, 16]]),
                                                                  in1=bass.AP(t16[ti], 16, [[32, 128], [0, 16], [1, 16]]), op=ALU.add),
                             reads=[T_t16[ti]], writes=[T_cand])
                        P.op('dve', lambda e, ti=ti: e.max(out=c16[ti][:, 0:8], in_=cand[:]), reads=[T_cand], writes=[T_c16[ti]])
                        P.op('dve', lambda e, ti=ti: e.match_replace(out=tmpc[:], in_to_replace=c16[ti][:, 0:8], in_values=cand[:], imm_value=NEG),
                             reads=[T_cand, T_c16[ti]], writes=[T_tmpc])
                        P.op('dve', lambda e, ti=ti: e.max(out=c16[ti][:, 8:16], in_=tmpc[:]), reads=[T_tmpc], writes=[T_c16[ti]])
                        P.op('dve', lambda e, ti=ti: e.tensor_scalar(out=scal[ti][:, 0:1], in0=c16[ti][:, 0:1], scalar1=-1.0, scalar2=None, op0=ALU.mult),
                             reads=[T_c16[ti]], writes=[T_scal[ti]])
                        P.op('act', lambda e, ti=ti: e.activation(out=j16[:], in_=c16[ti][:], func=AF.Exp, bias=scal[ti][:, 0:1], scale=1.0, accum_out=scal[ti][:, 1:2]),
                             reads=[T_c16[ti], T_scal[ti]], writes=[T_j16, T_scal[ti]])
                        P.op('act', lambda e, ti=ti: e.activation(out=scal[ti][:, 2:3], in_=scal[ti][:, 1:2], func=AF.Ln), reads=[T_scal[ti]], writes=[T_scal[ti]])
                        P.op('dve', lambda e, ti=ti: e.scalar_tensor_tensor(out=scal[ti][:, 3:4], in0=c16[ti][:, 15:16], scalar=scal[ti][:, 0:1], in1=scal[ti][:, 2:3], op0=ALU.add, op1=ALU.subtract),
                             reads=[T_c16[ti], T_scal[ti]], writes=[T_scal[ti]])
                        P.op('dve', lambda e, ti=ti: e.tensor_scalar(out=s1p[ti][:], in0=s_sb[ti][:, 0, :], scalar1=c16[ti][:, 15:16], scalar2=None, op0=ALU.subtract),
                             reads=[T_s[ti], T_c16[ti]], writes=[T_s1p[ti]])
                    NIT = 128
                    for k in range(NIT + 2):
                        if k < NIT:
                            eb, ti = k // 8, k % 8
                            u = k % 4
                            deng = 'dve' if (k % 5 in (1, 3)) else 'pool'
                            P.op(deng, lambda e, ti=ti, eb=eb, u=u: e.tensor_tensor(out=Dd[u][:].rearrange("p (a b) -> p a b", b=128), in0=bass.AP(s1p[ti], eb * 8, [[128, 128], [1, 8], [0, 128]]),
                                                                                 in1=bass.AP(s_sb[ti], 128, [[256, 128], [0, 8], [1, 128]]), op=ALU.add),
                                 reads=[T_s1p[ti], T_s[ti]], writes=[T_D[u]])
                        k1 = k - 1
                        if 0 <= k1 < NIT:
                            ti = k1 % 8
                            P.op('act', lambda e, ti=ti, k1=k1: e.activation(out=Ee[k1 % 2][:], in_=Dd[k1 % 4][:], func=AF.Exp, bias=scal[ti][:, 3:4], scale=1.0),
                                 reads=[T_D[k1 % 4], T_scal[ti]], writes=[T_E[k1 % 2]])
                        k2 = k - 2
                        if 0 <= k2 < NIT:
                            eb, ti = k2 // 8, k2 % 8
                            q, hh = ti // 2, ti % 2
                            gsl = eb % 2
                            P.op('dve', lambda e, k2=k2: e.scalar_tensor_tensor(out=Gm[k2 % 3][:], in0=Dd[k2 % 4][:], scalar=-1e-5, in1=Ee[k2 % 2][:], op0=ALU.is_ge, op1=ALU.mult),
                                 reads=[T_D[k2 % 4], T_E[k2 % 2]], writes=[T_Gm[k2 % 3]])
                            for h2 in range(2):
                                P.op('pe', lambda e, k2=k2, h2=h2, q=q, hh=hh, gsl=gsl: e.matmul(gps[gsl][32 * q:32 * q + 32, h2, :], lhsT=sel32b[:], rhs=Gm[k2 % 3][:, h2 * 512:(h2 + 1) * 512],
                                                                                           start=(hh == 0), stop=(hh == 1), tile_position=(0, 32 * q), skip_group_check=True),
                                     reads=[T_Gm[k2 % 3], T_c3], writes=[T_gps[gsl]], inc=(h2 == 1))
                            if ti == 7:
                                P.op('act', lambda e, gsl=gsl: e.activation(out=Go[gsl][:], in_=gps[gsl][:].rearrange("p a b -> p (a b)"), func=AF.Copy), reads=[T_gps[gsl]], writes=[T_Go[gsl]])
                                P.dma('sp', lambda e, gsl=gsl, tb=tb, eb=eb: e.dma_start(out=Gd.ap()[tb:tb + 128, eb * 1024:(eb + 1) * 1024], in_=Go[gsl][:]), reads=[T_Go[gsl]], writes=[T_Gd], part=True)
                P.flush()
            with ExitStack() as S:
                R2.reset(); R3.reset(); R4.reset(); RX.reset()
                RP = Region(123 * KB, 207 * KB)
                GI = 4
                ust = [RP.alloc("ust", [128, 2048], BF16) for i in range(2)]; T_ust = P.tiles_n("ust", 2)
                UT = [RP.alloc("UT", [128, 16, 128], BF16) for i in range(2)]; T_UT = P.tiles_n("UT", 2)
                vg = [RP.alloc("vg", [128, GI, 2048], BF16) for i in range(2)]; T_vg = P.tiles_n("vg", 2)
                gg = [RP.alloc("gg", [128, 8, GI * 128], BF16) for i in range(2)]; T_gg = P.tiles_n("gg", 2)
                actT = [RP.alloc("actT", [128, GI, 1024], BF16) for i in range(2)]; T_actT = P.tiles_n("actT", 2)
                gl = [RP.alloc("gl", [128, 512], BF16) for i in range(2)]; T_gl = P.tiles_n("gl", 2)
                utp = PS(S, "utp", [128, 8, 128], BF16); T_utp = P.tile("utp")
                gtp = PS(S, "gtp", [128, 8, 128], BF16); T_gtp = P.tile("gtp")
                pps = [PS(S, "pps%d" % i, [128, 512]) for i in range(2)]; T_pps = P.tiles_n("pps", 2)
                ops_ = [PS(S, "ops%d" % i, [128, 2, 512]) for i in range(2)]; T_ops = P.tiles_n("ops", 2)
                ci = 0
                oi = 0
                for g in range(16384 // (128 * GI)):
                    gs = g % 2
                    P.dma('pool', lambda e, g=g, gs=gs: e.dma_start(out=vg[gs][:], in_=bass.AP(pv, g * GI * 128 * 2048, [[2048, 128], [128 * 2048, GI], [1, 2048]])), writes=[T_vg[gs]])
                    P.dma('sp', lambda e, g=g, gs=gs: e.dma_start(out=gg[gs][:], in_=bass.AP(Gd, g * GI * 128, [[16384, 128], [128 * 16384, 8], [1, GI * 128]])), writes=[T_gg[gs]])
                    for c in range(GI):
                        us = ci % 2; ci += 1
                        ch = g * GI + c
                        P.dma('pool', lambda e, ch=ch, us=us: e.dma_start(out=ust[us][:], in_=pu.ap()[ch * 128:(ch + 1) * 128, :]), writes=[T_ust[us]])
                        for hf in range(2):
                            for k in range(8):
                                dc = hf * 8 + k
                                P.op('pe', lambda e, us=us, k=k, dc=dc: e.transpose(out=utp[:, k, :], in_=ust[us][:, dc * 128:(dc + 1) * 128], identity=identb[:]),
                                     reads=[T_ust[us], T_c2], writes=[T_utp], inc=(k == 7))
                            if hf == 0:
                                P.op('act', lambda e, us=us: e.activation(out=UT[us][:, 0:8, :], in_=utp[:], func=AF.Copy), reads=[T_utp], writes=[T_UT[us]])
                            else:
                                P.op('dve', lambda e, us=us: e.tensor_copy(UT[us][:, 8:16, :], utp[:]), reads=[T_utp], writes=[T_UT[us]])
                        for tt in range(8):
                            P.op('pe', lambda e, gs=gs, tt=tt, c=c: e.transpose(out=gtp[:, tt, :], in_=gg[gs][:, tt, c * 128:(c + 1) * 128], identity=identb[:]),
                                 reads=[T_gg[gs], T_c2], writes=[T_gtp], inc=(tt == 7))
                        for hf in range(2):
                            for dc in range(16):
                                P.op('pe', lambda e, us=us, dc=dc, hf=hf: e.matmul(pps[hf][:], lhsT=UT[us][:, dc, :], rhs=a3T[:, dc, hf * 512:(hf + 1) * 512], start=(dc == 0), stop=(dc == 15)),
                                     reads=[T_UT[us], T_a3T], writes=[T_pps[hf]], inc=(dc == 15))
                            P.op('act', lambda e, hf=hf: e.activation(out=gl[hf][:], in_=pps[hf][:], func=AF.Gelu), reads=[T_pps[hf]], writes=[T_gl[hf]])
                            P.op('dve', lambda e, hf=hf, gs=gs, c=c: e.tensor_tensor(out=actT[gs][:, c, hf * 512:(hf + 1) * 512].rearrange("p (a b) -> p a b", b=128),
                                                                                  in0=gl[hf][:].rearrange("p (a b) -> p a b", b=128), in1=gtp[:, hf * 4:(hf + 1) * 4, :], op=ALU.mult),
                                 reads=[T_gl[hf], T_gtp], writes=[T_actT[gs]])
                    for tt in range(8):
                        for dh in range(2):
                            ob = oi % 2; oi += 1
                            for d2 in range(2):
                                db = dh * 2 + d2
                                for c in range(GI):
                                    P.op('pe', lambda e, gs=gs, c=c, tt=tt, db=db, d2=d2, ob=ob: e.matmul(ops_[ob][:, d2, :], lhsT=actT[gs][:, c, tt * 128:(tt + 1) * 128], rhs=vg[gs][:, c, db * 512:(db + 1) * 512],
                                                                                                    start=(c == 0), stop=(c == GI - 1)),
                                         reads=[T_actT[gs], T_vg[gs]], writes=[T_ops[ob]], inc=(c == GI - 1 and d2 == 1))
                            P.op('dve', lambda e, tt=tt, dh=dh, ob=ob: e.tensor_tensor(out=h[:, tt, dh * 1024:(dh + 1) * 1024], in0=h[:, tt, dh * 1024:(dh + 1) * 1024],
                                                                                  in1=ops_[ob][:].rearrange("p a b -> p (a b)"), op=ALU.add),
                                 reads=[T_ops[ob], T_h[tt]], writes=[T_h[tt]])
                P.flush()

        with ExitStack() as S:
            R1.reset()
            load_gb(4)
            ot = [R1.alloc("ot", [128, 2048]) for i in range(2)]; T_ot = P.tiles_n("ot", 2)
            junk = R1.alloc("junkf", [128, 2048], BF16); T_j = P.tile("junkf")
            stat = R1.alloc("statf", [128, 24]); T_st = P.tiles_n("stf", 8)
            T_out = P.tile("out")
            for tt in range(8):
                s = tt % 2
                c = 3 * tt
                P.op('act', lambda e, tt=tt, c=c: e.activation(out=junk[:], in_=h[:, tt, :], func=AF.Square, accum_out=stat[:, c:c + 1]), reads=[T_h[tt]], writes=[T_j, T_st[tt]])
                P.op('act', lambda e, c=c: e.activation(out=stat[:, c + 1:c + 2], in_=stat[:, c:c + 1], func=AF.Sqrt, scale=1.0 / 2048, bias=EPS), reads=[T_st[tt]], writes=[T_st[tt]])
                P.op('dve', lambda e, c=c: e.reciprocal(stat[:, c + 2:c + 3], stat[:, c + 1:c + 2]), reads=[T_st[tt]], writes=[T_st[tt]])
                P.op('dve', lambda e, tt=tt, c=c, s=s: e.scalar_tensor_tensor(out=ot[s][:], in0=h[:, tt, :], scalar=stat[:, c + 2:c + 3], in1=gb[:], op0=ALU.mult, op1=ALU.mult),
                     reads=[T_h[tt], T_st[tt], T_gb], writes=[T_ot[s]])
                P.dma('sp', lambda e, tt=tt, s=s: e.dma_start(out=out.ap()[tt * 128:(tt + 1) * 128, :], in_=ot[s][:]), reads=[T_ot[s]], writes=[T_out], part=True)
            P.flush()
        print("bass instructions emitted:", P.n_instr, "dma sems:", P.nsem)
    return nc


_CACHE = {}


def _host_consts(att_rel_bias):
    kj = np.arange(640)[:, None]
    qi = np.arange(128)[None, :]
    dist = 512 + qi - kj
    idx = np.clip(dist, -128, 128) + 128
    valid = (qi // 64 <= kj // 64) & (kj // 64 <= 8 + qi // 64)
    rb = att_rel_bias[0]
    bt = rb[:, idx]
    bt = np.where(valid[None], bt, np.float32(NEG)).astype(np.float32)
    bt = bt.reshape(8, 5, 128, 128).transpose(2, 0, 1, 3)
    return np.ascontiguousarray(bt)


def kernel(x, mem, g_mix, w_in, sg_ln_g, sg_ln_b, sg_w_s, sg_b_s, att_rel_bias, w_up_a, w_up_b, w_out,
           g_mem_q, g_mem_kv, mem_w_q, mem_w_kv, mem_w_o, g_ffn, peer_w_q, peer_sub_keys, peer_u, peer_v, g_final,
           _stage=9):
    f = lambda a: np.ascontiguousarray(np.asarray(a, dtype=np.float32))
    x = f(x); mem = f(mem)
    nc = build(_stage)
    shared = {
        "gvecs": f(np.stack([np.asarray(g_mix)[0], np.asarray(g_mem_q)[0], np.asarray(g_mem_kv)[0], np.asarray(g_ffn)[0], np.asarray(g_final)])),
        "w_in": f(np.asarray(w_in)[0]), "sg_ln_g": f(np.asarray(sg_ln_g)[0]), "sg_ln_b": f(np.asarray(sg_ln_b)[0]),
        "sg_wsT": f(np.asarray(sg_w_s)[0].transpose(2, 0, 1)),
        "sg_bsT": f(np.asarray(sg_b_s)[0].T),
        "att_bt": _host_consts(f(att_rel_bias)),
        "w_up_a": f(np.asarray(w_up_a)[0]), "w_up_b": f(np.asarray(w_up_b)[0]), "w_out": f(np.asarray(w_out)[0]),
        "mem_w_q": f(np.asarray(mem_w_q)[0]), "mem_w_kv": f(np.asarray(mem_w_kv)[0]), "mem_w_o": f(np.asarray(mem_w_o)[0]),
        "peer_w_q": f(np.asarray(peer_w_q)[0]),
        "peer_keysT": f(np.asarray(peer_sub_keys)[0].transpose(2, 0, 1)),
        "peer_u": f(np.asarray(peer_u)[0]), "peer_v": f(np.asarray(peer_v)[0]),
        "ident": np.eye(128, dtype=np.float32),
        "sel32": np.ascontiguousarray(np.tile(np.eye(32, dtype=np.float32), (4, 1))),
    }
    in_maps = []
    for k in range(8):
        b, j = k // 4, k % 4
        xc = np.zeros((1536, 2048), np.float32)
        xc[512:] = x[b, j * 1024:(j + 1) * 1024]
        km = np.zeros((128, 12), np.float32)
        if j == 0:
            km[:, 0:4] = NEG
        else:
            xc[:512] = x[b, j * 1024 - 512:j * 1024]
        m = dict(shared)
        m["x"] = xc
        m["keymask"] = km
        m["mem"] = mem[b]
        in_maps.append(m)
    res = run_bass_kernel_spmd(nc, in_maps, core_ids=list(range(8)))
    outp = np.empty((2, 4096, 2048), np.float32)
    for k in range(8):
        b, j = k // 4, k % 4
        outp[b, j * 1024:(j + 1) * 1024] = res.results[k]["out"]
    return outp
```
